# Optimizing a Trainium2 kernel written in Bass

```python
import jax, jax.numpy as jnp
from jax import lax
import numpy as np

D_MODEL = 1024
BATCH = 8
SEQ = 2048
DEPTH = 2

CHUNK = 64
GLA_HEADS = 4
GLA_DK = 64
GLA_DV = 128
GLA_GATE_RANK = 16
GLA_TAU = 16.0
RET_HEADS = 4
RET_DK = 64
RET_DV = 128
ROPE_BASE = 10000.0
GDN_HEADS = 8
GDN_DK = 128
GDN_DV = 128
CONV_WIDTH = 4
D_FF = 3584
N_EXPERTS = 8
TOP_K = 2
NORM_EPS = 1e-5
L2_EPS = 1e-6
DEEPNORM_ALPHA = (2.0 * DEPTH) ** 0.25
DEEPNORM_BETA = (8.0 * DEPTH) ** -0.25
N_EVEN = (DEPTH + 1) // 2
N_ODD = DEPTH // 2

GLA_QK = GLA_HEADS * GLA_DK
GLA_V = GLA_HEADS * GLA_DV
RET_QK = RET_HEADS * RET_DK
RET_V = RET_HEADS * RET_DV
MIX_AB = GLA_V + RET_V
AB_SIZES = (GLA_QK, GLA_QK, GLA_V, GLA_GATE_RANK, GLA_V, RET_QK, RET_QK, RET_V, RET_V)
GDN_QK = GDN_HEADS * GDN_DK
GDN_V = GDN_HEADS * GDN_DV
GDN_CONV_CH = 2 * GDN_QK + GDN_V
C_SIZES = (GDN_CONV_CH, GDN_HEADS, GDN_HEADS, GDN_V)

kernel_name = "hybrid_gla_retnet_gdn_moe_deepnorm"


def _offsets(sizes):
    return [int(s) for s in np.cumsum(sizes)[:-1]]


def layer_norm(x, g, b):
    xf = x.astype(jnp.float32)
    mu = jnp.mean(xf, -1, keepdims=True)
    xc = xf - mu
    y = xc * lax.rsqrt(jnp.mean(xc * xc, -1, keepdims=True) + NORM_EPS) * g + b
    return y.astype(x.dtype)


def head_rms_norm(x, g):
    return x * lax.rsqrt(jnp.mean(x * x, -1, keepdims=True) + NORM_EPS) * g


def head_group_norm(x, g):
    xc = x - jnp.mean(x, -1, keepdims=True)
    return xc * lax.rsqrt(jnp.mean(xc * xc, -1, keepdims=True) + NORM_EPS) * g


def l2_normalize(x):
    return x * lax.rsqrt(jnp.sum(x * x, -1, keepdims=True) + L2_EPS)


def rotary(x, positions):
    d = x.shape[-1]
    inv_freq = ROPE_BASE ** (-jnp.arange(0, d, 2, dtype=jnp.float32) / d)
    ang = positions.astype(jnp.float32)[..., None] * inv_freq
    cos, sin = jnp.cos(ang)[:, :, None, :], jnp.sin(ang)[:, :, None, :]
    x1, x2 = x[..., : d // 2], x[..., d // 2:]
    return jnp.concatenate([x1 * cos - x2 * sin, x1 * sin + x2 * cos], -1)


def to_chunks(t):
    return t.reshape(t.shape[0], t.shape[1] // CHUNK, CHUNK, *t.shape[2:])


def chunk_states(decay, d_state):
    def step(s, inp):
        dec, ds = inp
        return s * dec[..., None] + ds, s
    s0 = jnp.zeros_like(d_state[:, 0])
    _, s_in = lax.scan(step, s0, (jnp.moveaxis(decay, 1, 0), jnp.moveaxis(d_state, 1, 0)))
    return jnp.moveaxis(s_in, 0, 1)


def gla_chunked(q, k, v, log_a):
    B, T, H, dk = q.shape
    q, k, v, log_a = to_chunks(q * dk ** -0.5), to_chunks(k), to_chunks(v), to_chunks(log_a)
    b = jnp.cumsum(log_a, axis=2)
    b_last = b[:, :, -1]
    q_dec = q * jnp.exp(b)
    causal = jnp.tril(jnp.ones((CHUNK, CHUNK), dtype=bool))
    att = jnp.einsum('bnihk,bnjhk->bnhij', q_dec, k * jnp.exp(-b))
    att = jnp.where(causal, att, 0.0)
    o_intra = jnp.einsum('bnhij,bnjhv->bnihv', att, v)
    k_end = k * jnp.exp(b_last[:, :, None] - b)
    d_state = jnp.einsum('bnjhk,bnjhv->bnhkv', k_end, v)
    s_in = chunk_states(jnp.exp(b_last), d_state)
    o_inter = jnp.einsum('bnihk,bnhkv->bnihv', q_dec, s_in)
    return (o_intra + o_inter).reshape(B, T, H, -1)


def retention_chunked(q, k, v):
    B, T, H, dk = q.shape
    log_gamma = jnp.log(1.0 - 2.0 ** (-5.0 - jnp.arange(H, dtype=jnp.float32)))
    q, k, v = to_chunks(q * dk ** -0.5), to_chunks(k), to_chunks(v)
    pos = jnp.arange(CHUNK, dtype=jnp.float32)
    diff = pos[:, None] - pos[None, :]
    dmat = jnp.where(diff >= 0, jnp.exp(log_gamma[:, None, None] * jnp.maximum(diff, 0.0)), 0.0)
    att = jnp.einsum('bnihk,bnjhk->bnhij', q, k) * dmat
    o_intra = jnp.einsum('bnhij,bnjhv->bnihv', att, v)
    xi = jnp.exp(log_gamma[None, :] * (pos[:, None] + 1.0))[:, :, None]
    zeta = jnp.exp(log_gamma[None, :] * (CHUNK - 1.0 - pos[:, None]))[:, :, None]
    d_state = jnp.einsum('bnjhk,bnjhv->bnhkv', k * zeta, v)
    decay = jnp.broadcast_to(jnp.exp(log_gamma * CHUNK)[:, None], d_state.shape[:-1])
    s_in = chunk_states(decay, d_state)
    o_inter = jnp.einsum('bnihk,bnhkv->bnihv', q, s_in) * xi
    return (o_intra + o_inter).reshape(B, T, H, -1)


def causal_conv(x, w):
    return lax.conv_general_dilated(
        x, w[:, None, :], window_strides=(1,), padding=[(CONV_WIDTH - 1, 0)],
        dimension_numbers=('NWC', 'WIO', 'NWC'), feature_group_count=x.shape[-1])


def gated_delta_rule_chunked(q, k, v, beta, g):
    B, T, H, dk = q.shape
    dv = v.shape[-1]
    n = T // CHUNK
    hc = lambda t: jnp.swapaxes(to_chunks(t), 2, 3)
    qc, kc, vc = hc(q * dk ** -0.5), hc(k), hc(v)
    bc, gc = hc(beta), jnp.cumsum(hc(g), axis=-1)
    incl = jnp.tril(jnp.ones((CHUNK, CHUNK), dtype=bool))
    strict = jnp.tril(jnp.ones((CHUNK, CHUNK), dtype=bool), -1)
    decay = jnp.exp(jnp.where(incl, gc[..., :, None] - gc[..., None, :], -jnp.inf))
    k_beta = kc * bc[..., None]
    lower = jnp.where(strict, jnp.einsum('bnhik,bnhjk->bnhij', k_beta, kc) * decay, 0.0)
    eye = jnp.broadcast_to(jnp.eye(CHUNK, dtype=jnp.float32), lower.shape)
    t_inv = lax.linalg.triangular_solve(lower, eye, left_side=True, lower=True, unit_diagonal=True)
    u = jnp.einsum('bnhij,bnhjv->bnhiv', t_inv, vc * bc[..., None])
    w = jnp.einsum('bnhij,bnhjk->bnhik', t_inv, k_beta * jnp.exp(gc)[..., None])
    a_qk = jnp.einsum('bnhik,bnhjk->bnhij', qc, kc) * decay
    q_dec = qc * jnp.exp(gc)[..., None]
    g_last = gc[..., -1]
    k_dec = kc * jnp.exp(g_last[..., None] - gc)[..., None]

    def step(s, inp):
        u_n, w_n, a_n, qd_n, kd_n, gl_n = inp
        v_new = u_n - jnp.einsum('bhck,bhkv->bhcv', w_n, s)
        o_n = jnp.einsum('bhck,bhkv->bhcv', qd_n, s) + jnp.einsum('bhij,bhjv->bhiv', a_n, v_new)
        s = s * jnp.exp(gl_n)[..., None, None] + jnp.einsum('bhck,bhcv->bhkv', kd_n, v_new)
        return s, o_n

    s0 = jnp.zeros((B, H, dk, dv), jnp.float32)
    xs = tuple(jnp.moveaxis(t, 1, 0) for t in (u, w, a_qk, q_dec, k_dec, g_last))
    _, o = lax.scan(step, s0, xs)
    return jnp.transpose(o, (1, 0, 3, 2, 4)).reshape(B, T, H, dv)


def mixer_gla_retention(x, positions, w_in, gla_w_gate2, gla_b_gate, gla_norm, ret_norm, w_out):
    B, T, _ = x.shape
    f32 = jnp.float32
    h = x @ w_in
    gq, gk, gv, ga, gr, rq, rk, rv, rg = jnp.split(h, _offsets(AB_SIZES), axis=-1)
    heads = lambda t, nh: t.astype(f32).reshape(B, T, nh, -1)
    log_a = jax.nn.log_sigmoid((ga @ gla_w_gate2 + gla_b_gate).astype(f32)) / GLA_TAU
    o_a = gla_chunked(heads(gq, GLA_HEADS), heads(gk, GLA_HEADS), heads(gv, GLA_HEADS), heads(log_a, GLA_HEADS))
    o_a = head_rms_norm(o_a, gla_norm).reshape(B, T, GLA_V) * jax.nn.silu(gr.astype(f32))
    q_r = rotary(heads(rq, RET_HEADS), positions)
    k_r = rotary(heads(rk, RET_HEADS), positions)
    o_b = retention_chunked(q_r, k_r, heads(rv, RET_HEADS))
    o_b = head_group_norm(o_b, ret_norm).reshape(B, T, RET_V) * jax.nn.silu(rg.astype(f32))
    o = jnp.concatenate([o_a, o_b], axis=-1).astype(x.dtype)
    return o @ w_out


def mixer_gated_deltanet(x, w_in, conv_w, a_log, dt_bias, out_norm, w_out):
    B, T, _ = x.shape
    f32 = jnp.float32
    h = x @ w_in
    qkv, a_in, b_in, gate = jnp.split(h, _offsets(C_SIZES), axis=-1)
    qkv = jax.nn.silu(causal_conv(qkv, conv_w)).astype(f32)
    q, k, v = jnp.split(qkv, [GDN_QK, 2 * GDN_QK], axis=-1)
    q = l2_normalize(q.reshape(B, T, GDN_HEADS, GDN_DK))
    k = l2_normalize(k.reshape(B, T, GDN_HEADS, GDN_DK))
    v = v.reshape(B, T, GDN_HEADS, GDN_DV)
    beta = jax.nn.sigmoid(b_in.astype(f32))
    g = -jnp.exp(a_log.astype(f32)) * jax.nn.softplus(a_in.astype(f32) + dt_bias)
    o = gated_delta_rule_chunked(q, k, v, beta, g)
    o = head_rms_norm(o, out_norm).reshape(B, T, GDN_V) * jax.nn.silu(gate.astype(f32))
    return o.astype(x.dtype) @ w_out


def swiglu(x, w_gu, w_down):
    gt, up = jnp.split(x @ w_gu, 2, axis=-1)
    return (jax.nn.silu(gt) * up) @ w_down


def moe_swiglu(x, w_router, b_router, w_gu, w_down):
    B, T, D = x.shape
    xt = x.reshape(B * T, D)
    logits = (xt @ w_router + b_router).astype(jnp.float32)
    top_val, top_idx = lax.top_k(logits, TOP_K)
    gates = jax.nn.softmax(top_val, axis=-1)
    combine = jnp.sum(jax.nn.one_hot(top_idx, N_EXPERTS, dtype=jnp.float32) * gates[..., None], axis=1)
    y = jnp.zeros_like(xt)
    for e in range(N_EXPERTS):
        y = y + combine[:, e:e + 1].astype(x.dtype) * swiglu(xt, w_gu[e], w_down[e])
    return y.reshape(B, T, D)


def setup_inputs(seed: int = 0) -> dict:
    key = jax.random.key(seed)
    ks = iter(jax.random.split(key, 40))
    nrm = lambda shape, scale: jax.random.normal(next(ks), shape, jnp.float32) * scale
    D, E, O = D_MODEL, N_EVEN, N_ODD
    x = jax.random.normal(next(ks), (BATCH, SEQ, D), jnp.float32)
    offset = jax.random.randint(next(ks), (BATCH, 1), 0, 4096, dtype=jnp.int32)
    positions = offset + jnp.arange(SEQ, dtype=jnp.int32)[None, :]
    ab_w_in = nrm((E, D, sum(AB_SIZES)), D ** -0.5)
    gla_w_gate2 = nrm((E, GLA_GATE_RANK, GLA_QK), GLA_GATE_RANK ** -0.5)
    gla_b_gate = nrm((E, GLA_QK), 0.1)
    gla_norm = 1.0 + nrm((E, GLA_HEADS, GLA_DV), 0.02)
    ret_norm = 1.0 + nrm((E, RET_HEADS, RET_DV), 0.02)
    ab_w_out = nrm((E, MIX_AB, D), MIX_AB ** -0.5 * DEEPNORM_BETA)
    ab_ln1_g = 1.0 + nrm((E, D), 0.02)
    ab_ln1_b = nrm((E, D), 0.02)
    ffn_w_gu = nrm((E, D, 2 * D_FF), D ** -0.5)
    ffn_w_down = nrm((E, D_FF, D), D_FF ** -0.5 * DEEPNORM_BETA)
    ab_ln2_g = 1.0 + nrm((E, D), 0.02)
    ab_ln2_b = nrm((E, D), 0.02)
    c_w_in = nrm((O, D, sum(C_SIZES)), D ** -0.5)
    c_conv_w = nrm((O, CONV_WIDTH, GDN_CONV_CH), CONV_WIDTH ** -0.5)
    c_a_log = jnp.log(jax.random.uniform(next(ks), (O, GDN_HEADS), jnp.float32, 1.0, 16.0))
    dt = jnp.exp(jax.random.uniform(next(ks), (O, GDN_HEADS), jnp.float32,
                                    float(np.log(1e-3)), float(np.log(1e-1))))
    c_dt_bias = dt + jnp.log(-jnp.expm1(-dt))
    c_norm = 1.0 + nrm((O, GDN_HEADS, GDN_DV), 0.02)
    c_w_out = nrm((O, GDN_V, D), GDN_V ** -0.5 * DEEPNORM_BETA)
    c_ln1_g = 1.0 + nrm((O, D), 0.02)
    c_ln1_b = nrm((O, D), 0.02)
    moe_w_router = nrm((O, D, N_EXPERTS), D ** -0.5)
    moe_b_router = nrm((O, N_EXPERTS), 0.01)
    moe_w_gu = nrm((O, N_EXPERTS, D, 2 * D_FF), D ** -0.5)
    moe_w_down = nrm((O, N_EXPERTS, D_FF, D), D_FF ** -0.5 * DEEPNORM_BETA)
    c_ln2_g = 1.0 + nrm((O, D), 0.02)
    c_ln2_b = nrm((O, D), 0.02)
    return {"x": x, "positions": positions,
            "ab_w_in": ab_w_in, "gla_w_gate2": gla_w_gate2, "gla_b_gate": gla_b_gate,
            "gla_norm": gla_norm, "ret_norm": ret_norm, "ab_w_out": ab_w_out,
            "ab_ln1_g": ab_ln1_g, "ab_ln1_b": ab_ln1_b, "ffn_w_gu": ffn_w_gu, "ffn_w_down": ffn_w_down,
            "ab_ln2_g": ab_ln2_g, "ab_ln2_b": ab_ln2_b,
            "c_w_in": c_w_in, "c_conv_w": c_conv_w, "c_a_log": c_a_log, "c_dt_bias": c_dt_bias,
            "c_norm": c_norm, "c_w_out": c_w_out, "c_ln1_g": c_ln1_g, "c_ln1_b": c_ln1_b,
            "moe_w_router": moe_w_router, "moe_b_router": moe_b_router,
            "moe_w_gu": moe_w_gu, "moe_w_down": moe_w_down, "c_ln2_g": c_ln2_g, "c_ln2_b": c_ln2_b}


def reference(x, positions, ab_w_in, gla_w_gate2, gla_b_gate, gla_norm, ret_norm, ab_w_out,
              ab_ln1_g, ab_ln1_b, ffn_w_gu, ffn_w_down, ab_ln2_g, ab_ln2_b,
              c_w_in, c_conv_w, c_a_log, c_dt_bias, c_norm, c_w_out, c_ln1_g, c_ln1_b,
              moe_w_router, moe_b_router, moe_w_gu, moe_w_down, c_ln2_g, c_ln2_b):
    for layer in range(DEPTH):
        i = layer // 2
        if layer % 2 == 0:
            h = mixer_gla_retention(x, positions, ab_w_in[i], gla_w_gate2[i], gla_b_gate[i],
                                    gla_norm[i], ret_norm[i], ab_w_out[i])
            x = layer_norm(DEEPNORM_ALPHA * x + h, ab_ln1_g[i], ab_ln1_b[i])
            x = layer_norm(DEEPNORM_ALPHA * x + swiglu(x, ffn_w_gu[i], ffn_w_down[i]),
                           ab_ln2_g[i], ab_ln2_b[i])
        else:
            h = mixer_gated_deltanet(x, c_w_in[i], c_conv_w[i], c_a_log[i], c_dt_bias[i],
                                     c_norm[i], c_w_out[i])
            x = layer_norm(DEEPNORM_ALPHA * x + h, c_ln1_g[i], c_ln1_b[i])
            y = moe_swiglu(x, moe_w_router[i], moe_b_router[i], moe_w_gu[i], moe_w_down[i])
            x = layer_norm(DEEPNORM_ALPHA * x + y, c_ln2_g[i], c_ln2_b[i])
    return x
```

```python
import contextlib
from concourse.bass_utils import run_bass_kernel_spmd
import numpy as np
import concourse.bass as bass
import concourse.mybir as mybir

F32 = mybir.dt.float32
BF16 = mybir.dt.bfloat16
I32 = mybir.dt.int32
AF = mybir.ActivationFunctionType
ALU = mybir.AluOpType
AX = mybir.AxisListType

ENGS = ("pe", "act", "dve", "pool", "sp")
N_DMA_SLOTS = 24


class _Op:
    __slots__ = ("eng", "fn", "reads", "writes", "dma", "deps", "signal", "idx",
                 "chan", "val", "slot", "region")

    def __init__(self, eng, fn, reads, writes, dma):
        self.eng = eng
        self.fn = fn
        self.reads = reads
        self.writes = writes
        self.dma = dma
        self.deps = ()
        self.signal = dma
        self.chan = None
        self.val = 0
        self.slot = None


def _conflict(a, b):
    n = min(len(a), len(b))
    return a[:n] == b[:n]


class Prog:
    def __init__(self, nc):
        self.nc = nc
        self.ops = []
        self.state = {}
        self.pending = {e: set() for e in ENGS}
        self.bar_start = 0
        self.cur_region = None
        self.regions = {}

    def barrier(self):
        deps = set()
        last = {}
        for o in self.ops[self.bar_start:]:
            if o.dma:
                deps.add(o.idx)
            else:
                last[o.eng] = o.idx
        deps |= set(last.values())
        for e in ENGS:
            self.pending[e] |= deps
        self.bar_start = len(self.ops)

    def op(self, eng, fn, reads=(), writes=(), dma=False):
        rk = tuple(self._k(r) for r in reads)
        wk = tuple(self._k(w) for w in writes)
        wk = wk + tuple(r for r in rk if r[0] == "ps" and r not in wk)
        o = _Op(eng, fn, rk, wk, dma)
        o.region = self.cur_region
        o.idx = len(self.ops)
        deps = set()
        for k in o.reads:
            grp = self.state.get(k[0])
            if grp:
                for k2, st in grp.items():
                    if _conflict(k, k2) and st[0] is not None:
                        deps.add(st[0])
        for k in o.writes:
            grp = self.state.setdefault(k[0], {})
            dead = []
            for k2, st in grp.items():
                if _conflict(k, k2):
                    if st[0] is not None:
                        deps.add(st[0])
                    deps.update(st[1])
                    if len(k2) >= len(k):
                        dead.append(k2)
            for k2 in dead:
                del grp[k2]
        for k in o.writes:
            self.state[k[0]][k] = [o.idx, []]
        for k in o.reads:
            grp = self.state.setdefault(k[0], {})
            if k in grp:
                grp[k][1].append(o.idx)
            else:
                w = None
                for k2, st in grp.items():
                    if _conflict(k, k2) and st[0] is not None:
                        w = st[0] if w is None else max(w, st[0])
                grp[k] = [w, [o.idx]]
        if self.pending[eng]:
            deps |= self.pending[eng]
            self.pending[eng] = set()
        deps.discard(o.idx)
        real = []
        best = {}
        for d in deps:
            od = self.ops[d]
            if o.eng == "pe" and od.eng == "pe" and not od.dma and not o.dma:
                continue
            if od.dma:
                real.append(d)
            elif d > best.get(od.eng, -1):
                best[od.eng] = d
        real.extend(best.values())
        for d in real:
            self.ops[d].signal = True
        o.deps = tuple(sorted(real))
        self.ops.append(o)
        return o

    def begin_region(self, flag_ap, flag_key):
        rid = len(self.regions) + 1
        fdep = None
        grp = self.state.get(flag_key[0])
        if grp:
            for k2, st in grp.items():
                if _conflict(flag_key, k2) and st[0] is not None:
                    fdep = st[0] if fdep is None else max(fdep, st[0])
        if fdep is not None:
            self.ops[fdep].signal = True
        self.regions[rid] = (flag_ap, fdep)
        self.cur_region = rid

    def end_region(self):
        self.cur_region = None

    @staticmethod
    def _k(k):
        return k if isinstance(k, tuple) else (k,)

    def emit(self, extra_ctx=()):
        nc = self.nc
        import contextlib
        with contextlib.ExitStack() as es:
            sems = {e: es.enter_context(nc.semaphore("s_" + e)) for e in ENGS}
            dsem = {}
            for q in ("sp", "act", "pool"):
                dsem[q] = [es.enter_context(nc.semaphore("d_%s_%d" % (q, i))) for i in range(N_DMA_SLOTS)]
            cnt = {e: 0 for e in ENGS}
            dcnt = {q: 0 for q in dsem}
            slot_uses = {q: [0] * N_DMA_SLOTS for q in dsem}
            for o in self.ops:
                if o.dma:
                    q = o.eng
                    s = dcnt[q] % N_DMA_SLOTS
                    dcnt[q] += 1
                    slot_uses[q][s] += 1
                    o.slot = s
                    o.chan = ("d", q, s)
                    o.val = 16 * slot_uses[q][s]
                elif o.signal:
                    cnt[o.eng] += 1
                    o.chan = ("c", o.eng)
                    o.val = cnt[o.eng]
            self.n_sig = dict(cnt)
            per = {e: [o for o in self.ops if o.eng == e] for e in ENGS}

            def semof(chan):
                if chan[0] == "c":
                    return sems[chan[1]]
                return dsem[chan[1]][chan[2]]

            ETYPE = {"pe": mybir.EngineType.PE, "act": mybir.EngineType.Activation, "dve": mybir.EngineType.DVE,
                     "pool": mybir.EngineType.Pool, "sp": mybir.EngineType.SP}
            flagregs = nc.alloc_registers("rflag", engines=list(ETYPE.values())) if self.regions else None

            def emit_one(o, eng, waited):
                need = {}
                for d in o.deps:
                    od = self.ops[d]
                    if od.val > need.get(od.chan, 0):
                        need[od.chan] = od.val
                if o.dma and o.val > 16:
                    ch = o.chan
                    if o.val - 16 > need.get(ch, 0):
                        need[ch] = o.val - 16
                for ch, v in need.items():
                    if waited.get(ch, 0) >= v:
                        continue
                    eng.wait_ge(semof(ch), v)
                    waited[ch] = v
                ins = o.fn(eng)
                if o.chan is not None:
                    ins.then_inc(semof(o.chan), 16 if o.dma else 1)

            def run(e, eng):
                waited = {}
                ops = per[e]
                i = 0
                while i < len(ops):
                    o = ops[i]
                    if o.region is None:
                        emit_one(o, eng, waited)
                        i += 1
                        continue
                    j = i
                    while j < len(ops) and ops[j].region == o.region:
                        j += 1
                    grp = ops[i:j]
                    rid = o.region
                    flag_ap, fdep = self.regions[rid]
                    if fdep is not None:
                        od = self.ops[fdep]
                        if waited.get(od.chan, 0) < od.val:
                            eng.wait_ge(semof(od.chan), od.val)
                            waited[od.chan] = od.val
                    reg = flagregs[ETYPE[e]]
                    eng.reg_load(reg, flag_ap)
                    snap = dict(waited)
                    with eng.If_ne(reg, 0):
                        for g in grp:
                            emit_one(g, eng, waited)
                    comp = {}
                    for g in grp:
                        if g.chan is not None:
                            comp[g.chan] = comp.get(g.chan, 0) + (16 if g.dma else 1)
                    if comp:
                        with eng.Else():
                            for ch, c in comp.items():
                                eng.sem_inc(semof(ch), c)
                    waited.clear()
                    waited.update(snap)
                    i = j
            last = {}
            for o in self.ops:
                if o.dma:
                    last[o.chan] = max(last.get(o.chan, 0), o.val)

            with nc.Block() as block:
                @block.tensor
                def _(eng):
                    run("pe", eng)

                @block.scalar
                def _(eng):
                    run("act", eng)

                @block.vector
                def _(eng):
                    run("dve", eng)

                @block.gpsimd
                def _(eng):
                    run("pool", eng)

                @block.sync
                def _(eng):
                    run("sp", eng)
                    for ch, v in last.items():
                        eng.wait_ge(semof(ch), v)


T = 2048
NT = 16
D = 1024
ALPHA = 4.0 ** 0.25
EPS = 1e-5


class B:
    __slots__ = ("ap", "key")

    def __init__(self, ap, key):
        self.ap = ap
        self.key = key if isinstance(key, tuple) else (key,)

    def __getitem__(self, idx):
        return B(self.ap[idx], self.key)

    def sub(self, idx, *k):
        return B(self.ap[idx], self.key + tuple(k))

    def k(self, *k):
        return B(self.ap, self.key + tuple(k))

    def re(self, pat, **kw):
        return B(self.ap.rearrange(pat, **kw), self.key)

    def bc(self, shape, axis):
        return B(self.ap.unsqueeze(axis).to_broadcast(shape), self.key)


def _keys(*bs):
    return [b.key for b in bs if isinstance(b, B)]


def _a(x):
    return x.ap if isinstance(x, B) else x


class K:
    def __init__(self, nc, arena, arena_cols, psum_banks):
        self.nc = nc
        self.P = Prog(nc)
        self.arena = arena
        self.cols = arena_cols
        self.off = 0
        self.banks = psum_banks
        self.nbank = 0
        self.uid = 0
        self.defer = None
        self.bank_range = (0, len(psum_banks))
        self.bank_ctr = {}

    def _op(self, *a, **kw):
        if self.defer is not None:
            self.defer.append((a, kw))
        else:
            self.P.op(*a, **kw)

    def record(self, fn, banks=None):
        old = self.defer
        oldb = self.bank_range
        self.defer = []
        if banks is not None:
            self.bank_range = banks
        fn()
        ops = self.defer
        self.defer = old
        self.bank_range = oldb
        return ops

    @staticmethod
    def merge_lists(a, b):
        out = []
        na, nb = len(a), len(b)
        ia = ib = 0
        while ia < na or ib < nb:
            if ib >= nb or (ia < na and ia * nb <= ib * na):
                out.append(a[ia]); ia += 1
            else:
                out.append(b[ib]); ib += 1
        return out

    def issue_merged(self, a, b):
        na, nb = len(a), len(b)
        ia = ib = 0
        while ia < na or ib < nb:
            if ib >= nb or (ia < na and ia * nb <= ib * na):
                x = a[ia]; ia += 1
            else:
                x = b[ib]; ib += 1
            self._op(*x[0], **x[1])

    def alloc(self, name, free_shape, dt=F32):
        n = int(np.prod(free_shape))
        words = n if dt == F32 or dt == I32 else (n + 1) // 2
        assert self.off + words <= self.cols, (name, self.off, words, self.cols)
        ap = self.arena[:, self.off:self.off + words]
        self.off += words
        if dt == BF16:
            ap = ap.bitcast(BF16)
            if ap.shape[-1] != n:
                ap = ap[:, 0:n]
        elif dt == I32:
            ap = ap.bitcast(I32)
        if len(free_shape) == 2:
            ap = ap.rearrange("p (a b) -> p a b", b=free_shape[1])
        elif len(free_shape) == 3:
            ap = ap.rearrange("p (a b c) -> p a b c", b=free_shape[1], c=free_shape[2])
        return B(ap, name)

    def mark(self):
        return self.off

    def reset(self, m):
        self.off = m

    def psum(self, dt=F32):
        lo, hi = self.bank_range
        b = lo + self.bank_ctr.setdefault((lo, hi), 0) % (hi - lo)
        self.bank_ctr[(lo, hi)] += 1
        ap = self.banks[b]
        if dt == BF16:
            ap = ap.bitcast(BF16)
        return B(ap, ("ps", b))

    def mm(self, out, lhsT, rhs, start=True, stop=True):
        self._op("pe", lambda e: e.matmul(out.ap, lhsT.ap, rhs.ap, start=start, stop=stop),
                  reads=_keys(lhsT, rhs), writes=_keys(out))

    def tr(self, out, in_, ident):
        self._op("pe", lambda e: e.transpose(out.ap, in_.ap, ident.ap),
                  reads=_keys(in_, ident), writes=_keys(out))

    def act(self, out, in_, func, bias=None, scale=1.0, accum=None):
        kw = {}
        if bias is not None:
            kw["bias"] = _a(bias)
        if accum is not None:
            kw["accum_out"] = accum.ap
        self._op("act", lambda e: e.activation(out.ap, in_.ap, func, scale=_a(scale), **kw),
                  reads=_keys(in_, bias, scale), writes=_keys(out, accum))

    def tt(self, out, a, b, op, eng="dve"):
        self._op(eng, lambda e: e.tensor_tensor(out.ap, a.ap, b.ap, op), reads=_keys(a, b), writes=_keys(out))

    def ts(self, out, a, s1, s2, op0, op1=None, eng="dve"):
        if op1 is None:
            f = lambda e: e.tensor_scalar(out.ap, a.ap, _a(s1), None, op0)
        else:
            f = lambda e: e.tensor_scalar(out.ap, a.ap, _a(s1), _a(s2), op0, op1)
        self._op(eng, f, reads=_keys(a, s1, s2), writes=_keys(out))

    def stt(self, out, a, s, b, op0, op1, eng="dve"):
        self._op(eng, lambda e: e.scalar_tensor_tensor(out.ap, a.ap, _a(s), b.ap, op0, op1),
                  reads=_keys(a, s, b), writes=_keys(out))

    def cp(self, out, in_, eng="dve"):
        if eng == "act":
            self._op("act", lambda e: e.copy(out.ap, in_.ap), reads=_keys(in_), writes=_keys(out))
        else:
            self._op(eng, lambda e: e.tensor_copy(out.ap, in_.ap), reads=_keys(in_), writes=_keys(out))

    def red(self, out, in_, op=None, eng="dve"):
        op = op or ALU.add
        self._op(eng, lambda e: e.tensor_reduce(out.ap, in_.ap, AX.X, op), reads=_keys(in_), writes=_keys(out))

    def recip(self, out, in_):
        self._op("dve", lambda e: e.reciprocal(out.ap, in_.ap), reads=_keys(in_), writes=_keys(out))

    def memset(self, out, val, eng="dve"):
        self._op(eng, lambda e: e.memset(out.ap, val), writes=_keys(out))

    def dma(self, q, out, in_):
        rk = _keys(in_)
        wk = _keys(out)
        self._op(q, lambda e: e.dma_start(out=_a(out), in_=_a(in_)), reads=rk, writes=wk, dma=True)


def host_consts():
    c = {}
    c["ident"] = np.eye(128, dtype=np.float32)
    j = np.arange(128)[:, None]
    i = np.arange(128)[None, :]
    c["tri"] = (j <= i).astype(np.float32)
    c["upp"] = (j > i).astype(np.float32)
    c["ones"] = np.ones((128, 128), np.float32)
    lg = np.log(1.0 - 2.0 ** (-5.0 - np.arange(4, dtype=np.float64)))
    p = np.arange(128, dtype=np.float64)[:, None]
    eq = 0.125 * np.exp(lg[None, :] * (p + 1.0))
    ek = np.exp(-lg[None, :] * (p + 1.0))
    ee = np.exp(lg[None, :] * (127.0 - p))
    rep = lambda a: np.repeat(a, 64, axis=1)
    c["ret_eqk"] = np.concatenate([rep(eq), rep(ek)], 1).astype(np.float32)
    c["ret_ee"] = rep(ee).astype(np.float32)
    dS = np.exp(lg * 128.0)
    ds = np.zeros((128, 2), np.float64)
    for h in range(4):
        ds[(h % 2) * 64:(h % 2) * 64 + 64, h // 2] = dS[h]
    c["ret_ds"] = ds.astype(np.float32)
    c["inv_freq"] = (10000.0 ** (-np.arange(0, 64, 2, dtype=np.float32) / 64)).astype(np.float32)[None, :]
    return c


L0_PERM = None


def l0_perm():
    r = lambda a, b: list(range(a, b))
    return np.array(r(0, 256) + r(256, 512) + r(1552, 1808) + r(1808, 2064) + r(512, 1024) + r(2064, 2576)
                    + r(1040, 1552) + r(2576, 3088) + r(1024, 1040))


def layer_norm_tile(k, Y, g_bc, b_bc, out, scr):
    st = scr["st"]
    junk = scr["junk"]
    k.red(st[:, 0:1].k(0), Y)
    k.tt(junk, Y, Y, ALU.mult, eng="pool")
    k.red(st[:, 1:2].k(1), junk)
    k.ts(st[:, 2:3].k(2), st[:, 0:1].k(0), 1.0 / D, None, ALU.mult)
    k.tt(st[:, 3:4].k(3), st[:, 2:3].k(2), st[:, 2:3].k(2), ALU.mult)
    k.stt(st[:, 4:5].k(4), st[:, 1:2].k(1), 1.0 / D, st[:, 3:4].k(3), ALU.mult, ALU.subtract)
    k.act(st[:, 5:6].k(5), st[:, 4:5].k(4), AF.Sqrt, bias=scr["eps"], scale=1.0)
    k.recip(st[:, 6:7].k(6), st[:, 5:6].k(5))
    k.stt(st[:, 7:8].k(7), st[:, 2:3].k(2), -1.0, st[:, 6:7].k(6), ALU.mult, ALU.mult)
    k.act(junk, Y, AF.Identity, bias=st[:, 7:8].k(7), scale=st[:, 6:7].k(6))
    k.tt(junk, junk, g_bc, ALU.mult, eng="pool")
    k.tt(out, junk, b_bc, ALU.add)


def ln_tile(k, Y, g_bc, b_bc, out, st, junk):
    k.red(st[:, 0:1].k(0), Y)
    k.act(junk, Y, AF.Square)
    k.red(st[:, 1:2].k(1), junk)
    k.ts(st[:, 2:3].k(2), st[:, 0:1].k(0), 1.0 / D, None, ALU.mult)
    k.tt(st[:, 3:4].k(3), st[:, 2:3].k(2), st[:, 2:3].k(2), ALU.mult)
    k.stt(st[:, 4:5].k(4), st[:, 1:2].k(1), 1.0 / D, st[:, 3:4].k(3), ALU.mult, ALU.subtract)
    k.act(st[:, 5:6].k(5), st[:, 4:5].k(4), AF.Sqrt, bias=EPS, scale=1.0)
    k.recip(st[:, 6:7].k(6), st[:, 5:6].k(5))
    k.stt(st[:, 7:8].k(7), st[:, 2:3].k(2), -1.0, st[:, 6:7].k(6), ALU.mult, ALU.mult)
    k.act(junk, Y, AF.Identity, bias=st[:, 7:8].k(7), scale=st[:, 6:7].k(6))
    k.tt(junk, junk, g_bc, ALU.mult)
    k.tt(out, junk, b_bc, ALU.add)


def load_consts(k, dr):
    C = {}
    for n in ("ident", "tri", "upp", "ones"):
        C[n] = k.alloc(n, [128])
        k.dma("sp", C[n], dr[n])
    return C


def transpose_tile(k, C, src, dst_bf, nchunk=8, dst32=None):
    for g in range(0, nchunk, 4):
        ps = k.psum()
        n = min(4, nchunk - g)
        for c in range(n):
            k.tr(ps[:, c * 128:(c + 1) * 128], src[:, (g + c) * 128:(g + c + 1) * 128], C["ident"])
        psv = ps[:, 0:n * 128].re("p (a b) -> p a b", b=128)
        if (g // 4) % 2 == 0:
            k.cp(dst_bf[:, g:g + n, :], psv, eng="act")
        else:
            k.cp(dst_bf[:, g:g + n, :], psv, eng="dve")
        if dst32 is not None:
            k.cp(dst32[:, g:g + n, :], psv, eng="dve")


def phase_l0(k, dr, C, X):
    m0 = k.mark()
    Win = k.alloc("Win", [8, 3088], BF16)
    wv = dr["w_in0"].rearrange("(c p) n -> p c n", p=128)
    for c in range(8):
        k.dma("pool", Win.sub((slice(None), c, slice(None)), c), wv[:, c, :])
    Wout = k.alloc("Wout", [8, 1024], BF16)
    wo = dr["w_out0"].rearrange("(c p) n -> p c n", p=128)
    for c in range(0, 8, 4):
        k.dma("pool", Wout.sub((slice(None), slice(c, c + 4), slice(None)), c), wo[:, c:c + 4, :])
    wg2 = k.alloc("wg2", [256])
    k.dma("sp", wg2[0:16, :], dr["w_gate2"])
    bg = k.alloc("bg", [256])
    k.dma("sp", bg[0:1, :], dr["b_gate"])
    normw = k.alloc("normw", [1024])
    k.dma("sp", normw, dr["norm0"].partition_broadcast(128))
    g_bc = k.alloc("ln_g", [1024])
    b_bc = k.alloc("ln_b", [1024])
    k.dma("sp", g_bc, dr["ln1_g"].partition_broadcast(128))
    k.dma("sp", b_bc, dr["ln1_b"].partition_broadcast(128))
    Etab = k.alloc("Etab", [1024])
    k.dma("sp", Etab[:, 512:1024], dr["ret_eqk"])
    Ee = k.alloc("Ee", [2, 256])
    k.dma("sp", Ee[:, 1, :], dr["ret_ee"])
    dS_2 = [k.alloc("dS%d" % i_, [4]) for i_ in range(2)]
    for i_ in range(2):
        k.dma("sp", dS_2[i_][:, 2:4], dr["ret_ds"])
    invf = k.alloc("invf", [32])
    k.dma("sp", invf, dr["inv_freq"].partition_broadcast(128))
    posi = k.alloc("posi", [16], I32)
    k.dma("sp", posi, dr["pos"])
    posf = k.alloc("posf", [16])
    k.cp(posf, posi)
    cosT = k.alloc("cosT", [16, 32])
    sinT = k.alloc("sinT", [16, 32])
    ang = k.alloc("ang", [16, 32])
    k.tt(ang, invf.bc([128, 16, 32], 1), posf.bc([128, 16, 32], 2), ALU.mult)
    PI = float(np.pi)
    rr = k.alloc("rr", [16, 32])
    qi = k.alloc("qi", [16, 32], I32)
    qf = k.alloc("qf", [16, 32])
    gg = k.alloc("gg", [16, 32])

    def sin_table(dst, shift):
        k.ts(rr, ang, 1.0 / (2 * PI), shift, ALU.mult, ALU.add)
        k.cp(qi, rr)
        k.cp(qf, qi)
        k.tt(rr, rr, qf, ALU.subtract)
        k.ts(gg, rr, 0.5, None, ALU.is_gt)
        k.tt(rr, rr, gg, ALU.subtract)
        k.ts(gg, rr, -0.5, None, ALU.is_lt)
        k.tt(rr, rr, gg, ALU.add)
        k.act(dst, rr, AF.Sin, scale=2 * PI)

    sin_table(sinT, 0.0)
    sin_table(cosT, 0.25)

    XTn = k.alloc("XTn", [8, 128], BF16)
    qk32 = k.alloc("qk32", [1024])
    v8b_2 = [k.alloc("v8b%d" % i_, [1024], BF16) for i_ in range(2)]
    Gb_2 = [k.alloc("Gb%d" % i_, [1024], BF16) for i_ in range(2)]
    ga32 = k.alloc("ga32", [16])
    gaT = k.alloc("gaT", [128])
    l32 = k.alloc("l32", [256])
    kend_2 = [k.alloc("kend%d" % i_, [2, 256], BF16) for i_ in range(2)]
    qkT_2 = [k.alloc("qkT%d" % i_, [8, 128], BF16) for i_ in range(2)]
    attm = [k.alloc("attm%d" % i, [4, 128], BF16) for i in range(2)]
    o32 = k.alloc("o32", [1024])
    junk = k.alloc("junk", [1024])
    junkb = k.alloc("junkb", [1024])
    OT = k.alloc("OT", [8, 128], BF16)
    S32 = k.alloc("S32", [4, 128])
    Sb = k.alloc("Sb", [4, 128], BF16)
    st = k.alloc("st", [64])
    lnst = k.alloc("lnst", [8])
    k.memset(S32, 0.0)
    k.memset(Sb, 0.0, eng="pool")
    k.memset(st, 0.0)
    cols = [(0, 512), (512, 512), (1024, 512), (1536, 512), (2048, 512), (2560, 512), (3072, 16)]

    def front(n):
        i = n % 2
        v8b, Gb, kend, qkT, dS = v8b_2[i], Gb_2[i], kend_2[i], qkT_2[i], dS_2[i]
        Xn = X.sub((slice(None), n, slice(None)), n)
        transpose_tile(k, C, Xn, XTn)
        for gi, (c0, w) in enumerate(cols):
            ps = k.psum()
            for c in range(8):
                k.mm(ps[:, 0:w], XTn[:, c, :], Win[:, c, c0:c0 + w].k(c), start=(c == 0), stop=(c == 7))
            if gi < 2:
                k.cp(qk32[:, c0:c0 + w], ps[:, 0:w], eng="dve")
            elif gi < 4:
                k.cp(v8b[:, c0 - 1024:c0 - 1024 + w], ps[:, 0:w], eng="act")
            elif gi < 6:
                k.act(junk[:, 0:512], ps[:, 0:w], AF.Silu)
                k.tt(Gb[:, c0 - 2048:c0 - 2048 + w], junk[:, 0:512], normw[:, c0 - 2048:c0 - 2048 + w], ALU.mult, eng="pool")
            else:
                k.cp(ga32, ps[:, 0:16], eng="dve")
        ps = k.psum()
        k.tr(ps[0:16, 0:128], ga32, C["ident"])
        k.cp(gaT[0:16, :], ps[0:16, 0:128], eng="dve")
        psz = k.psum()
        k.mm(psz[:, 0:256], gaT[0:16, :], wg2[0:16, :], start=True, stop=False)
        k.mm(psz[:, 0:256], C["ones"][0:1, :], bg[0:1, :], start=False, stop=True)
        k.act(l32, psz[:, 0:256], AF.Exp, scale=-1.0)
        k.act(l32, l32, AF.Ln, bias=1.0, scale=1.0)
        psb = k.psum()
        k.mm(psb[:, 0:256], C["tri"], l32)
        k.mm(psb[:, 256:512], C["upp"], l32)
        psc = k.psum()
        k.mm(psc[:, 0:1], l32[:, 0:128], C["ones"][:, 0:1])
        k.mm(psc[:, 1:2], l32[:, 128:256], C["ones"][:, 0:1])
        k.act(Etab[:, 0:256], psb[:, 0:256], AF.Exp, scale=-1.0 / 16.0)
        k.ts(Etab[:, 0:256], Etab[:, 0:256], 0.125, None, ALU.mult)
        k.act(Etab[:, 256:512], psb[:, 0:256], AF.Exp, scale=1.0 / 16.0)
        k.act(Ee[:, 0, :], psb[:, 256:512], AF.Exp, scale=-1.0 / 16.0)
        k.act(dS[:, 0:2], psc[:, 0:2], AF.Exp, scale=-1.0 / 16.0)
        rv = qk32[:, 512:1024].re("p (h t m) -> p h t m", t=2, m=32)
        x1 = rv[:, :, 0, :]
        x2 = rv[:, :, 1, :]
        cs = B(cosT.ap[:, n, :].unsqueeze(1).to_broadcast([128, 8, 32]), cosT.key)
        sn = B(sinT.ap[:, n, :].unsqueeze(1).to_broadcast([128, 8, 32]), sinT.key)
        jv = junk.re("p (a h m) -> p a h m", h=8, m=32)
        k.tt(jv[:, 0], x1, cs, ALU.mult)
        k.tt(jv[:, 1], x2, sn, ALU.mult)
        k.tt(jv[:, 2], x1, sn, ALU.mult, eng="pool")
        k.tt(jv[:, 3], x2, cs, ALU.mult, eng="pool")
        k.tt(x1, jv[:, 0], jv[:, 1], ALU.subtract)
        k.tt(x2, jv[:, 2], jv[:, 3], ALU.add)
        kv = qk32.re("p (a b) -> p a b", b=512)[:, :, 256:512]
        k.tt(kend, kv, Ee, ALU.mult)
        k.tt(qk32, qk32, Etab, ALU.mult)
        transpose_tile(k, C, qk32, qkT)

    def back(n):
        i = n % 2
        v8b, Gb, kend, qkT, dS = v8b_2[i], Gb_2[i], kend_2[i], qkT_2[i], dS_2[i]
        junk = junkb
        Xn = X.sub((slice(None), n, slice(None)), n)
        for hg in range(2):
            psA = [k.psum(), k.psum()]
            for h4 in range(4):
                qc = hg * 4 + h4 // 2
                kc = qc + 2
                r0 = (h4 % 2) * 64
                k.mm(psA[h4 % 2][:, (h4 // 2) * 128:(h4 // 2 + 1) * 128], qkT[r0:r0 + 64, kc, :], qkT[r0:r0 + 64, qc, :])
            am = attm[hg]
            amv = am.re("p (a b) c -> p a b c", b=2)
            for par in range(2):
                k.tt(amv[:, :, par, :], psA[par][:, 0:256].re("p (a b) -> p a b", b=128), C["tri"].bc([128, 2, 128], 1), ALU.mult)
            psO = [k.psum(), k.psum()]
            for h4 in range(4):
                h = hg * 4 + h4
                qc = hg * 4 + h4 // 2
                r0 = (h4 % 2) * 64
                pair = hg * 2 + h4 // 2
                po = psO[h4 % 2][:, (h4 // 2) * 128:(h4 // 2 + 1) * 128]
                k.mm(po, qkT[r0:r0 + 64, qc, :], Sb[r0:r0 + 64, pair, :], start=True, stop=False)
                k.mm(po, am[:, h4, :], v8b[:, h * 128:(h + 1) * 128], start=False, stop=True)
            ovh = o32[:, hg * 512:(hg + 1) * 512].re("p (a b c) -> p a b c", b=2, c=128)
            for par in range(2):
                k.cp(B(ovh.ap[:, :, par, :], ("o32", hg)), psO[par][:, 0:256].re("p (a b) -> p a b", b=128), eng="act")
            psD = k.psum()
            for h4 in range(4):
                h = hg * 4 + h4
                k.mm(psD[:, h4 * 128:(h4 + 1) * 128], kend[:, hg, (h4 // 2) * 128:(h4 // 2 + 1) * 128], v8b[:, h * 128:(h + 1) * 128])
            for h4 in range(4):
                r0 = (h4 % 2) * 64
                pair = hg * 2 + h4 // 2
                k.stt(S32[r0:r0 + 64, pair, :].k(hg), S32[r0:r0 + 64, pair, :].k(hg), dS[r0:r0 + 64, pair:pair + 1],
                      psD[r0:r0 + 64, h4 * 128:(h4 + 1) * 128], ALU.mult, ALU.add)
            k.cp(Sb[:, hg * 2:hg * 2 + 2, :].k(hg), S32[:, hg * 2:hg * 2 + 2, :].k(hg), eng="pool")
        ov = o32.re("p (h v) -> p h v", v=128)
        k.tt(junk, o32, o32, ALU.mult, eng="pool")
        k.red(st[:, 0:8].k("s2"), junk.re("p (h v) -> p h v", v=128))
        k.red(st[:, 12:16].k("s1"), ov[:, 4:8, :])
        k.ts(st[:, 16:24].k("mean"), st[:, 8:16].k("s1"), 1.0 / 128, None, ALU.mult)
        k.tt(st[:, 24:32].k("msq"), st[:, 16:24].k("mean"), st[:, 16:24].k("mean"), ALU.mult)
        k.stt(st[:, 32:40].k("var"), st[:, 0:8].k("s2"), 1.0 / 128, st[:, 24:32].k("msq"), ALU.mult, ALU.subtract)
        k.act(st[:, 32:40].k("var"), st[:, 32:40].k("var"), AF.Sqrt, bias=EPS, scale=1.0)
        k.recip(st[:, 40:48].k("rstd"), st[:, 32:40].k("var"))
        k.stt(st[:, 48:56].k("nb"), st[:, 16:24].k("mean"), -1.0, st[:, 40:48].k("rstd"), ALU.mult, ALU.mult)
        k.tt(ov, ov, st[:, 40:48].k("rstd").bc([128, 8, 128], 2), ALU.mult)
        k.tt(ov, ov, st[:, 48:56].k("nb").bc([128, 8, 128], 2), ALU.add)
        k.tt(o32, o32, Gb, ALU.mult)
        transpose_tile(k, C, o32, OT)
        for half in range(2):
            ps = k.psum()
            for c in range(8):
                k.mm(ps, OT[:, c, :], Wout[:, c, half * 512:(half + 1) * 512], start=(c == 0), stop=(c == 7))
            k.stt(Xn[:, half * 512:(half + 1) * 512], Xn[:, half * 512:(half + 1) * 512], ALPHA, ps, ALU.mult, ALU.add)
        ln_tile(k, Xn, g_bc, b_bc, Xn, lnst, junk)

    FB, BB = (0, 5), (5, 8)
    k.issue_merged(k.record(lambda: front(0), banks=FB), [])
    for n in range(NT):
        fr = k.record(lambda: front(n + 1), banks=FB) if n + 1 < NT else []
        bk = k.record(lambda: back(n), banks=BB)
        k.issue_merged(bk, fr)
    k.reset(m0)


def phase_ffn(k, dr, C, X, experts, ln_g, ln_b, out_dram=None, router=None):
    k.P.barrier()
    m0 = k.mark()
    g_bc = k.alloc("ln_g", [1024])
    b_bc = k.alloc("ln_b", [1024])
    k.dma("sp", g_bc, ln_g.partition_broadcast(128))
    k.dma("sp", b_bc, ln_b.partition_broadcast(128))
    XTh = k.alloc("XTh", [8, 1024], BF16)
    actb = k.alloc("actb", [28, 1024], BF16)
    NW = 3
    wgu = [k.alloc("wgu%d" % i, [8, 256], BF16) for i in range(NW)]
    ND = 6
    wdb = [k.alloc("wd%d" % i, [1024], BF16) for i in range(ND)]
    sg = [k.alloc("sg%d" % i, [512]) for i in range(2)]
    junk = k.alloc("junk", [1024])
    lnst = k.alloc("lnst", [8])
    wi = 0
    di = 0
    si = 0
    if router is not None:
        Wr = k.alloc("Wr", [8, 8])
        k.dma("sp", Wr, router[0].rearrange("(c p) e -> p c e", p=128))
        br = k.alloc("br", [8])
        k.dma("sp", br[0:1, :], router[1])
        CB = k.alloc("CB", [16, 8])
        XT32 = k.alloc("XT32", [8, 128])
        rt = k.alloc("rt", [64])
    for half in range(2):
        for t in range(8):
            n = half * 8 + t
            Xn = X.sub((slice(None), n, slice(None)), n)
            if router is None:
                transpose_tile(k, C, Xn, B(XTh.ap[:, :, t * 128:(t + 1) * 128], ("XTh", t)))
            else:
                transpose_tile(k, C, Xn, B(XTh.ap[:, :, t * 128:(t + 1) * 128], ("XTh", t)), dst32=XT32)
                ps = k.psum()
                for c in range(8):
                    k.mm(ps[:, 0:8], XT32[:, c, :], Wr[:, c, :], start=(c == 0), stop=False)
                k.mm(ps[:, 0:8], C["ones"][0:1, :], br[0:1, :], start=False, stop=True)
                lg = rt[:, 0:8].k("lg")
                k.cp(lg, ps[:, 0:8])
                k.red(rt[:, 8:9].k("m1"), lg, op=ALU.max)
                k.ts(rt[:, 16:24].k("eq1"), lg, rt[:, 8:9].k("m1"), None, ALU.is_equal)
                k.stt(rt[:, 24:32].k("l2"), rt[:, 16:24].k("eq1"), -1e30, lg, ALU.mult, ALU.add)
                k.red(rt[:, 9:10].k("m2"), rt[:, 24:32].k("l2"), op=ALU.max)
                k.ts(rt[:, 32:40].k("eq2"), rt[:, 24:32].k("l2"), rt[:, 9:10].k("m2"), None, ALU.is_equal)
                k.tt(rt[:, 10:11].k("d"), rt[:, 9:10].k("m2"), rt[:, 8:9].k("m1"), ALU.subtract)
                k.act(rt[:, 11:12].k("e"), rt[:, 10:11].k("d"), AF.Exp)
                k.ts(rt[:, 12:13].k("den"), rt[:, 11:12].k("e"), 1.0, None, ALU.add)
                k.recip(rt[:, 13:14].k("g1"), rt[:, 12:13].k("den"))
                k.tt(rt[:, 14:15].k("g2"), rt[:, 11:12].k("e"), rt[:, 13:14].k("g1"), ALU.mult)
                cbn = CB[:, n, :].k(n)
                k.ts(cbn, rt[:, 16:24].k("eq1"), rt[:, 13:14].k("g1"), None, ALU.mult)
                k.stt(cbn, rt[:, 32:40].k("eq2"), rt[:, 14:15].k("g2"), cbn, ALU.mult, ALU.add)
                k.act(Xn, Xn, AF.Identity, scale=ALPHA)
        for ei, (wgu_l, wd) in enumerate(experts):
            comb = None if router is None else (CB, ei)
            for f in range(28):
                wb = wgu[wi % NW]
                wi += 1
                k.dma("pool", wb, wgu_l[f])
                for tb in range(2):
                    psG = k.psum()
                    psU = k.psum()
                    for c in range(8):
                        k.mm(psG, wb[:, c, 0:128], XTh[:, c, tb * 512:(tb + 1) * 512], start=(c == 0), stop=(c == 7))
                    for c in range(8):
                        k.mm(psU, wb[:, c, 128:256], XTh[:, c, tb * 512:(tb + 1) * 512], start=(c == 0), stop=(c == 7))
                    s = sg[si % 2]
                    si += 1
                    k.act(s, psG, AF.Silu)
                    k.tt(actb.sub((slice(None), f, slice(tb * 512, (tb + 1) * 512)), f, tb), s, psU, ALU.mult)
            for tg in range(2):
                pss = [k.psum() for _ in range(8)]
                for f in range(28):
                    db = wdb[di % ND]
                    di += 1
                    k.dma("pool", db, wd[f * 128:(f + 1) * 128, :])
                    for t4 in range(4):
                        t = tg * 4 + t4
                        for hh in range(2):
                            k.mm(pss[t4 * 2 + hh], actb[:, f, t * 128:(t + 1) * 128].k(f, t // 4),
                                 db[:, hh * 512:(hh + 1) * 512], start=(f == 0), stop=(f == 27))
                for t4 in range(4):
                    n = half * 8 + tg * 4 + t4
                    Xn = X.sub((slice(None), n, slice(None)), n)
                    for hh in range(2):
                        xs = Xn[:, hh * 512:(hh + 1) * 512]
                        if comb is None:
                            k.stt(xs, xs, ALPHA, pss[t4 * 2 + hh], ALU.mult, ALU.add)
                        else:
                            cb, e = comb
                            k.stt(xs, pss[t4 * 2 + hh], cb[:, n, e:e + 1].k(n), xs, ALU.mult, ALU.add)
        for t in range(8):
            n = half * 8 + t
            Xn = X.sub((slice(None), n, slice(None)), n)
            ln_tile(k, Xn, g_bc, b_bc, Xn, lnst, junk)
            if out_dram is not None:
                k.dma("sp", out_dram[n * 128:(n + 1) * 128, :], Xn)
    k.reset(m0)


def host_consts_l1():
    c = {}
    p = np.arange(128)[:, None]
    f = np.arange(128)[None, :]
    same = (p // 64) == (f // 64)
    c["tri64"] = ((p <= f) & same).astype(np.float32)
    c["blk64"] = same.astype(np.float32)
    c["nstrict64"] = -((f < p) & same).astype(np.float32)
    c["slt"] = (p < f).astype(np.float32)
    c["iota512"] = np.tile(np.arange(512, dtype=np.float32)[None, :], (128, 1))
    return c


def gdn_pass_a(k, dr, C, X, scr):
    k.P.barrier()
    m0 = k.mark()
    Wg = k.alloc("Wg", [8, 1040], BF16)
    wv = dr["wg_l"].rearrange("(c p) n -> p c n", p=128)
    for c in range(0, 8, 4):
        k.dma("pool", Wg.sub((slice(None), slice(c, c + 4), slice(None)), c), wv[:, c:c + 4, :])
    cw = k.alloc("cw", [24, 4])
    k.dma("sp", cw, dr["convw_l"])
    cnb = k.alloc("cnb", [1024])
    k.dma("sp", cnb, dr["c_norm"].partition_broadcast(128))
    dtb = k.alloc("dtb", [8])
    k.dma("sp", dtb, dr["c_dt_bias"].partition_broadcast(128))
    nA = k.alloc("nA", [8])
    k.dma("sp", nA, dr["c_a_log"].partition_broadcast(128))
    k.act(nA, nA, AF.Exp)
    k.ts(nA, nA, -1.0, None, ALU.mult)
    XTb = k.alloc("XTb", [8, 512], BF16)
    NWB = 3
    wqb = [k.alloc("wq%d" % i, [8, 128], BF16) for i in range(NWB)]
    pre = [k.alloc("pre%d" % i, [515]) for i in range(2)]
    acc = [k.alloc("acc%d" % i, [512]) for i in range(3)]
    sbuf_s = [k.alloc("s%d" % i, [512]) for i in range(3)]
    sq = [k.alloc("sq%d" % i, [512]) for i in range(3)]
    rsb = [k.alloc("rs%d" % i, [512]) for i in range(3)]
    outb = [k.alloc("ob%d" % i, [512], BF16) for i in range(4)]
    carry = k.alloc("carry", [24, 3])
    k.memset(carry, 0.0)
    junk = k.alloc("junk", [1024])
    Gt = [k.alloc("Gt%d" % i, [1024], BF16) for i in range(2)]
    ab = [k.alloc("ab%d" % i, [16]) for i in range(2)]
    zt = k.alloc("zt", [8])
    BG = scr["BG"]
    it = 0
    for b in range(4):
        for t in range(4):
            n = 4 * b + t
            Xn = X.sub((slice(None), n, slice(None)), n)
            transpose_tile(k, C, Xn, B(XTb.ap[:, :, t * 128:(t + 1) * 128], ("XTb", t)))
        def head(ch, it):
            wb = wqb[it % NWB]
            pr = pre[it % 2]
            ac = acc[it % 3]
            if it < 2:
                k.dma("pool", wb, dr["wqkv_l"][ch])
            ps = k.psum()
            for c in range(8):
                k.mm(ps, wb[:, c, :], XTb[:, c, :], start=(c == 0), stop=(c == 7))
            if it + 2 < 96:
                k.dma("pool", wqb[(it + 2) % NWB], dr["wqkv_l"][(it + 2) % 24])
            k.cp(pr[:, 3:515], ps, eng="act")
            k.cp(pr[:, 0:3], carry[:, ch, :].k(ch), eng="pool")
            k.cp(carry[:, ch, :].k(ch), pr[:, 512:515], eng="pool")
            k.ts(ac, pr[:, 0:512], cw[:, ch, 0:1], None, ALU.mult)
            k.stt(ac, pr[:, 1:513], cw[:, ch, 1:2], ac, ALU.mult, ALU.add)
            k.stt(ac, pr[:, 2:514], cw[:, ch, 2:3], ac, ALU.mult, ALU.add)
            k.stt(ac, pr[:, 3:515], cw[:, ch, 3:4], ac, ALU.mult, ALU.add)

        def mid(ch, it):
            ac = acc[it % 3]
            s = sbuf_s[it % 3]
            q2 = sq[it % 3]
            rs = rsb[it % 3]
            ob = outb[it % 4]
            if ch < 16:
                k.act(s, ac, AF.Silu)
                k.tt(q2, s, s, ALU.mult, eng="pool")
                pss = k.psum()
                k.mm(pss[0:1, :], C["ones"][:, 0:1], q2)
                k.act(rs[0:1, :], pss[0:1, :], AF.Ln, bias=1e-6, scale=1.0)
                k.act(rs[0:1, :], rs[0:1, :], AF.Exp, scale=-0.5)
            else:
                k.act(ob, ac, AF.Silu)

        def tail(ch, it):
            s = sbuf_s[it % 3]
            rs = rsb[it % 3]
            ob = outb[it % 4]
            if ch < 16:
                psb = k.psum()
                k.mm(psb, C["ones"][0:1, :], rs[0:1, :])
                k.stt(ob, s, (128.0 ** -0.5) if ch < 8 else 1.0, psb, ALU.mult, ALU.mult)
                dst = scr["qs"] if ch < 8 else scr["ks"]
                h = ch % 8
            else:
                dst = scr["vs"]
                h = ch - 16
            dv = dst.ap[4 * b:4 * b + 4, :, h, :].rearrange("n k t -> k n t")
            k._op("sp", (lambda dv, ob: lambda e: e.dma_start(out=dv, in_=ob.ap.rearrange("p (n t) -> p n t", t=128)))(dv, ob),
                  reads=[ob.key], writes=[dst.key + (4 * b + tt_, h) for tt_ in range(4)], dma=True)

        HB, MB, TB = (0, 3), (3, 5), (5, 8)
        k.issue_merged(k.record(lambda: head(0, it), banks=HB), [])
        k.issue_merged(k.record(lambda: mid(0, it), banks=MB), k.record(lambda: head(1, it + 1), banks=HB))
        for ch in range(24):
            tl = k.record(lambda: tail(ch, it), banks=TB)
            md = k.record(lambda: mid(ch + 1, it + 1), banks=MB) if ch + 1 < 24 else []
            hd_ops = k.record(lambda: head(ch + 2, it + 2), banks=HB) if ch + 2 < 24 else []
            k.issue_merged(tl, k.merge_lists(md, hd_ops))
            it += 1
        for t in range(4):
            n = 4 * b + t
            G = Gt[n % 2]
            a = ab[n % 2]
            for gi, (c0, w) in enumerate([(0, 512), (512, 512), (1024, 16)]):
                ps = k.psum()
                for c in range(8):
                    k.mm(ps[:, 0:w], XTb[:, c, t * 128:(t + 1) * 128], Wg[:, c, c0:c0 + w], start=(c == 0), stop=(c == 7))
                if gi < 2:
                    k.act(junk[:, 0:512], ps, AF.Silu)
                    k.tt(G[:, c0:c0 + 512], junk[:, 0:512], cnb[:, c0:c0 + 512], ALU.mult, eng="pool")
                else:
                    k.cp(a, ps[:, 0:16])
            k.dma("sp", scr["gs"].sub(n, n), G)
            k.act(BG[:, n, 8:16].k(n), a[:, 8:16], AF.Sigmoid)
            k.tt(zt, a[:, 0:8], dtb, ALU.add)
            k.act(zt, zt, AF.Exp)
            k.act(zt, zt, AF.Ln, bias=1.0, scale=1.0)
            k.tt(BG[:, n, 0:8].k(n), zt, nA, ALU.mult)
    k.reset(m0)


def gdn_pass_b(k, dr, C, C1, X, scr, ln_g, ln_b):
    k.P.barrier()
    m0 = k.mark()
    Wout = k.alloc("Wout", [8, 1024], BF16)
    wo = dr["w_out1"].rearrange("(c p) n -> p c n", p=128)
    for c in range(0, 8, 4):
        k.dma("pool", Wout.sub((slice(None), slice(c, c + 4), slice(None)), c), wo[:, c:c + 4, :])
    g_bc = k.alloc("ln_g", [1024])
    b_bc = k.alloc("ln_b", [1024])
    k.dma("sp", g_bc, ln_g.partition_broadcast(128))
    k.dma("sp", b_bc, ln_b.partition_broadcast(128))
    identb = k.alloc("identb", [128], BF16)
    k.cp(identb, C["ident"])
    BG = scr["BG"]
    qT = [k.alloc("qT%d" % i, [8, 128], BF16) for i in range(2)]
    kT = [k.alloc("kT%d" % i, [8, 128], BF16) for i in range(2)]
    vT = [k.alloc("vT%d" % i, [8, 128], BF16) for i in range(2)]
    Gt = [k.alloc("G%d" % i, [1024], BF16) for i in range(2)]
    sm = k.alloc("sm", [64])
    junk = k.alloc("junk", [8, 128])
    junk2 = k.alloc("junk2", [8, 128])
    egr_2 = [k.alloc("egr%d" % i_, [8, 128]) for i_ in range(2)]
    E = k.alloc("E", [8, 128])
    Dl = k.alloc("Dl", [8, 128])
    aqk_2 = [k.alloc("aqk%d" % i_, [8, 128], BF16) for i_ in range(2)]
    Pm = k.alloc("Pm", [8, 128])
    PT = k.alloc("PT", [8, 128])
    RT = k.alloc("RT", [8, 128])
    Tb = k.alloc("Tb", [8, 128], BF16)
    Pb = k.alloc("Pb", [8, 128], BF16)
    PTb = k.alloc("PTb", [8, 128], BF16)
    vb = k.alloc("vb", [8, 128], BF16)
    kbg = k.alloc("kbg", [8, 128], BF16)
    kdec_2 = [k.alloc("kdec%d" % i_, [8, 128], BF16) for i_ in range(2)]
    u32_2 = [k.alloc("u32%d" % i_, [8, 128]) for i_ in range(2)]
    wT_2 = [k.alloc("wT%d" % i_, [8, 128], BF16) for i_ in range(2)]
    qdT_2 = [k.alloc("qdT%d" % i_, [8, 128], BF16) for i_ in range(2)]
    vnew = k.alloc("vnew", [8, 128], BF16)
    o32 = k.alloc("o32", [8, 128])
    OT = k.alloc("OT", [8, 128], BF16)
    S32 = k.alloc("S32", [8, 128])
    Sb = k.alloc("Sb", [8, 128], BF16)
    st = k.alloc("st", [16])
    lnst = k.alloc("lnst", [8])
    k.memset(S32, 0.0)
    k.memset(Sb, 0.0, eng="pool")
    tri = C1["tri64"]
    HG = [(0, 4), (4, 8)]

    def g4(buf, hg):
        return B(buf.ap[:, hg * 4:hg * 4 + 4, :], buf.key + (hg,))

    def hd(buf, h):
        return B(buf.ap[:, h, :], buf.key + (h // 4,))

    def ps4(ps):
        return ps.re("p (a b) -> p a b", b=128)

    def load(n):
        i = n % 2
        k.dma("sp", qT[i], scr["qs"].sub(n, n))
        k.dma("sp", kT[i], scr["ks"].sub(n, n))
        k.dma("sp", vT[i], scr["vs"].sub(n, n))
        k.dma("sp", Gt[i], scr["gs"].sub(n, n))

    load(0)

    def prep(n):
        i = n % 2
        q_, k_, v_, G = qT[i], kT[i], vT[i], Gt[i]
        egr, aqk, kdec, u32, wT, qdT = egr_2[i], aqk_2[i], kdec_2[i], u32_2[i], wT_2[i], qdT_2[i]
        g32 = BG[:, n, 0:8].k(n)
        beta = BG[:, n, 8:16].k(n)
        ps = k.psum()
        k.mm(ps[:, 0:8], tri, g32)
        k.mm(ps[:, 8:16], C1["blk64"], g32)
        gc = sm[:, 0:8].k("gc")
        k.cp(gc, ps[:, 0:8])
        k.act(sm[:, 8:16].k("egc"), ps[:, 0:8], AF.Exp)
        k.tt(sm[:, 16:24].k("bge"), beta, sm[:, 8:16].k("egc"), ALU.mult)
        k.tt(sm[:, 24:32].k("dd"), ps[:, 8:16], gc, ALU.subtract)
        k.act(sm[:, 32:40].k("edec"), sm[:, 24:32].k("dd"), AF.Exp)
        k.cp(junk, g32.bc([128, 8, 128], 2))
        psR = [k.psum(), k.psum()]
        for h in range(8):
            k.mm(psR[h // 4][:, (h % 4) * 128:(h % 4 + 1) * 128], junk[:, h, :], tri)
        for hg in range(2):
            k.act(g4(egr, hg), ps4(psR[hg]), AF.Exp)
            k.tt(g4(E, hg), ps4(psR[hg]), gc[:, hg * 4:hg * 4 + 4].bc([128, 4, 128], 2), ALU.subtract)
        k.ts(Dl, E, 0.0, -1.0, ALU.max, ALU.mult)
        k.act(Dl, Dl, AF.Exp)
        k.tt(Dl, Dl, C1["nstrict64"].bc([128, 8, 128], 1), ALU.mult, eng="pool")
        k.tt(Dl, Dl, beta.bc([128, 8, 128], 2), ALU.mult)
        k.ts(E, E, 0.0, None, ALU.min)
        k.act(E, E, AF.Exp)
        k.tt(E, E, tri.bc([128, 8, 128], 1), ALU.mult, eng="pool")
        psKQ = [k.psum(), k.psum()]
        psKK = [k.psum(), k.psum()]
        for h in range(8):
            sl = slice((h % 4) * 128, (h % 4 + 1) * 128)
            k.mm(psKQ[h // 4][:, sl], k_[:, h, :], q_[:, h, :])
            k.mm(psKK[h // 4][:, sl], k_[:, h, :], k_[:, h, :])
        for hg in range(2):
            k.tt(g4(aqk, hg), ps4(psKQ[hg]), g4(E, hg), ALU.mult)
            k.tt(g4(Pm, hg), ps4(psKK[hg]), g4(Dl, hg), ALU.mult)
        psT = [k.psum(), k.psum()]
        for h in range(8):
            k.tr(psT[h // 4][:, (h % 4) * 128:(h % 4 + 1) * 128], hd(Pm, h), C["ident"])
        for hg in range(2):
            k.cp(g4(PT, hg), ps4(psT[hg]), eng="act")
            k.tt(g4(RT, hg), ps4(psT[hg]), C["ident"].bc([128, 4, 128], 1), ALU.add)
        for m in range(1, 6):
            lo = m >= 2
            Pa, PTa, RTa = (Pb, PTb, Tb) if lo else (Pm, PT, RT)
            psP = [k.psum(), k.psum()]
            psPT = [k.psum(), k.psum()] if m < 5 else None
            for hg in range(2):
                for h in range(hg * 4, hg * 4 + 4):
                    k.mm(psP[hg][:, (h % 4) * 128:(h % 4 + 1) * 128], hd(PTa, h), hd(Pa, h))
                if m < 5:
                    for h in range(hg * 4, hg * 4 + 4):
                        k.mm(psPT[hg][:, (h % 4) * 128:(h % 4 + 1) * 128], hd(Pa, h), hd(PTa, h))
            nlo = m >= 1
            Po, PTo = (Pb, PTb) if m >= 2 else (Pm, PT)
            for hg in range(2):
                k.cp(g4(Po, hg), ps4(psP[hg]), eng="act")
                if m < 5:
                    k.cp(g4(PTo, hg), ps4(psPT[hg]), eng="dve")
            psR2 = [k.psum(), k.psum()]
            for h in range(8):
                k.mm(psR2[h // 4][:, (h % 4) * 128:(h % 4 + 1) * 128], hd(Po, h), hd(RTa, h))
            for hg in range(2):
                k.tt(g4(RT, hg), g4(RT, hg), ps4(psR2[hg]), ALU.add)
                if m < 5:
                    k.cp(g4(Tb, hg), g4(RT, hg), eng="act")
                if m == 1:
                    k.cp(g4(Pb, hg), g4(Pm, hg), eng="act")
                    k.cp(g4(PTb, hg), g4(PT, hg), eng="pool")
        k.cp(Tb, RT, eng="act")
        psK = k.psum(BF16)
        psV = k.psum(BF16)
        for h in range(8):
            k.tr(psK[:, h * 128:(h + 1) * 128], k_[:, h, :], identb)
        for h in range(8):
            k.tr(psV[:, h * 128:(h + 1) * 128], v_[:, h, :], identb)
        pK = psK.re("p (a b) -> p a b", b=128)
        pV = psV.re("p (a b) -> p a b", b=128)
        k.tt(vb, pV, beta.bc([128, 8, 128], 2), ALU.mult)
        k.tt(kbg, pK, sm[:, 16:24].k("bge").bc([128, 8, 128], 2), ALU.mult)
        k.tt(kdec, pK, sm[:, 32:40].k("edec").bc([128, 8, 128], 2), ALU.mult)
        psU = [k.psum(), k.psum()]
        psW = [k.psum(), k.psum()]
        for h in range(8):
            sl = slice((h % 4) * 128, (h % 4 + 1) * 128)
            k.mm(psU[h // 4][:, sl], hd(Tb, h), hd(vb, h))
            k.mm(psW[h // 4][:, sl], hd(kbg, h), hd(Tb, h))
        for hg in range(2):
            k.cp(g4(u32, hg), ps4(psU[hg]), eng="act")
            k.cp(g4(wT, hg), ps4(psW[hg]), eng="dve")
        k.tt(qdT, q_, egr, ALU.mult, eng="pool")

    def rec(n):
        i = n % 2
        G = Gt[i]
        egr, aqk, kdec, u32, wT, qdT = egr_2[i], aqk_2[i], kdec_2[i], u32_2[i], wT_2[i], qdT_2[i]
        Xn = X.sub((slice(None), n, slice(None)), n)
        for c in range(2):
            r = slice(c * 64, c * 64 + 64)
            psWS = [k.psum(), k.psum()]
            for h in range(8):
                k.mm(psWS[h // 4][:, (h % 4) * 128:(h % 4 + 1) * 128], hd(wT, h), Sb[:, h, :])
            for hg in range(2):
                k.tt(vnew[r, hg * 4:hg * 4 + 4, :], u32[r, hg * 4:hg * 4 + 4, :], ps4(psWS[hg])[r], ALU.subtract)
            psS = [k.psum(), k.psum()]
            for h in range(8):
                k.mm(psS[h // 4][:, (h % 4) * 128:(h % 4 + 1) * 128], kdec[r, h, :], vnew[r, h, :])
            col = c * 64 + 63
            for h in range(8):
                k.stt(S32[:, h, :], S32[:, h, :], egr[:, h, col:col + 1], psS[h // 4][:, (h % 4) * 128:(h % 4 + 1) * 128],
                      ALU.mult, ALU.add)
            psO = [k.psum(), k.psum()]
            for h in range(8):
                po = psO[h // 4][:, (h % 4) * 128:(h % 4 + 1) * 128]
                k.mm(po, hd(qdT, h), Sb[:, h, :], start=True, stop=False)
                k.mm(po, aqk[r, h, :], vnew[r, h, :], start=False, stop=True)
            for hg in range(2):
                k.cp(o32[r, hg * 4:hg * 4 + 4, :], ps4(psO[hg])[r], eng="act")
            k.cp(Sb, S32, eng="act")
        k.tt(junk2, o32, o32, ALU.mult, eng="pool")
        k.red(st[:, 0:8].k("s2"), junk2)
        k.act(st[:, 0:8].k("s2"), st[:, 0:8].k("s2"), AF.Sqrt, bias=EPS, scale=1.0 / 128)
        k.recip(st[:, 8:16].k("rstd"), st[:, 0:8].k("s2"))
        k.tt(o32, o32, st[:, 8:16].k("rstd").bc([128, 8, 128], 2), ALU.mult)
        of = o32.re("p a b -> p (a b)")
        k.tt(of, of, G, ALU.mult)
        transpose_tile(k, C, of, OT)
        for half in range(2):
            ps = k.psum()
            for c in range(8):
                k.mm(ps, OT[:, c, :], Wout[:, c, half * 512:(half + 1) * 512], start=(c == 0), stop=(c == 7))
            k.stt(Xn[:, half * 512:(half + 1) * 512], Xn[:, half * 512:(half + 1) * 512], ALPHA, ps, ALU.mult, ALU.add)
        ln_tile(k, Xn, g_bc, b_bc, Xn, lnst, junk2.re("p a b -> p (a b)"))

    pr = k.record(lambda: prep(0), banks=(0, 5))
    k.issue_merged(pr, [])
    for n in range(NT):
        if n + 1 < NT:
            load(n + 1)
            pr = k.record(lambda: prep(n + 1), banks=(0, 5))
        else:
            pr = []
        rc_ = k.record(lambda: rec(n), banks=(5, 8))
        k.issue_merged(rc_, pr)
    k.reset(m0)


ARENA = 53120
SPARSE_MOE = True


def wgu_layout(w):
    g = w[:, :3584].reshape(8, 128, 28, 128)
    u = w[:, 3584:].reshape(8, 128, 28, 128)
    gu = np.concatenate([g, u], axis=3)
    return np.ascontiguousarray(gu.transpose(2, 1, 0, 3))


def shared_inputs(inp):
    m = dict(host_consts())
    m.update(host_consts_l1())
    m["inv_freq"] = m["inv_freq"].reshape(32)
    m["w_in0"] = np.ascontiguousarray(inp["ab_w_in"][0][:, l0_perm()])
    m["w_out0"] = np.ascontiguousarray(inp["ab_w_out"][0])
    m["w_gate2"] = np.ascontiguousarray(inp["gla_w_gate2"][0])
    m["b_gate"] = np.ascontiguousarray(inp["gla_b_gate"][0].reshape(1, 256))
    m["norm0"] = np.concatenate([inp["gla_norm"][0].reshape(512), inp["ret_norm"][0].reshape(512)])
    m["ln1_g"] = np.ascontiguousarray(inp["ab_ln1_g"][0]); m["ln1_b"] = np.ascontiguousarray(inp["ab_ln1_b"][0])
    m["ln2_g"] = np.ascontiguousarray(inp["ab_ln2_g"][0]); m["ln2_b"] = np.ascontiguousarray(inp["ab_ln2_b"][0])
    m["wgu0"] = wgu_layout(inp["ffn_w_gu"][0]); m["wd0"] = np.ascontiguousarray(inp["ffn_w_down"][0])
    cw = inp["c_w_in"][0]
    m["wqkv_l"] = np.ascontiguousarray(cw[:, 0:3072].reshape(8, 128, 24, 128).transpose(2, 1, 0, 3))
    m["wg_l"] = np.ascontiguousarray(np.concatenate([cw[:, 3088:4112], cw[:, 3072:3088]], axis=1))
    m["convw_l"] = np.ascontiguousarray(inp["c_conv_w"][0].reshape(4, 24, 128).transpose(2, 1, 0))
    m["c_a_log"] = np.ascontiguousarray(inp["c_a_log"][0]); m["c_dt_bias"] = np.ascontiguousarray(inp["c_dt_bias"][0])
    m["c_norm"] = np.ascontiguousarray(inp["c_norm"][0].reshape(1024))
    m["w_out1"] = np.ascontiguousarray(inp["c_w_out"][0])
    m["ln3_g"] = np.ascontiguousarray(inp["c_ln1_g"][0]); m["ln3_b"] = np.ascontiguousarray(inp["c_ln1_b"][0])
    m["ln4_g"] = np.ascontiguousarray(inp["c_ln2_g"][0]); m["ln4_b"] = np.ascontiguousarray(inp["c_ln2_b"][0])
    m["w_router"] = np.ascontiguousarray(inp["moe_w_router"][0]); m["b_router"] = np.ascontiguousarray(inp["moe_b_router"][0].reshape(1, 8))
    for e in range(8):
        m["mgu%d" % e] = wgu_layout(inp["moe_w_gu"][0, e])
        m["mwd%d" % e] = np.ascontiguousarray(inp["moe_w_down"][0, e])
    return m


IN_SPECS = [("x", [T, D], F32), ("pos", [128, 16], I32)] + \
    [(n, [128, 128], F32) for n in ("ident", "tri", "upp", "ones", "tri64", "blk64", "nstrict64", "slt")] + [("iota512", [128, 512], F32)] + \
    [("ret_eqk", [128, 512], F32), ("ret_ee", [128, 256], F32), ("ret_ds", [128, 2], F32), ("inv_freq", [32], F32),
     ("w_in0", [1024, 3088], F32), ("w_out0", [1024, 1024], F32), ("w_gate2", [16, 256], F32), ("b_gate", [1, 256], F32),
     ("norm0", [1024], F32)] + [("ln%d_%s" % (i, s), [1024], F32) for i in (1, 2, 3, 4) for s in "gb"] + \
    [("wgu0", [28, 128, 8, 256], F32), ("wd0", [3584, 1024], F32),
     ("wqkv_l", [24, 128, 8, 128], F32), ("wg_l", [1024, 1040], F32), ("convw_l", [128, 24, 4], F32),
     ("c_a_log", [8], F32), ("c_dt_bias", [8], F32), ("c_norm", [1024], F32), ("w_out1", [1024, 1024], F32),
     ("w_router", [1024, 8], F32), ("b_router", [1, 8], F32)] + \
    [("mgu%d" % e, [28, 128, 8, 256], F32) for e in range(8)] + [("mwd%d" % e, [3584, 1024], F32) for e in range(8)]


def build_program(phases=("l0", "ffn", "gdn", "moe"), need=None):
    nc = bass.Bass("TRN2", target_bir_lowering=False)
    dr = {}
    for (n, shp, dt) in IN_SPECS:
        if need is None or n in need:
            dr[n] = nc.dram_tensor(n, list(shp), dt, kind="ExternalInput").ap()
    out = nc.dram_tensor("out", [T, D], F32, kind="ExternalOutput").ap()
    scr = {}
    for n in ("qs", "ks", "vs"):
        scr[n] = B(nc.dram_tensor("scr_" + n, [16, 128, 8, 128], BF16).ap(), n)
    scr["gs"] = B(nc.dram_tensor("scr_gs", [16, 128, 1024], BF16).ap(), "gs")
    with contextlib.ExitStack() as es:
        arena = es.enter_context(nc.sbuf_tensor("arena", [128, ARENA], F32))
        banks = [es.enter_context(nc.psum_tensor("pb%d" % i, [128, 512], F32)) for i in range(8)]
        k = K(nc, arena, ARENA, [b[:, :] for b in banks])
        C = load_consts(k, dr)
        X = k.alloc("X", [16, 1024])
        for n in range(NT):
            k.dma("sp", X.sub((slice(None), n, slice(None)), n), dr["x"][n * 128:(n + 1) * 128, :])
        last = phases[-1]
        if "l0" in phases:
            phase_l0(k, dr, C, X)
        if "ffn" in phases:
            phase_ffn(k, dr, C, X, [(dr["wgu0"], dr["wd0"])], dr["ln2_g"], dr["ln2_b"],
                      out_dram=out if last == "ffn" else None)
        if "gdn" in phases:
            k.P.barrier()
            C1 = {}
            for n in ("tri64", "blk64", "nstrict64"):
                C1[n] = k.alloc(n, [128])
                k.dma("sp", C1[n], dr[n])
            scr["BG"] = k.alloc("BG", [16, 16])
            gdn_pass_a(k, dr, C, X, scr)
            gdn_pass_b(k, dr, C, C1, X, scr, dr["ln3_g"], dr["ln3_b"])
        if "moe" in phases:
            if SPARSE_MOE:
                phase_moe_sparse(k, dr, C, X, dr["ln4_g"], dr["ln4_b"], out)
            else:
                phase_ffn(k, dr, C, X, [(dr["mgu%d" % e], dr["mwd%d" % e]) for e in range(8)], dr["ln4_g"], dr["ln4_b"],
                          out_dram=out, router=(dr["w_router"], dr["b_router"]))
        if last not in ("ffn", "moe"):
            for n in range(NT):
                k.dma("sp", out[n * 128:(n + 1) * 128, :], X.sub((slice(None), n, slice(None)), n))
        print("ops", len(k.P.ops), "arena peak?", k.off, flush=True)
        k.P.emit()
    return nc


def phase_moe_sparse(k, dr, C, X, ln_g, ln_b, out_dram):
    BLK = 512
    k.P.barrier()
    m0 = k.mark()
    Xb = k.alloc("Xb", [16, 1024], BF16)
    M = k.alloc("M", [16, 8])
    CB = k.alloc("CB", [16, 8])
    RK = k.alloc("RK", [16, 8])
    OFF = k.alloc("OFF", [17, 8])
    BLOCKS = [(0, 512), (512, 128), (640, 128), (768, 256), (1024, 512), (1536, 512)]
    NB = len(BLOCKS)
    Ff = k.alloc("Ff", [NB, 8])
    Fi = k.alloc("Fi", [NB, 8], I32)
    iota = k.alloc("iota", [512])
    k.dma("sp", iota, dr["iota512"])
    slt = k.alloc("slt", [128])
    k.dma("sp", slt, dr["slt"])
    m1 = k.mark()
    Wr = k.alloc("Wr", [8, 8])
    k.dma("sp", Wr, dr["w_router"].rearrange("(c p) e -> p c e", p=128))
    br = k.alloc("br", [8])
    k.dma("sp", br[0:1, :], dr["b_router"])
    XT32 = k.alloc("XT32", [8, 128])
    rt = k.alloc("rt", [64])
    for n in range(NT):
        Xn = X.sub((slice(None), n, slice(None)), n)
        for g in range(2):
            ps = k.psum()
            for c in range(4):
                k.tr(ps[:, c * 128:(c + 1) * 128], Xn[:, (g * 4 + c) * 128:(g * 4 + c + 1) * 128], C["ident"])
            k.cp(XT32[:, g * 4:g * 4 + 4, :], ps.re("p (a b) -> p a b", b=128), eng="dve" if g else "act")
        ps = k.psum()
        for c in range(8):
            k.mm(ps[:, 0:8], XT32[:, c, :], Wr[:, c, :], start=(c == 0), stop=False)
        k.mm(ps[:, 0:8], C["ones"][0:1, :], br[0:1, :], start=False, stop=True)
        lg = rt[:, 0:8].k("lg")
        k.cp(lg, ps[:, 0:8])
        k.red(rt[:, 8:9].k("m1"), lg, op=ALU.max)
        k.ts(rt[:, 16:24].k("eq1"), lg, rt[:, 8:9].k("m1"), None, ALU.is_equal)
        k.stt(rt[:, 24:32].k("l2"), rt[:, 16:24].k("eq1"), -1e30, lg, ALU.mult, ALU.add)
        k.red(rt[:, 9:10].k("m2"), rt[:, 24:32].k("l2"), op=ALU.max)
        k.ts(rt[:, 32:40].k("eq2"), rt[:, 24:32].k("l2"), rt[:, 9:10].k("m2"), None, ALU.is_equal)
        k.tt(rt[:, 10:11].k("d"), rt[:, 9:10].k("m2"), rt[:, 8:9].k("m1"), ALU.subtract)
        k.act(rt[:, 11:12].k("e"), rt[:, 10:11].k("d"), AF.Exp)
        k.ts(rt[:, 12:13].k("den"), rt[:, 11:12].k("e"), 1.0, None, ALU.add)
        k.recip(rt[:, 13:14].k("g1"), rt[:, 12:13].k("den"))
        k.tt(rt[:, 14:15].k("g2"), rt[:, 11:12].k("e"), rt[:, 13:14].k("g1"), ALU.mult)
        cbn = CB[:, n, :].k(n)
        k.ts(cbn, rt[:, 16:24].k("eq1"), rt[:, 13:14].k("g1"), None, ALU.mult)
        k.stt(cbn, rt[:, 32:40].k("eq2"), rt[:, 14:15].k("g2"), cbn, ALU.mult, ALU.add)
        k.tt(M[:, n, :].k(n), rt[:, 16:24].k("eq1"), rt[:, 32:40].k("eq2"), ALU.add)
        k.cp(Xb[:, n, :].k(n), Xn, eng="act")
        k.act(Xn, Xn, AF.Identity, scale=ALPHA)
    Mf = M.re("p a b -> p (a b)")
    ps = k.psum()
    k.mm(ps[:, 0:128], slt, Mf)
    k.mm(ps[:, 128:256], C["ones"], Mf)
    TOT = k.alloc("TOT", [16, 8])
    k.cp(RK.re("p a b -> p (a b)"), ps[:, 0:128])
    k.cp(TOT.re("p a b -> p (a b)"), ps[:, 128:256])
    k.memset(OFF[:, 0, :].k(0), 0.0)
    for t in range(16):
        k.tt(OFF[:, t + 1, :].k(t + 1), OFF[:, t, :].k(t), TOT[:, t, :], ALU.add)
    k.tt(RK, RK, OFF[:, 0:16, :], ALU.add)
    for j, (boff, bw) in enumerate(BLOCKS):
        k.ts(Ff[:, j, :], OFF[:, 16, :].k(16), float(boff), None, ALU.is_gt)
    k.cp(Fi, Ff)
    k.P.barrier()
    k.reset(m1)
    Sel = k.alloc("Sel", [16, 512], BF16)
    SelT = k.alloc("SelT", [4, 8, 128], BF16)
    XTe = k.alloc("XTe", [8, 512], BF16)
    actb = k.alloc("actb", [28, 512], BF16)
    Ye = k.alloc("Ye", [4, 1024], BF16)
    NW = 3
    wgu = [k.alloc("wgu%d" % i, [8, 256], BF16) for i in range(NW)]
    ND = 4
    wdb = [k.alloc("wd%d" % i, [1024], BF16) for i in range(ND)]
    sg = [k.alloc("sg%d" % i, [512]) for i in range(2)]
    rj = k.alloc("rj", [16])
    identb = k.alloc("identb", [128], BF16)
    k.cp(identb, C["ident"])
    wi = di = si = 0
    for e in range(8):
        wgu_l = dr["mgu%d" % e]
        wd = dr["mwd%d" % e]
        for j, (boff, W) in enumerate(BLOCKS):
            NS = W // 128
            k.P.begin_region(Fi.ap[0:1, j, e:e + 1], Fi.key)
            k.ts(rj, RK[:, :, e], -float(boff), None, ALU.add)
            for t in range(16):
                k.ts(Sel[:, t, 0:W].k(t), iota[:, 0:W], rj[:, t:t + 1], M[:, t, e:e + 1], ALU.is_equal, ALU.mult)
            for c in range(8):
                ps = k.psum()
                for t in range(16):
                    k.mm(ps[:, 0:W], Xb[:, t, c * 128:(c + 1) * 128], Sel[:, t, 0:W].k(t), start=(t == 0), stop=(t == 15))
                k.cp(XTe[:, c, 0:W].k(c), ps[:, 0:W], eng="act" if c % 2 else "dve")
            for f in range(28):
                wb = wgu[wi % NW]
                wi += 1
                k.dma("pool", wb, wgu_l[f])
                psG = k.psum()
                psU = k.psum()
                for c in range(8):
                    k.mm(psG[:, 0:W], wb[:, c, 0:128], XTe[:, c, 0:W].k(c), start=(c == 0), stop=(c == 7))
                for c in range(8):
                    k.mm(psU[:, 0:W], wb[:, c, 128:256], XTe[:, c, 0:W].k(c), start=(c == 0), stop=(c == 7))
                s = sg[si % 2]
                si += 1
                k.act(s[:, 0:W], psG[:, 0:W], AF.Silu)
                k.tt(actb[:, f, 0:W].k(f), s[:, 0:W], psU[:, 0:W], ALU.mult)
            pss = [k.psum() for _ in range(2 * NS)]
            for f in range(28):
                db = wdb[di % ND]
                di += 1
                k.dma("pool", db, wd[f * 128:(f + 1) * 128, :])
                for s4 in range(NS):
                    for hh in range(2):
                        k.mm(pss[s4 * 2 + hh], actb[:, f, s4 * 128:(s4 + 1) * 128].k(f), db[:, hh * 512:(hh + 1) * 512],
                             start=(f == 0), stop=(f == 27))
            for s4 in range(NS):
                for hh in range(2):
                    k.cp(Ye[:, s4, hh * 512:(hh + 1) * 512].k(s4, hh), pss[s4 * 2 + hh], eng="act" if hh else "dve")
            for th in range(2):
                for s4 in range(NS):
                    psT = k.psum(BF16)
                    for t8 in range(8):
                        t = th * 8 + t8
                        k.tr(psT[:, t8 * 128:(t8 + 1) * 128], Sel[:, t, s4 * 128:(s4 + 1) * 128].k(t), identb)
                    k.cp(SelT[:, s4, :, :].k(s4), psT.re("p (a b) -> p a b", b=128), eng="act" if s4 % 2 else "dve")
                for t8 in range(8):
                    t = th * 8 + t8
                    Xn = X.sub((slice(None), t, slice(None)), t)
                    for hh in range(2):
                        ps = k.psum()
                        for s4 in range(NS):
                            k.mm(ps, SelT[:, s4, t8, :].k(s4), Ye[:, s4, hh * 512:(hh + 1) * 512].k(s4, hh),
                                 start=(s4 == 0), stop=(s4 == NS - 1))
                        xs = Xn[:, hh * 512:(hh + 1) * 512]
                        k.stt(xs, ps, CB[:, t, e:e + 1].k(t), xs, ALU.mult, ALU.add)
            k.P.end_region()
    k.P.barrier()
    k.reset(m1)
    g_bc = k.alloc("ln_g", [1024])
    b_bc = k.alloc("ln_b", [1024])
    k.dma("sp", g_bc, ln_g.partition_broadcast(128))
    k.dma("sp", b_bc, ln_b.partition_broadcast(128))
    junk = k.alloc("junk", [1024])
    lnst = k.alloc("lnst", [8])
    for n in range(NT):
        Xn = X.sub((slice(None), n, slice(None)), n)
        ln_tile(k, Xn, g_bc, b_bc, Xn, lnst, junk)
        k.dma("sp", out_dram[n * 128:(n + 1) * 128, :], Xn)
    k.reset(m0)


_NC_CACHE = {}


def kernel(**inputs):
    inp = {k_: np.asarray(v) for k_, v in inputs.items()}
    shared = shared_inputs(inp)
    if "nc" not in _NC_CACHE:
        _NC_CACHE["nc"] = build_program(("l0", "ffn", "gdn", "moe"))
    nc = _NC_CACHE["nc"]
    in_maps = []
    for b in range(8):
        m = dict(shared)
        m["x"] = np.ascontiguousarray(inp["x"][b], dtype=np.float32)
        m["pos"] = np.ascontiguousarray(inp["positions"][b].astype(np.int32).reshape(16, 128).T)
        in_maps.append(m)
    res = run_bass_kernel_spmd(nc, in_maps, core_ids=list(range(8)))
    return np.stack([np.asarray(r["out"], dtype=np.float32) for r in res.results], axis=0)
```

```python
import contextlib
from concourse.bass_utils import run_bass_kernel_spmd
import numpy as np
import concourse.bass as bass
import concourse.mybir as mybir

F32 = mybir.dt.float32
BF16 = mybir.dt.bfloat16
I32 = mybir.dt.int32
AF = mybir.ActivationFunctionType
ALU = mybir.AluOpType
AX = mybir.AxisListType

ENGS = ("pe", "act", "dve", "pool", "sp")
N_DMA_SLOTS = 24


class _Op:
    __slots__ = ("eng", "fn", "reads", "writes", "dma", "deps", "signal", "idx",
                 "chan", "val", "slot", "region")

    def __init__(self, eng, fn, reads, writes, dma):
        self.eng = eng
        self.fn = fn
        self.reads = reads
        self.writes = writes
        self.dma = dma
        self.deps = ()
        self.signal = dma
        self.chan = None
        self.val = 0
        self.slot = None


def _conflict(a, b):
    n = min(len(a), len(b))
    return a[:n] == b[:n]


class Prog:
    def __init__(self, nc):
        self.nc = nc
        self.ops = []
        self.state = {}
        self.pending = {e: set() for e in ENGS}
        self.bar_start = 0
        self.cur_region = None
        self.regions = {}

    def barrier(self):
        deps = set()
        last = {}
        for o in self.ops[self.bar_start:]:
            if o.dma:
                deps.add(o.idx)
            else:
                last[o.eng] = o.idx
        deps |= set(last.values())
        for e in ENGS:
            self.pending[e] |= deps
        self.bar_start = len(self.ops)

    def op(self, eng, fn, reads=(), writes=(), dma=False):
        rk = tuple(self._k(r) for r in reads)
        wk = tuple(self._k(w) for w in writes)
        wk = wk + tuple(r for r in rk if r[0] == "ps" and r not in wk)
        o = _Op(eng, fn, rk, wk, dma)
        o.region = self.cur_region
        o.idx = len(self.ops)
        deps = set()
        for k in o.reads:
            grp = self.state.get(k[0])
            if grp:
                for k2, st in grp.items():
                    if _conflict(k, k2) and st[0] is not None:
                        deps.add(st[0])
        for k in o.writes:
            grp = self.state.setdefault(k[0], {})
            dead = []
            for k2, st in grp.items():
                if _conflict(k, k2):
                    if st[0] is not None:
                        deps.add(st[0])
                    deps.update(st[1])
                    if len(k2) >= len(k):
                        dead.append(k2)
            for k2 in dead:
                del grp[k2]
        for k in o.writes:
            self.state[k[0]][k] = [o.idx, []]
        for k in o.reads:
            grp = self.state.setdefault(k[0], {})
            if k in grp:
                grp[k][1].append(o.idx)
            else:
                w = None
                for k2, st in grp.items():
                    if _conflict(k, k2) and st[0] is not None:
                        w = st[0] if w is None else max(w, st[0])
                grp[k] = [w, [o.idx]]
        if self.pending[eng]:
            deps |= self.pending[eng]
            self.pending[eng] = set()
        deps.discard(o.idx)
        real = []
        best = {}
        for d in deps:
            od = self.ops[d]
            if o.eng == "pe" and od.eng == "pe" and not od.dma and not o.dma:
                continue
            if od.dma:
                real.append(d)
            elif d > best.get(od.eng, -1):
                best[od.eng] = d
        real.extend(best.values())
        for d in real:
            self.ops[d].signal = True
        o.deps = tuple(sorted(real))
        self.ops.append(o)
        return o

    def begin_region(self, flag_ap, flag_key):
        rid = len(self.regions) + 1
        fdep = None
        grp = self.state.get(flag_key[0])
        if grp:
            for k2, st in grp.items():
                if _conflict(flag_key, k2) and st[0] is not None:
                    fdep = st[0] if fdep is None else max(fdep, st[0])
        if fdep is not None:
            self.ops[fdep].signal = True
        self.regions[rid] = (flag_ap, fdep)
        self.cur_region = rid

    def end_region(self):
        self.cur_region = None

    @staticmethod
    def _k(k):
        return k if isinstance(k, tuple) else (k,)

    def emit(self, extra_ctx=()):
        nc = self.nc
        import contextlib
        with contextlib.ExitStack() as es:
            sems = {e: es.enter_context(nc.semaphore("s_" + e)) for e in ENGS}
            dsem = {}
            for q in ("sp", "act", "pool"):
                dsem[q] = [es.enter_context(nc.semaphore("d_%s_%d" % (q, i))) for i in range(N_DMA_SLOTS)]
            cnt = {e: 0 for e in ENGS}
            dcnt = {q: 0 for q in dsem}
            slot_uses = {q: [0] * N_DMA_SLOTS for q in dsem}
            for o in self.ops:
                if o.dma:
                    q = o.eng
                    s = dcnt[q] % N_DMA_SLOTS
                    dcnt[q] += 1
                    slot_uses[q][s] += 1
                    o.slot = s
                    o.chan = ("d", q, s)
                    o.val = 16 * slot_uses[q][s]
                elif o.signal:
                    cnt[o.eng] += 1
                    o.chan = ("c", o.eng)
                    o.val = cnt[o.eng]
            self.n_sig = dict(cnt)
            per = {e: [o for o in self.ops if o.eng == e] for e in ENGS}

            def semof(chan):
                if chan[0] == "c":
                    return sems[chan[1]]
                return dsem[chan[1]][chan[2]]

            ETYPE = {"pe": mybir.EngineType.PE, "act": mybir.EngineType.Activation, "dve": mybir.EngineType.DVE,
                     "pool": mybir.EngineType.Pool, "sp": mybir.EngineType.SP}
            flagregs = nc.alloc_registers("rflag", engines=list(ETYPE.values())) if self.regions else None

            def emit_one(o, eng, waited):
                need = {}
                for d in o.deps:
                    od = self.ops[d]
                    if od.val > need.get(od.chan, 0):
                        need[od.chan] = od.val
                if o.dma and o.val > 16:
                    ch = o.chan
                    if o.val - 16 > need.get(ch, 0):
                        need[ch] = o.val - 16
                for ch, v in need.items():
                    if waited.get(ch, 0) >= v:
                        continue
                    eng.wait_ge(semof(ch), v)
                    waited[ch] = v
                ins = o.fn(eng)
                if o.chan is not None:
                    ins.then_inc(semof(o.chan), 16 if o.dma else 1)

            def run(e, eng):
                waited = {}
                ops = per[e]
                i = 0
                while i < len(ops):
                    o = ops[i]
                    if o.region is None:
                        emit_one(o, eng, waited)
                        i += 1
                        continue
                    j = i
                    while j < len(ops) and ops[j].region == o.region:
                        j += 1
                    grp = ops[i:j]
                    rid = o.region
                    flag_ap, fdep = self.regions[rid]
                    if fdep is not None:
                        od = self.ops[fdep]
                        if waited.get(od.chan, 0) < od.val:
                            eng.wait_ge(semof(od.chan), od.val)
                            waited[od.chan] = od.val
                    reg = flagregs[ETYPE[e]]
                    eng.reg_load(reg, flag_ap)
                    snap = dict(waited)
                    with eng.If_ne(reg, 0):
                        for g in grp:
                            emit_one(g, eng, waited)
                    comp = {}
                    first = {}
                    for g in grp:
                        if g.chan is not None:
                            inc = 16 if g.dma else 1
                            comp[g.chan] = comp.get(g.chan, 0) + inc
                            first.setdefault(g.chan, g.val - inc)
                    if comp:
                        with eng.Else():
                            for ch, c in comp.items():
                                if first[ch] > 0:
                                    eng.wait_ge(semof(ch), first[ch])
                                eng.sem_inc(semof(ch), c)
                    waited.clear()
                    waited.update(snap)
                    i = j
            last = {}
            for o in self.ops:
                if o.dma:
                    last[o.chan] = max(last.get(o.chan, 0), o.val)

            with nc.Block() as block:
                @block.tensor
                def _(eng):
                    run("pe", eng)

                @block.scalar
                def _(eng):
                    run("act", eng)

                @block.vector
                def _(eng):
                    run("dve", eng)

                @block.gpsimd
                def _(eng):
                    run("pool", eng)

                @block.sync
                def _(eng):
                    run("sp", eng)
                    for ch, v in last.items():
                        eng.wait_ge(semof(ch), v)


T = 2048
NT = 16
D = 1024
ALPHA = 4.0 ** 0.25
EPS = 1e-5


class B:
    __slots__ = ("ap", "key")

    def __init__(self, ap, key):
        self.ap = ap
        self.key = key if isinstance(key, tuple) else (key,)

    def __getitem__(self, idx):
        return B(self.ap[idx], self.key)

    def sub(self, idx, *k):
        return B(self.ap[idx], self.key + tuple(k))

    def k(self, *k):
        return B(self.ap, self.key + tuple(k))

    def re(self, pat, **kw):
        return B(self.ap.rearrange(pat, **kw), self.key)

    def bc(self, shape, axis):
        return B(self.ap.unsqueeze(axis).to_broadcast(shape), self.key)


def _keys(*bs):
    return [b.key for b in bs if isinstance(b, B)]


def _a(x):
    return x.ap if isinstance(x, B) else x


class K:
    def __init__(self, nc, arena, arena_cols, psum_banks):
        self.nc = nc
        self.P = Prog(nc)
        self.arena = arena
        self.cols = arena_cols
        self.off = 0
        self.banks = psum_banks
        self.nbank = 0
        self.uid = 0
        self.defer = None
        self.bank_range = (0, len(psum_banks))
        self.bank_ctr = {}

    def _op(self, *a, **kw):
        if self.defer is not None:
            self.defer.append((a, kw))
        else:
            self.P.op(*a, **kw)

    def record(self, fn, banks=None):
        old = self.defer
        oldb = self.bank_range
        self.defer = []
        if banks is not None:
            self.bank_range = banks
        fn()
        ops = self.defer
        self.defer = old
        self.bank_range = oldb
        return ops

    @staticmethod
    def merge_lists(a, b):
        out = []
        na, nb = len(a), len(b)
        ia = ib = 0
        while ia < na or ib < nb:
            if ib >= nb or (ia < na and ia * nb <= ib * na):
                out.append(a[ia]); ia += 1
            else:
                out.append(b[ib]); ib += 1
        return out

    def issue_merged(self, a, b):
        na, nb = len(a), len(b)
        ia = ib = 0
        while ia < na or ib < nb:
            if ib >= nb or (ia < na and ia * nb <= ib * na):
                x = a[ia]; ia += 1
            else:
                x = b[ib]; ib += 1
            self._op(*x[0], **x[1])

    def alloc(self, name, free_shape, dt=F32):
        n = int(np.prod(free_shape))
        words = n if dt == F32 or dt == I32 else (n + 1) // 2
        assert self.off + words <= self.cols, (name, self.off, words, self.cols)
        ap = self.arena[:, self.off:self.off + words]
        self.off += words
        if dt == BF16:
            ap = ap.bitcast(BF16)
            if ap.shape[-1] != n:
                ap = ap[:, 0:n]
        elif dt == I32:
            ap = ap.bitcast(I32)
        if len(free_shape) == 2:
            ap = ap.rearrange("p (a b) -> p a b", b=free_shape[1])
        elif len(free_shape) == 3:
            ap = ap.rearrange("p (a b c) -> p a b c", b=free_shape[1], c=free_shape[2])
        return B(ap, name)

    def mark(self):
        return self.off

    def reset(self, m):
        self.off = m

    def psum(self, dt=F32):
        lo, hi = self.bank_range
        b = lo + self.bank_ctr.setdefault((lo, hi), 0) % (hi - lo)
        self.bank_ctr[(lo, hi)] += 1
        ap = self.banks[b]
        if dt == BF16:
            ap = ap.bitcast(BF16)
        return B(ap, ("ps", b))

    def mm(self, out, lhsT, rhs, start=True, stop=True):
        self._op("pe", lambda e: e.matmul(out.ap, lhsT.ap, rhs.ap, start=start, stop=stop),
                  reads=_keys(lhsT, rhs), writes=_keys(out))

    def tr(self, out, in_, ident):
        self._op("pe", lambda e: e.transpose(out.ap, in_.ap, ident.ap),
                  reads=_keys(in_, ident), writes=_keys(out))

    def act(self, out, in_, func, bias=None, scale=1.0, accum=None):
        kw = {}
        if bias is not None:
            kw["bias"] = _a(bias)
        if accum is not None:
            kw["accum_out"] = accum.ap
        self._op("act", lambda e: e.activation(out.ap, in_.ap, func, scale=_a(scale), **kw),
                  reads=_keys(in_, bias, scale), writes=_keys(out, accum))

    def tt(self, out, a, b, op, eng="dve"):
        self._op(eng, lambda e: e.tensor_tensor(out.ap, a.ap, b.ap, op), reads=_keys(a, b), writes=_keys(out))

    def ts(self, out, a, s1, s2, op0, op1=None, eng="dve"):
        if op1 is None:
            f = lambda e: e.tensor_scalar(out.ap, a.ap, _a(s1), None, op0)
        else:
            f = lambda e: e.tensor_scalar(out.ap, a.ap, _a(s1), _a(s2), op0, op1)
        self._op(eng, f, reads=_keys(a, s1, s2), writes=_keys(out))

    def stt(self, out, a, s, b, op0, op1, eng="dve"):
        self._op(eng, lambda e: e.scalar_tensor_tensor(out.ap, a.ap, _a(s), b.ap, op0, op1),
                  reads=_keys(a, s, b), writes=_keys(out))

    def cp(self, out, in_, eng="dve"):
        if eng == "act":
            self._op("act", lambda e: e.copy(out.ap, in_.ap), reads=_keys(in_), writes=_keys(out))
        else:
            self._op(eng, lambda e: e.tensor_copy(out.ap, in_.ap), reads=_keys(in_), writes=_keys(out))

    def red(self, out, in_, op=None, eng="dve"):
        op = op or ALU.add
        self._op(eng, lambda e: e.tensor_reduce(out.ap, in_.ap, AX.X, op), reads=_keys(in_), writes=_keys(out))

    def recip(self, out, in_):
        self._op("dve", lambda e: e.reciprocal(out.ap, in_.ap), reads=_keys(in_), writes=_keys(out))

    def memset(self, out, val, eng="dve"):
        self._op(eng, lambda e: e.memset(out.ap, val), writes=_keys(out))

    def dma(self, q, out, in_):
        rk = _keys(in_)
        wk = _keys(out)
        self._op(q, lambda e: e.dma_start(out=_a(out), in_=_a(in_)), reads=rk, writes=wk, dma=True)


def host_consts():
    c = {}
    c["ident"] = np.eye(128, dtype=np.float32)
    j = np.arange(128)[:, None]
    i = np.arange(128)[None, :]
    c["tri"] = (j <= i).astype(np.float32)
    c["upp"] = (j > i).astype(np.float32)
    c["ones"] = np.ones((128, 128), np.float32)
    lg = np.log(1.0 - 2.0 ** (-5.0 - np.arange(4, dtype=np.float64)))
    p = np.arange(128, dtype=np.float64)[:, None]
    eq = 0.125 * np.exp(lg[None, :] * (p + 1.0))
    ek = np.exp(-lg[None, :] * (p + 1.0))
    ee = np.exp(lg[None, :] * (127.0 - p))
    rep = lambda a: np.repeat(a, 64, axis=1)
    c["ret_eqk"] = np.concatenate([rep(eq), rep(ek)], 1).astype(np.float32)
    c["ret_ee"] = rep(ee).astype(np.float32)
    dS = np.exp(lg * 128.0)
    ds = np.zeros((128, 2), np.float64)
    for h in range(4):
        ds[(h % 2) * 64:(h % 2) * 64 + 64, h // 2] = dS[h]
    c["ret_ds"] = ds.astype(np.float32)
    c["inv_freq"] = (10000.0 ** (-np.arange(0, 64, 2, dtype=np.float32) / 64)).astype(np.float32)[None, :]
    return c


L0_PERM = None


def l0_perm():
    r = lambda a, b: list(range(a, b))
    return np.array(r(0, 256) + r(256, 512) + r(1552, 1808) + r(1808, 2064) + r(512, 1024) + r(2064, 2576)
                    + r(1040, 1552) + r(2576, 3088) + r(1024, 1040))


def layer_norm_tile(k, Y, g_bc, b_bc, out, scr):
    st = scr["st"]
    junk = scr["junk"]
    k.red(st[:, 0:1].k(0), Y)
    k.tt(junk, Y, Y, ALU.mult, eng="pool")
    k.red(st[:, 1:2].k(1), junk)
    k.ts(st[:, 2:3].k(2), st[:, 0:1].k(0), 1.0 / D, None, ALU.mult)
    k.tt(st[:, 3:4].k(3), st[:, 2:3].k(2), st[:, 2:3].k(2), ALU.mult)
    k.stt(st[:, 4:5].k(4), st[:, 1:2].k(1), 1.0 / D, st[:, 3:4].k(3), ALU.mult, ALU.subtract)
    k.act(st[:, 5:6].k(5), st[:, 4:5].k(4), AF.Sqrt, bias=scr["eps"], scale=1.0)
    k.recip(st[:, 6:7].k(6), st[:, 5:6].k(5))
    k.stt(st[:, 7:8].k(7), st[:, 2:3].k(2), -1.0, st[:, 6:7].k(6), ALU.mult, ALU.mult)
    k.act(junk, Y, AF.Identity, bias=st[:, 7:8].k(7), scale=st[:, 6:7].k(6))
    k.tt(junk, junk, g_bc, ALU.mult, eng="pool")
    k.tt(out, junk, b_bc, ALU.add)


def ln_tile(k, Y, g_bc, b_bc, out, st, junk):
    k.red(st[:, 0:1].k(0), Y)
    k.act(junk, Y, AF.Square)
    k.red(st[:, 1:2].k(1), junk)
    k.ts(st[:, 2:3].k(2), st[:, 0:1].k(0), 1.0 / D, None, ALU.mult)
    k.tt(st[:, 3:4].k(3), st[:, 2:3].k(2), st[:, 2:3].k(2), ALU.mult)
    k.stt(st[:, 4:5].k(4), st[:, 1:2].k(1), 1.0 / D, st[:, 3:4].k(3), ALU.mult, ALU.subtract)
    k.act(st[:, 5:6].k(5), st[:, 4:5].k(4), AF.Sqrt, bias=EPS, scale=1.0)
    k.recip(st[:, 6:7].k(6), st[:, 5:6].k(5))
    k.stt(st[:, 7:8].k(7), st[:, 2:3].k(2), -1.0, st[:, 6:7].k(6), ALU.mult, ALU.mult)
    k.act(junk, Y, AF.Identity, bias=st[:, 7:8].k(7), scale=st[:, 6:7].k(6))
    k.tt(junk, junk, g_bc, ALU.mult)
    k.tt(out, junk, b_bc, ALU.add)


def load_consts(k, dr):
    C = {}
    for n in ("ident", "tri", "upp", "ones"):
        C[n] = k.alloc(n, [128])
        k.dma("sp", C[n], dr[n])
    return C


def transpose_tile(k, C, src, dst_bf, nchunk=8, dst32=None):
    for g in range(0, nchunk, 4):
        ps = k.psum()
        n = min(4, nchunk - g)
        for c in range(n):
            k.tr(ps[:, c * 128:(c + 1) * 128], src[:, (g + c) * 128:(g + c + 1) * 128], C["ident"])
        psv = ps[:, 0:n * 128].re("p (a b) -> p a b", b=128)
        if (g // 4) % 2 == 0:
            k.cp(dst_bf[:, g:g + n, :], psv, eng="act")
        else:
            k.cp(dst_bf[:, g:g + n, :], psv, eng="dve")
        if dst32 is not None:
            k.cp(dst32[:, g:g + n, :], psv, eng="dve")


def phase_l0(k, dr, C, X):
    m0 = k.mark()
    Win = k.alloc("Win", [8, 3088], BF16)
    wv = dr["w_in0"].rearrange("(c p) n -> p c n", p=128)
    for c in range(8):
        k.dma("pool", Win.sub((slice(None), c, slice(None)), c), wv[:, c, :])
    Wout = k.alloc("Wout", [8, 1024], BF16)
    wo = dr["w_out0"].rearrange("(c p) n -> p c n", p=128)
    for c in range(0, 8, 4):
        k.dma("pool", Wout.sub((slice(None), slice(c, c + 4), slice(None)), c), wo[:, c:c + 4, :])
    wg2 = k.alloc("wg2", [256])
    k.dma("sp", wg2[0:16, :], dr["w_gate2"])
    bg = k.alloc("bg", [256])
    k.dma("sp", bg[0:1, :], dr["b_gate"])
    normw = k.alloc("normw", [1024])
    k.dma("sp", normw, dr["norm0"].partition_broadcast(128))
    g_bc = k.alloc("ln_g", [1024])
    b_bc = k.alloc("ln_b", [1024])
    k.dma("sp", g_bc, dr["ln1_g"].partition_broadcast(128))
    k.dma("sp", b_bc, dr["ln1_b"].partition_broadcast(128))
    Etab = k.alloc("Etab", [1024])
    k.dma("sp", Etab[:, 512:1024], dr["ret_eqk"])
    Ee = k.alloc("Ee", [2, 256])
    k.dma("sp", Ee[:, 1, :], dr["ret_ee"])
    dS_2 = [k.alloc("dS%d" % i_, [4]) for i_ in range(2)]
    for i_ in range(2):
        k.dma("sp", dS_2[i_][:, 2:4], dr["ret_ds"])
    invf = k.alloc("invf", [32])
    k.dma("sp", invf, dr["inv_freq"].partition_broadcast(128))
    posi = k.alloc("posi", [16], I32)
    k.dma("sp", posi, dr["pos"])
    posf = k.alloc("posf", [16])
    k.cp(posf, posi)
    cosT = k.alloc("cosT", [16, 32])
    sinT = k.alloc("sinT", [16, 32])
    ang = k.alloc("ang", [16, 32])
    k.tt(ang, invf.bc([128, 16, 32], 1), posf.bc([128, 16, 32], 2), ALU.mult)
    PI = float(np.pi)
    rr = k.alloc("rr", [16, 32])
    qi = k.alloc("qi", [16, 32], I32)
    qf = k.alloc("qf", [16, 32])
    gg = k.alloc("gg", [16, 32])

    def sin_table(dst, shift):
        k.ts(rr, ang, 1.0 / (2 * PI), shift, ALU.mult, ALU.add)
        k.cp(qi, rr)
        k.cp(qf, qi)
        k.tt(rr, rr, qf, ALU.subtract)
        k.ts(gg, rr, 0.5, None, ALU.is_gt)
        k.tt(rr, rr, gg, ALU.subtract)
        k.ts(gg, rr, -0.5, None, ALU.is_lt)
        k.tt(rr, rr, gg, ALU.add)
        k.act(dst, rr, AF.Sin, scale=2 * PI)

    sin_table(sinT, 0.0)
    sin_table(cosT, 0.25)

    XTn = k.alloc("XTn", [8, 128], BF16)
    qk32 = k.alloc("qk32", [1024])
    v8b_2 = [k.alloc("v8b%d" % i_, [1024], BF16) for i_ in range(2)]
    Gb_2 = [k.alloc("Gb%d" % i_, [1024], BF16) for i_ in range(2)]
    ga32 = k.alloc("ga32", [16])
    gaT = k.alloc("gaT", [128])
    l32 = k.alloc("l32", [256])
    kend_2 = [k.alloc("kend%d" % i_, [2, 256], BF16) for i_ in range(2)]
    qkT_2 = [k.alloc("qkT%d" % i_, [8, 128], BF16) for i_ in range(2)]
    attm = [k.alloc("attm%d" % i, [4, 128], BF16) for i in range(2)]
    o32 = k.alloc("o32", [1024])
    junk = k.alloc("junk", [1024])
    junkb = k.alloc("junkb", [1024])
    OT = k.alloc("OT", [8, 128], BF16)
    S32 = k.alloc("S32", [4, 128])
    Sb = k.alloc("Sb", [4, 128], BF16)
    st = k.alloc("st", [64])
    lnst = k.alloc("lnst", [8])
    k.memset(S32, 0.0)
    k.memset(Sb, 0.0, eng="pool")
    k.memset(st, 0.0)
    cols = [(0, 512), (512, 512), (1024, 512), (1536, 512), (2048, 512), (2560, 512), (3072, 16)]

    def front(n):
        i = n % 2
        v8b, Gb, kend, qkT, dS = v8b_2[i], Gb_2[i], kend_2[i], qkT_2[i], dS_2[i]
        Xn = X.sub((slice(None), n, slice(None)), n)
        transpose_tile(k, C, Xn, XTn)
        for gi, (c0, w) in enumerate(cols):
            ps = k.psum()
            for c in range(8):
                k.mm(ps[:, 0:w], XTn[:, c, :], Win[:, c, c0:c0 + w].k(c), start=(c == 0), stop=(c == 7))
            if gi < 2:
                k.cp(qk32[:, c0:c0 + w], ps[:, 0:w], eng="dve")
            elif gi < 4:
                k.cp(v8b[:, c0 - 1024:c0 - 1024 + w], ps[:, 0:w], eng="act")
            elif gi < 6:
                k.act(junk[:, 0:512], ps[:, 0:w], AF.Silu)
                k.tt(Gb[:, c0 - 2048:c0 - 2048 + w], junk[:, 0:512], normw[:, c0 - 2048:c0 - 2048 + w], ALU.mult, eng="pool")
            else:
                k.cp(ga32, ps[:, 0:16], eng="dve")
        ps = k.psum()
        k.tr(ps[0:16, 0:128], ga32, C["ident"])
        k.cp(gaT[0:16, :], ps[0:16, 0:128], eng="dve")
        psz = k.psum()
        k.mm(psz[:, 0:256], gaT[0:16, :], wg2[0:16, :], start=True, stop=False)
        k.mm(psz[:, 0:256], C["ones"][0:1, :], bg[0:1, :], start=False, stop=True)
        k.act(l32, psz[:, 0:256], AF.Exp, scale=-1.0)
        k.act(l32, l32, AF.Ln, bias=1.0, scale=1.0)
        psb = k.psum()
        k.mm(psb[:, 0:256], C["tri"], l32)
        k.mm(psb[:, 256:512], C["upp"], l32)
        psc = k.psum()
        k.mm(psc[:, 0:1], l32[:, 0:128], C["ones"][:, 0:1])
        k.mm(psc[:, 1:2], l32[:, 128:256], C["ones"][:, 0:1])
        k.act(Etab[:, 0:256], psb[:, 0:256], AF.Exp, scale=-1.0 / 16.0)
        k.ts(Etab[:, 0:256], Etab[:, 0:256], 0.125, None, ALU.mult)
        k.act(Etab[:, 256:512], psb[:, 0:256], AF.Exp, scale=1.0 / 16.0)
        k.act(Ee[:, 0, :], psb[:, 256:512], AF.Exp, scale=-1.0 / 16.0)
        k.act(dS[:, 0:2], psc[:, 0:2], AF.Exp, scale=-1.0 / 16.0)
        rv = qk32[:, 512:1024].re("p (h t m) -> p h t m", t=2, m=32)
        x1 = rv[:, :, 0, :]
        x2 = rv[:, :, 1, :]
        cs = B(cosT.ap[:, n, :].unsqueeze(1).to_broadcast([128, 8, 32]), cosT.key)
        sn = B(sinT.ap[:, n, :].unsqueeze(1).to_broadcast([128, 8, 32]), sinT.key)
        jv = junk.re("p (a h m) -> p a h m", h=8, m=32)
        k.tt(jv[:, 0], x1, cs, ALU.mult)
        k.tt(jv[:, 1], x2, sn, ALU.mult)
        k.tt(jv[:, 2], x1, sn, ALU.mult, eng="pool")
        k.tt(jv[:, 3], x2, cs, ALU.mult, eng="pool")
        k.tt(x1, jv[:, 0], jv[:, 1], ALU.subtract)
        k.tt(x2, jv[:, 2], jv[:, 3], ALU.add)
        kv = qk32.re("p (a b) -> p a b", b=512)[:, :, 256:512]
        k.tt(kend, kv, Ee, ALU.mult)
        k.tt(qk32, qk32, Etab, ALU.mult)
        transpose_tile(k, C, qk32, qkT)

    def back(n):
        i = n % 2
        v8b, Gb, kend, qkT, dS = v8b_2[i], Gb_2[i], kend_2[i], qkT_2[i], dS_2[i]
        junk = junkb
        Xn = X.sub((slice(None), n, slice(None)), n)
        for hg in range(2):
            psA = [k.psum(), k.psum()]
            for h4 in range(4):
                qc = hg * 4 + h4 // 2
                kc = qc + 2
                r0 = (h4 % 2) * 64
                k.mm(psA[h4 % 2][:, (h4 // 2) * 128:(h4 // 2 + 1) * 128], qkT[r0:r0 + 64, kc, :], qkT[r0:r0 + 64, qc, :])
            am = attm[hg]
            amv = am.re("p (a b) c -> p a b c", b=2)
            for par in range(2):
                k.tt(amv[:, :, par, :], psA[par][:, 0:256].re("p (a b) -> p a b", b=128), C["tri"].bc([128, 2, 128], 1), ALU.mult)
            psO = [k.psum(), k.psum()]
            for h4 in range(4):
                h = hg * 4 + h4
                qc = hg * 4 + h4 // 2
                r0 = (h4 % 2) * 64
                pair = hg * 2 + h4 // 2
                po = psO[h4 % 2][:, (h4 // 2) * 128:(h4 // 2 + 1) * 128]
                k.mm(po, qkT[r0:r0 + 64, qc, :], Sb[r0:r0 + 64, pair, :], start=True, stop=False)
                k.mm(po, am[:, h4, :], v8b[:, h * 128:(h + 1) * 128], start=False, stop=True)
            ovh = o32[:, hg * 512:(hg + 1) * 512].re("p (a b c) -> p a b c", b=2, c=128)
            for par in range(2):
                k.cp(B(ovh.ap[:, :, par, :], ("o32", hg)), psO[par][:, 0:256].re("p (a b) -> p a b", b=128), eng="act")
            psD = k.psum()
            for h4 in range(4):
                h = hg * 4 + h4
                k.mm(psD[:, h4 * 128:(h4 + 1) * 128], kend[:, hg, (h4 // 2) * 128:(h4 // 2 + 1) * 128], v8b[:, h * 128:(h + 1) * 128])
            for h4 in range(4):
                r0 = (h4 % 2) * 64
                pair = hg * 2 + h4 // 2
                k.stt(S32[r0:r0 + 64, pair, :].k(hg), S32[r0:r0 + 64, pair, :].k(hg), dS[r0:r0 + 64, pair:pair + 1],
                      psD[r0:r0 + 64, h4 * 128:(h4 + 1) * 128], ALU.mult, ALU.add)
            k.cp(Sb[:, hg * 2:hg * 2 + 2, :].k(hg), S32[:, hg * 2:hg * 2 + 2, :].k(hg), eng="pool")
        ov = o32.re("p (h v) -> p h v", v=128)
        k.tt(junk, o32, o32, ALU.mult, eng="pool")
        k.red(st[:, 0:8].k("s2"), junk.re("p (h v) -> p h v", v=128))
        k.red(st[:, 12:16].k("s1"), ov[:, 4:8, :])
        k.ts(st[:, 16:24].k("mean"), st[:, 8:16].k("s1"), 1.0 / 128, None, ALU.mult)
        k.tt(st[:, 24:32].k("msq"), st[:, 16:24].k("mean"), st[:, 16:24].k("mean"), ALU.mult)
        k.stt(st[:, 32:40].k("var"), st[:, 0:8].k("s2"), 1.0 / 128, st[:, 24:32].k("msq"), ALU.mult, ALU.subtract)
        k.act(st[:, 32:40].k("var"), st[:, 32:40].k("var"), AF.Sqrt, bias=EPS, scale=1.0)
        k.recip(st[:, 40:48].k("rstd"), st[:, 32:40].k("var"))
        k.stt(st[:, 48:56].k("nb"), st[:, 16:24].k("mean"), -1.0, st[:, 40:48].k("rstd"), ALU.mult, ALU.mult)
        k.tt(ov, ov, st[:, 40:48].k("rstd").bc([128, 8, 128], 2), ALU.mult)
        k.tt(ov, ov, st[:, 48:56].k("nb").bc([128, 8, 128], 2), ALU.add)
        k.tt(o32, o32, Gb, ALU.mult)
        transpose_tile(k, C, o32, OT)
        for half in range(2):
            ps = k.psum()
            for c in range(8):
                k.mm(ps, OT[:, c, :], Wout[:, c, half * 512:(half + 1) * 512], start=(c == 0), stop=(c == 7))
            k.stt(Xn[:, half * 512:(half + 1) * 512], Xn[:, half * 512:(half + 1) * 512], ALPHA, ps, ALU.mult, ALU.add)
        ln_tile(k, Xn, g_bc, b_bc, Xn, lnst, junk)

    FB, BB = (0, 5), (5, 8)
    k.issue_merged(k.record(lambda: front(0), banks=FB), [])
    for n in range(NT):
        fr = k.record(lambda: front(n + 1), banks=FB) if n + 1 < NT else []
        bk = k.record(lambda: back(n), banks=BB)
        k.issue_merged(bk, fr)
    k.reset(m0)


def phase_ffn(k, dr, C, X, experts, ln_g, ln_b, out_dram=None, router=None):
    k.P.barrier()
    m0 = k.mark()
    g_bc = k.alloc("ln_g", [1024])
    b_bc = k.alloc("ln_b", [1024])
    k.dma("sp", g_bc, ln_g.partition_broadcast(128))
    k.dma("sp", b_bc, ln_b.partition_broadcast(128))
    XTh = k.alloc("XTh", [8, 1024], BF16)
    actb = k.alloc("actb", [28, 1024], BF16)
    NW = 3
    wgu = [k.alloc("wgu%d" % i, [8, 256], BF16) for i in range(NW)]
    ND = 6
    wdb = [k.alloc("wd%d" % i, [1024], BF16) for i in range(ND)]
    sg = [k.alloc("sg%d" % i, [512]) for i in range(2)]
    junk = k.alloc("junk", [1024])
    lnst = k.alloc("lnst", [8])
    wi = 0
    di = 0
    si = 0
    if router is not None:
        Wr = k.alloc("Wr", [8, 8])
        k.dma("sp", Wr, router[0].rearrange("(c p) e -> p c e", p=128))
        br = k.alloc("br", [8])
        k.dma("sp", br[0:1, :], router[1])
        CB = k.alloc("CB", [16, 8])
        XT32 = k.alloc("XT32", [8, 128])
        rt = k.alloc("rt", [64])
    for half in range(2):
        for t in range(8):
            n = half * 8 + t
            Xn = X.sub((slice(None), n, slice(None)), n)
            if router is None:
                transpose_tile(k, C, Xn, B(XTh.ap[:, :, t * 128:(t + 1) * 128], ("XTh", t)))
            else:
                transpose_tile(k, C, Xn, B(XTh.ap[:, :, t * 128:(t + 1) * 128], ("XTh", t)), dst32=XT32)
                ps = k.psum()
                for c in range(8):
                    k.mm(ps[:, 0:8], XT32[:, c, :], Wr[:, c, :], start=(c == 0), stop=False)
                k.mm(ps[:, 0:8], C["ones"][0:1, :], br[0:1, :], start=False, stop=True)
                lg = rt[:, 0:8].k("lg")
                k.cp(lg, ps[:, 0:8])
                k.red(rt[:, 8:9].k("m1"), lg, op=ALU.max)
                k.ts(rt[:, 16:24].k("eq1"), lg, rt[:, 8:9].k("m1"), None, ALU.is_equal)
                k.stt(rt[:, 24:32].k("l2"), rt[:, 16:24].k("eq1"), -1e30, lg, ALU.mult, ALU.add)
                k.red(rt[:, 9:10].k("m2"), rt[:, 24:32].k("l2"), op=ALU.max)
                k.ts(rt[:, 32:40].k("eq2"), rt[:, 24:32].k("l2"), rt[:, 9:10].k("m2"), None, ALU.is_equal)
                k.tt(rt[:, 10:11].k("d"), rt[:, 9:10].k("m2"), rt[:, 8:9].k("m1"), ALU.subtract)
                k.act(rt[:, 11:12].k("e"), rt[:, 10:11].k("d"), AF.Exp)
                k.ts(rt[:, 12:13].k("den"), rt[:, 11:12].k("e"), 1.0, None, ALU.add)
                k.recip(rt[:, 13:14].k("g1"), rt[:, 12:13].k("den"))
                k.tt(rt[:, 14:15].k("g2"), rt[:, 11:12].k("e"), rt[:, 13:14].k("g1"), ALU.mult)
                cbn = CB[:, n, :].k(n)
                k.ts(cbn, rt[:, 16:24].k("eq1"), rt[:, 13:14].k("g1"), None, ALU.mult)
                k.stt(cbn, rt[:, 32:40].k("eq2"), rt[:, 14:15].k("g2"), cbn, ALU.mult, ALU.add)
                k.act(Xn, Xn, AF.Identity, scale=ALPHA)
        for ei, (wgu_l, wd) in enumerate(experts):
            comb = None if router is None else (CB, ei)
            for f in range(28):
                wb = wgu[wi % NW]
                wi += 1
                k.dma("pool", wb, wgu_l[f])
                for tb in range(2):
                    psG = k.psum()
                    psU = k.psum()
                    for c in range(8):
                        k.mm(psG, wb[:, c, 0:128], XTh[:, c, tb * 512:(tb + 1) * 512], start=(c == 0), stop=(c == 7))
                    for c in range(8):
                        k.mm(psU, wb[:, c, 128:256], XTh[:, c, tb * 512:(tb + 1) * 512], start=(c == 0), stop=(c == 7))
                    s = sg[si % 2]
                    si += 1
                    k.act(s, psG, AF.Silu)
                    k.tt(actb.sub((slice(None), f, slice(tb * 512, (tb + 1) * 512)), f, tb), s, psU, ALU.mult)
            for tg in range(2):
                pss = [k.psum() for _ in range(8)]
                for f in range(28):
                    db = wdb[di % ND]
                    di += 1
                    k.dma("pool", db, wd[f * 128:(f + 1) * 128, :])
                    for t4 in range(4):
                        t = tg * 4 + t4
                        for hh in range(2):
                            k.mm(pss[t4 * 2 + hh], actb[:, f, t * 128:(t + 1) * 128].k(f, t // 4),
                                 db[:, hh * 512:(hh + 1) * 512], start=(f == 0), stop=(f == 27))
                for t4 in range(4):
                    n = half * 8 + tg * 4 + t4
                    Xn = X.sub((slice(None), n, slice(None)), n)
                    for hh in range(2):
                        xs = Xn[:, hh * 512:(hh + 1) * 512]
                        if comb is None:
                            k.stt(xs, xs, ALPHA, pss[t4 * 2 + hh], ALU.mult, ALU.add)
                        else:
                            cb, e = comb
                            k.stt(xs, pss[t4 * 2 + hh], cb[:, n, e:e + 1].k(n), xs, ALU.mult, ALU.add)
        for t in range(8):
            n = half * 8 + t
            Xn = X.sub((slice(None), n, slice(None)), n)
            ln_tile(k, Xn, g_bc, b_bc, Xn, lnst, junk)
            if out_dram is not None:
                k.dma("sp", out_dram[n * 128:(n + 1) * 128, :], Xn)
    k.reset(m0)


def host_consts_l1():
    c = {}
    p = np.arange(128)[:, None]
    f = np.arange(128)[None, :]
    same = (p // 64) == (f // 64)
    c["tri64"] = ((p <= f) & same).astype(np.float32)
    c["blk64"] = same.astype(np.float32)
    c["nstrict64"] = -((f < p) & same).astype(np.float32)
    c["slt"] = (p < f).astype(np.float32)
    c["iota512"] = np.tile(np.arange(512, dtype=np.float32)[None, :], (128, 1))
    return c


def gdn_pass_a(k, dr, C, X, scr):
    k.P.barrier()
    m0 = k.mark()
    Wg = k.alloc("Wg", [8, 1040], BF16)
    wv = dr["wg_l"].rearrange("(c p) n -> p c n", p=128)
    for c in range(0, 8, 4):
        k.dma("pool", Wg.sub((slice(None), slice(c, c + 4), slice(None)), c), wv[:, c:c + 4, :])
    cw = k.alloc("cw", [24, 4])
    k.dma("sp", cw, dr["convw_l"])
    cnb = k.alloc("cnb", [1024])
    k.dma("sp", cnb, dr["c_norm"].partition_broadcast(128))
    dtb = k.alloc("dtb", [8])
    k.dma("sp", dtb, dr["c_dt_bias"].partition_broadcast(128))
    nA = k.alloc("nA", [8])
    k.dma("sp", nA, dr["c_a_log"].partition_broadcast(128))
    k.act(nA, nA, AF.Exp)
    k.ts(nA, nA, -1.0, None, ALU.mult)
    XTb = k.alloc("XTb", [8, 512], BF16)
    NWB = 3
    wqb = [k.alloc("wq%d" % i, [8, 128], BF16) for i in range(NWB)]
    pre = [k.alloc("pre%d" % i, [515]) for i in range(2)]
    acc = [k.alloc("acc%d" % i, [512]) for i in range(3)]
    sbuf_s = [k.alloc("s%d" % i, [512]) for i in range(3)]
    sq = [k.alloc("sq%d" % i, [512]) for i in range(3)]
    rsb = [k.alloc("rs%d" % i, [512]) for i in range(3)]
    outb = [k.alloc("ob%d" % i, [512], BF16) for i in range(4)]
    carry = k.alloc("carry", [24, 3])
    k.memset(carry, 0.0)
    junk = k.alloc("junk", [1024])
    Gt = [k.alloc("Gt%d" % i, [1024], BF16) for i in range(2)]
    ab = [k.alloc("ab%d" % i, [16]) for i in range(2)]
    zt = k.alloc("zt", [8])
    BG = scr["BG"]
    it = 0
    for b in range(4):
        for t in range(4):
            n = 4 * b + t
            Xn = X.sub((slice(None), n, slice(None)), n)
            transpose_tile(k, C, Xn, B(XTb.ap[:, :, t * 128:(t + 1) * 128], ("XTb", t)))
        def head(ch, it):
            wb = wqb[it % NWB]
            pr = pre[it % 2]
            ac = acc[it % 3]
            if it < 2:
                k.dma("pool", wb, dr["wqkv_l"][ch])
            ps = k.psum()
            for c in range(8):
                k.mm(ps, wb[:, c, :], XTb[:, c, :], start=(c == 0), stop=(c == 7))
            if it + 2 < 96:
                k.dma("pool", wqb[(it + 2) % NWB], dr["wqkv_l"][(it + 2) % 24])
            k.cp(pr[:, 3:515], ps, eng="act")
            k.cp(pr[:, 0:3], carry[:, ch, :].k(ch), eng="pool")
            k.cp(carry[:, ch, :].k(ch), pr[:, 512:515], eng="pool")
            k.ts(ac, pr[:, 0:512], cw[:, ch, 0:1], None, ALU.mult)
            k.stt(ac, pr[:, 1:513], cw[:, ch, 1:2], ac, ALU.mult, ALU.add)
            k.stt(ac, pr[:, 2:514], cw[:, ch, 2:3], ac, ALU.mult, ALU.add)
            k.stt(ac, pr[:, 3:515], cw[:, ch, 3:4], ac, ALU.mult, ALU.add)

        def mid(ch, it):
            ac = acc[it % 3]
            s = sbuf_s[it % 3]
            q2 = sq[it % 3]
            rs = rsb[it % 3]
            ob = outb[it % 4]
            if ch < 16:
                k.act(s, ac, AF.Silu)
                k.tt(q2, s, s, ALU.mult, eng="pool")
                pss = k.psum()
                k.mm(pss[0:1, :], C["ones"][:, 0:1], q2)
                k.act(rs[0:1, :], pss[0:1, :], AF.Ln, bias=1e-6, scale=1.0)
                k.act(rs[0:1, :], rs[0:1, :], AF.Exp, scale=-0.5)
            else:
                k.act(ob, ac, AF.Silu)

        def tail(ch, it):
            s = sbuf_s[it % 3]
            rs = rsb[it % 3]
            ob = outb[it % 4]
            if ch < 16:
                psb = k.psum()
                k.mm(psb, C["ones"][0:1, :], rs[0:1, :])
                k.stt(ob, s, (128.0 ** -0.5) if ch < 8 else 1.0, psb, ALU.mult, ALU.mult)
                dst = scr["qs"] if ch < 8 else scr["ks"]
                h = ch % 8
            else:
                dst = scr["vs"]
                h = ch - 16
            dv = dst.ap[4 * b:4 * b + 4, :, h, :].rearrange("n k t -> k n t")
            k._op("sp", (lambda dv, ob: lambda e: e.dma_start(out=dv, in_=ob.ap.rearrange("p (n t) -> p n t", t=128)))(dv, ob),
                  reads=[ob.key], writes=[dst.key + (4 * b + tt_, h) for tt_ in range(4)], dma=True)

        HB, MB, TB = (0, 3), (3, 5), (5, 8)
        k.issue_merged(k.record(lambda: head(0, it), banks=HB), [])
        k.issue_merged(k.record(lambda: mid(0, it), banks=MB), k.record(lambda: head(1, it + 1), banks=HB))
        for ch in range(24):
            tl = k.record(lambda: tail(ch, it), banks=TB)
            md = k.record(lambda: mid(ch + 1, it + 1), banks=MB) if ch + 1 < 24 else []
            hd_ops = k.record(lambda: head(ch + 2, it + 2), banks=HB) if ch + 2 < 24 else []
            k.issue_merged(tl, k.merge_lists(md, hd_ops))
            it += 1
        for t in range(4):
            n = 4 * b + t
            G = Gt[n % 2]
            a = ab[n % 2]
            for gi, (c0, w) in enumerate([(0, 512), (512, 512), (1024, 16)]):
                ps = k.psum()
                for c in range(8):
                    k.mm(ps[:, 0:w], XTb[:, c, t * 128:(t + 1) * 128], Wg[:, c, c0:c0 + w], start=(c == 0), stop=(c == 7))
                if gi < 2:
                    k.act(junk[:, 0:512], ps, AF.Silu)
                    k.tt(G[:, c0:c0 + 512], junk[:, 0:512], cnb[:, c0:c0 + 512], ALU.mult, eng="pool")
                else:
                    k.cp(a, ps[:, 0:16])
            k.dma("sp", scr["gs"].sub(n, n), G)
            k.act(BG[:, n, 8:16].k(n), a[:, 8:16], AF.Sigmoid)
            k.tt(zt, a[:, 0:8], dtb, ALU.add)
            k.act(zt, zt, AF.Exp)
            k.act(zt, zt, AF.Ln, bias=1.0, scale=1.0)
            k.tt(BG[:, n, 0:8].k(n), zt, nA, ALU.mult)
    k.reset(m0)


def gdn_pass_b(k, dr, C, C1, X, scr, ln_g, ln_b):
    k.P.barrier()
    m0 = k.mark()
    Wout = k.alloc("Wout", [8, 1024], BF16)
    wo = dr["w_out1"].rearrange("(c p) n -> p c n", p=128)
    for c in range(0, 8, 4):
        k.dma("pool", Wout.sub((slice(None), slice(c, c + 4), slice(None)), c), wo[:, c:c + 4, :])
    g_bc = k.alloc("ln_g", [1024])
    b_bc = k.alloc("ln_b", [1024])
    k.dma("sp", g_bc, ln_g.partition_broadcast(128))
    k.dma("sp", b_bc, ln_b.partition_broadcast(128))
    identb = k.alloc("identb", [128], BF16)
    k.cp(identb, C["ident"])
    BG = scr["BG"]
    qT = [k.alloc("qT%d" % i, [8, 128], BF16) for i in range(2)]
    kT = [k.alloc("kT%d" % i, [8, 128], BF16) for i in range(2)]
    vT = [k.alloc("vT%d" % i, [8, 128], BF16) for i in range(2)]
    Gt = [k.alloc("G%d" % i, [1024], BF16) for i in range(2)]
    sm = k.alloc("sm", [64])
    junk = k.alloc("junk", [8, 128])
    junk2 = k.alloc("junk2", [8, 128])
    egr_2 = [k.alloc("egr%d" % i_, [8, 128]) for i_ in range(2)]
    E = k.alloc("E", [8, 128])
    Dl = k.alloc("Dl", [8, 128])
    aqk_2 = [k.alloc("aqk%d" % i_, [8, 128], BF16) for i_ in range(2)]
    Pm = k.alloc("Pm", [8, 128])
    PT = k.alloc("PT", [8, 128])
    RT = k.alloc("RT", [8, 128])
    Tb = k.alloc("Tb", [8, 128], BF16)
    Pb = k.alloc("Pb", [8, 128], BF16)
    PTb = k.alloc("PTb", [8, 128], BF16)
    vb = k.alloc("vb", [8, 128], BF16)
    kbg = k.alloc("kbg", [8, 128], BF16)
    kdec_2 = [k.alloc("kdec%d" % i_, [8, 128], BF16) for i_ in range(2)]
    u32_2 = [k.alloc("u32%d" % i_, [8, 128]) for i_ in range(2)]
    wT_2 = [k.alloc("wT%d" % i_, [8, 128], BF16) for i_ in range(2)]
    qdT_2 = [k.alloc("qdT%d" % i_, [8, 128], BF16) for i_ in range(2)]
    vnew = k.alloc("vnew", [8, 128], BF16)
    o32 = k.alloc("o32", [8, 128])
    OT = k.alloc("OT", [8, 128], BF16)
    S32 = k.alloc("S32", [8, 128])
    Sb = k.alloc("Sb", [8, 128], BF16)
    st = k.alloc("st", [16])
    lnst = k.alloc("lnst", [8])
    k.memset(S32, 0.0)
    k.memset(Sb, 0.0, eng="pool")
    tri = C1["tri64"]
    HG = [(0, 4), (4, 8)]

    def g4(buf, hg):
        return B(buf.ap[:, hg * 4:hg * 4 + 4, :], buf.key + (hg,))

    def hd(buf, h):
        return B(buf.ap[:, h, :], buf.key + (h // 4,))

    def ps4(ps):
        return ps.re("p (a b) -> p a b", b=128)

    def load(n):
        i = n % 2
        k.dma("sp", qT[i], scr["qs"].sub(n, n))
        k.dma("sp", kT[i], scr["ks"].sub(n, n))
        k.dma("sp", vT[i], scr["vs"].sub(n, n))
        k.dma("sp", Gt[i], scr["gs"].sub(n, n))

    load(0)

    def prep(n):
        i = n % 2
        q_, k_, v_, G = qT[i], kT[i], vT[i], Gt[i]
        egr, aqk, kdec, u32, wT, qdT = egr_2[i], aqk_2[i], kdec_2[i], u32_2[i], wT_2[i], qdT_2[i]
        g32 = BG[:, n, 0:8].k(n)
        beta = BG[:, n, 8:16].k(n)
        ps = k.psum()
        k.mm(ps[:, 0:8], tri, g32)
        k.mm(ps[:, 8:16], C1["blk64"], g32)
        gc = sm[:, 0:8].k("gc")
        k.cp(gc, ps[:, 0:8])
        k.act(sm[:, 8:16].k("egc"), ps[:, 0:8], AF.Exp)
        k.tt(sm[:, 16:24].k("bge"), beta, sm[:, 8:16].k("egc"), ALU.mult)
        k.tt(sm[:, 24:32].k("dd"), ps[:, 8:16], gc, ALU.subtract)
        k.act(sm[:, 32:40].k("edec"), sm[:, 24:32].k("dd"), AF.Exp)
        k.cp(junk, g32.bc([128, 8, 128], 2))
        psR = [k.psum(), k.psum()]
        for h in range(8):
            k.mm(psR[h // 4][:, (h % 4) * 128:(h % 4 + 1) * 128], junk[:, h, :], tri)
        for hg in range(2):
            k.act(g4(egr, hg), ps4(psR[hg]), AF.Exp)
            k.tt(g4(E, hg), ps4(psR[hg]), gc[:, hg * 4:hg * 4 + 4].bc([128, 4, 128], 2), ALU.subtract)
        k.ts(Dl, E, 0.0, -1.0, ALU.max, ALU.mult)
        k.act(Dl, Dl, AF.Exp)
        k.tt(Dl, Dl, C1["nstrict64"].bc([128, 8, 128], 1), ALU.mult, eng="pool")
        k.tt(Dl, Dl, beta.bc([128, 8, 128], 2), ALU.mult)
        k.ts(E, E, 0.0, None, ALU.min)
        k.act(E, E, AF.Exp)
        k.tt(E, E, tri.bc([128, 8, 128], 1), ALU.mult, eng="pool")
        psKQ = [k.psum(), k.psum()]
        psKK = [k.psum(), k.psum()]
        for h in range(8):
            sl = slice((h % 4) * 128, (h % 4 + 1) * 128)
            k.mm(psKQ[h // 4][:, sl], k_[:, h, :], q_[:, h, :])
            k.mm(psKK[h // 4][:, sl], k_[:, h, :], k_[:, h, :])
        for hg in range(2):
            k.tt(g4(aqk, hg), ps4(psKQ[hg]), g4(E, hg), ALU.mult)
            k.tt(g4(Pm, hg), ps4(psKK[hg]), g4(Dl, hg), ALU.mult)
        psT = [k.psum(), k.psum()]
        for h in range(8):
            k.tr(psT[h // 4][:, (h % 4) * 128:(h % 4 + 1) * 128], hd(Pm, h), C["ident"])
        for hg in range(2):
            k.cp(g4(PT, hg), ps4(psT[hg]), eng="act")
            k.tt(g4(RT, hg), ps4(psT[hg]), C["ident"].bc([128, 4, 128], 1), ALU.add)
        for m in range(1, 6):
            lo = m >= 2
            Pa, PTa, RTa = (Pb, PTb, Tb) if lo else (Pm, PT, RT)
            psP = [k.psum(), k.psum()]
            psPT = [k.psum(), k.psum()] if m < 5 else None
            for hg in range(2):
                for h in range(hg * 4, hg * 4 + 4):
                    k.mm(psP[hg][:, (h % 4) * 128:(h % 4 + 1) * 128], hd(PTa, h), hd(Pa, h))
                if m < 5:
                    for h in range(hg * 4, hg * 4 + 4):
                        k.mm(psPT[hg][:, (h % 4) * 128:(h % 4 + 1) * 128], hd(Pa, h), hd(PTa, h))
            nlo = m >= 1
            Po, PTo = (Pb, PTb) if m >= 2 else (Pm, PT)
            for hg in range(2):
                k.cp(g4(Po, hg), ps4(psP[hg]), eng="act")
                if m < 5:
                    k.cp(g4(PTo, hg), ps4(psPT[hg]), eng="dve")
            psR2 = [k.psum(), k.psum()]
            for h in range(8):
                k.mm(psR2[h // 4][:, (h % 4) * 128:(h % 4 + 1) * 128], hd(Po, h), hd(RTa, h))
            for hg in range(2):
                k.tt(g4(RT, hg), g4(RT, hg), ps4(psR2[hg]), ALU.add)
                if m < 5:
                    k.cp(g4(Tb, hg), g4(RT, hg), eng="act")
                if m == 1:
                    k.cp(g4(Pb, hg), g4(Pm, hg), eng="act")
                    k.cp(g4(PTb, hg), g4(PT, hg), eng="pool")
        k.cp(Tb, RT, eng="act")
        psK = k.psum(BF16)
        psV = k.psum(BF16)
        for h in range(8):
            k.tr(psK[:, h * 128:(h + 1) * 128], k_[:, h, :], identb)
        for h in range(8):
            k.tr(psV[:, h * 128:(h + 1) * 128], v_[:, h, :], identb)
        pK = psK.re("p (a b) -> p a b", b=128)
        pV = psV.re("p (a b) -> p a b", b=128)
        k.tt(vb, pV, beta.bc([128, 8, 128], 2), ALU.mult)
        k.tt(kbg, pK, sm[:, 16:24].k("bge").bc([128, 8, 128], 2), ALU.mult)
        k.tt(kdec, pK, sm[:, 32:40].k("edec").bc([128, 8, 128], 2), ALU.mult)
        psU = [k.psum(), k.psum()]
        psW = [k.psum(), k.psum()]
        for h in range(8):
            sl = slice((h % 4) * 128, (h % 4 + 1) * 128)
            k.mm(psU[h // 4][:, sl], hd(Tb, h), hd(vb, h))
            k.mm(psW[h // 4][:, sl], hd(kbg, h), hd(Tb, h))
        for hg in range(2):
            k.cp(g4(u32, hg), ps4(psU[hg]), eng="act")
            k.cp(g4(wT, hg), ps4(psW[hg]), eng="dve")
        k.tt(qdT, q_, egr, ALU.mult, eng="pool")

    def rec(n):
        i = n % 2
        G = Gt[i]
        egr, aqk, kdec, u32, wT, qdT = egr_2[i], aqk_2[i], kdec_2[i], u32_2[i], wT_2[i], qdT_2[i]
        Xn = X.sub((slice(None), n, slice(None)), n)
        for c in range(2):
            r = slice(c * 64, c * 64 + 64)
            psWS = [k.psum(), k.psum()]
            for h in range(8):
                k.mm(psWS[h // 4][:, (h % 4) * 128:(h % 4 + 1) * 128], hd(wT, h), Sb[:, h, :])
            for hg in range(2):
                k.tt(vnew[r, hg * 4:hg * 4 + 4, :], u32[r, hg * 4:hg * 4 + 4, :], ps4(psWS[hg])[r], ALU.subtract)
            psS = [k.psum(), k.psum()]
            for h in range(8):
                k.mm(psS[h // 4][:, (h % 4) * 128:(h % 4 + 1) * 128], kdec[r, h, :], vnew[r, h, :])
            col = c * 64 + 63
            for h in range(8):
                k.stt(S32[:, h, :], S32[:, h, :], egr[:, h, col:col + 1], psS[h // 4][:, (h % 4) * 128:(h % 4 + 1) * 128],
                      ALU.mult, ALU.add)
            psO = [k.psum(), k.psum()]
            for h in range(8):
                po = psO[h // 4][:, (h % 4) * 128:(h % 4 + 1) * 128]
                k.mm(po, hd(qdT, h), Sb[:, h, :], start=True, stop=False)
                k.mm(po, aqk[r, h, :], vnew[r, h, :], start=False, stop=True)
            for hg in range(2):
                k.cp(o32[r, hg * 4:hg * 4 + 4, :], ps4(psO[hg])[r], eng="act")
            k.cp(Sb, S32, eng="act")
        k.tt(junk2, o32, o32, ALU.mult, eng="pool")
        k.red(st[:, 0:8].k("s2"), junk2)
        k.act(st[:, 0:8].k("s2"), st[:, 0:8].k("s2"), AF.Sqrt, bias=EPS, scale=1.0 / 128)
        k.recip(st[:, 8:16].k("rstd"), st[:, 0:8].k("s2"))
        k.tt(o32, o32, st[:, 8:16].k("rstd").bc([128, 8, 128], 2), ALU.mult)
        of = o32.re("p a b -> p (a b)")
        k.tt(of, of, G, ALU.mult)
        transpose_tile(k, C, of, OT)
        for half in range(2):
            ps = k.psum()
            for c in range(8):
                k.mm(ps, OT[:, c, :], Wout[:, c, half * 512:(half + 1) * 512], start=(c == 0), stop=(c == 7))
            k.stt(Xn[:, half * 512:(half + 1) * 512], Xn[:, half * 512:(half + 1) * 512], ALPHA, ps, ALU.mult, ALU.add)
        ln_tile(k, Xn, g_bc, b_bc, Xn, lnst, junk2.re("p a b -> p (a b)"))

    pr = k.record(lambda: prep(0), banks=(0, 5))
    k.issue_merged(pr, [])
    for n in range(NT):
        if n + 1 < NT:
            load(n + 1)
            pr = k.record(lambda: prep(n + 1), banks=(0, 5))
        else:
            pr = []
        rc_ = k.record(lambda: rec(n), banks=(5, 8))
        k.issue_merged(rc_, pr)
    k.reset(m0)


ARENA = 53120
SPARSE_MOE = True


def wgu_layout(w):
    g = w[:, :3584].reshape(8, 128, 28, 128)
    u = w[:, 3584:].reshape(8, 128, 28, 128)
    gu = np.concatenate([g, u], axis=3)
    return np.ascontiguousarray(gu.transpose(2, 1, 0, 3))


def shared_inputs(inp):
    m = dict(host_consts())
    m.update(host_consts_l1())
    m["inv_freq"] = m["inv_freq"].reshape(32)
    m["w_in0"] = np.ascontiguousarray(inp["ab_w_in"][0][:, l0_perm()])
    m["w_out0"] = np.ascontiguousarray(inp["ab_w_out"][0])
    m["w_gate2"] = np.ascontiguousarray(inp["gla_w_gate2"][0])
    m["b_gate"] = np.ascontiguousarray(inp["gla_b_gate"][0].reshape(1, 256))
    m["norm0"] = np.concatenate([inp["gla_norm"][0].reshape(512), inp["ret_norm"][0].reshape(512)])
    m["ln1_g"] = np.ascontiguousarray(inp["ab_ln1_g"][0]); m["ln1_b"] = np.ascontiguousarray(inp["ab_ln1_b"][0])
    m["ln2_g"] = np.ascontiguousarray(inp["ab_ln2_g"][0]); m["ln2_b"] = np.ascontiguousarray(inp["ab_ln2_b"][0])
    m["wgu0"] = wgu_layout(inp["ffn_w_gu"][0]); m["wd0"] = np.ascontiguousarray(inp["ffn_w_down"][0])
    cw = inp["c_w_in"][0]
    m["wqkv_l"] = np.ascontiguousarray(cw[:, 0:3072].reshape(8, 128, 24, 128).transpose(2, 1, 0, 3))
    m["wg_l"] = np.ascontiguousarray(np.concatenate([cw[:, 3088:4112], cw[:, 3072:3088]], axis=1))
    m["convw_l"] = np.ascontiguousarray(inp["c_conv_w"][0].reshape(4, 24, 128).transpose(2, 1, 0))
    m["c_a_log"] = np.ascontiguousarray(inp["c_a_log"][0]); m["c_dt_bias"] = np.ascontiguousarray(inp["c_dt_bias"][0])
    m["c_norm"] = np.ascontiguousarray(inp["c_norm"][0].reshape(1024))
    m["w_out1"] = np.ascontiguousarray(inp["c_w_out"][0])
    m["ln3_g"] = np.ascontiguousarray(inp["c_ln1_g"][0]); m["ln3_b"] = np.ascontiguousarray(inp["c_ln1_b"][0])
    m["ln4_g"] = np.ascontiguousarray(inp["c_ln2_g"][0]); m["ln4_b"] = np.ascontiguousarray(inp["c_ln2_b"][0])
    m["w_router"] = np.ascontiguousarray(inp["moe_w_router"][0]); m["b_router"] = np.ascontiguousarray(inp["moe_b_router"][0].reshape(1, 8))
    for e in range(8):
        m["mgu%d" % e] = wgu_layout(inp["moe_w_gu"][0, e])
        m["mwd%d" % e] = np.ascontiguousarray(inp["moe_w_down"][0, e])
    return m


IN_SPECS = [("x", [T, D], F32), ("pos", [128, 16], I32)] + \
    [(n, [128, 128], F32) for n in ("ident", "tri", "upp", "ones", "tri64", "blk64", "nstrict64", "slt")] + [("iota512", [128, 512], F32)] + \
    [("ret_eqk", [128, 512], F32), ("ret_ee", [128, 256], F32), ("ret_ds", [128, 2], F32), ("inv_freq", [32], F32),
     ("w_in0", [1024, 3088], F32), ("w_out0", [1024, 1024], F32), ("w_gate2", [16, 256], F32), ("b_gate", [1, 256], F32),
     ("norm0", [1024], F32)] + [("ln%d_%s" % (i, s), [1024], F32) for i in (1, 2, 3, 4) for s in "gb"] + \
    [("wgu0", [28, 128, 8, 256], F32), ("wd0", [3584, 1024], F32),
     ("wqkv_l", [24, 128, 8, 128], F32), ("wg_l", [1024, 1040], F32), ("convw_l", [128, 24, 4], F32),
     ("c_a_log", [8], F32), ("c_dt_bias", [8], F32), ("c_norm", [1024], F32), ("w_out1", [1024, 1024], F32),
     ("w_router", [1024, 8], F32), ("b_router", [1, 8], F32)] + \
    [("mgu%d" % e, [28, 128, 8, 256], F32) for e in range(8)] + [("mwd%d" % e, [3584, 1024], F32) for e in range(8)]


def build_program(phases=("l0", "ffn", "gdn", "moe"), need=None):
    nc = bass.Bass("TRN2", target_bir_lowering=False)
    dr = {}
    for (n, shp, dt) in IN_SPECS:
        if need is None or n in need:
            dr[n] = nc.dram_tensor(n, list(shp), dt, kind="ExternalInput").ap()
    out = nc.dram_tensor("out", [T, D], F32, kind="ExternalOutput").ap()
    scr = {}
    for n in ("qs", "ks", "vs"):
        scr[n] = B(nc.dram_tensor("scr_" + n, [16, 128, 8, 128], BF16).ap(), n)
    scr["gs"] = B(nc.dram_tensor("scr_gs", [16, 128, 1024], BF16).ap(), "gs")
    with contextlib.ExitStack() as es:
        arena = es.enter_context(nc.sbuf_tensor("arena", [128, ARENA], F32))
        banks = [es.enter_context(nc.psum_tensor("pb%d" % i, [128, 512], F32)) for i in range(8)]
        k = K(nc, arena, ARENA, [b[:, :] for b in banks])
        C = load_consts(k, dr)
        X = k.alloc("X", [16, 1024])
        for n in range(NT):
            k.dma("sp", X.sub((slice(None), n, slice(None)), n), dr["x"][n * 128:(n + 1) * 128, :])
        last = phases[-1]
        if "l0" in phases:
            phase_l0(k, dr, C, X)
        if "ffn" in phases:
            phase_ffn(k, dr, C, X, [(dr["wgu0"], dr["wd0"])], dr["ln2_g"], dr["ln2_b"],
                      out_dram=out if last == "ffn" else None)
        if "gdn" in phases:
            k.P.barrier()
            C1 = {}
            for n in ("tri64", "blk64", "nstrict64"):
                C1[n] = k.alloc(n, [128])
                k.dma("sp", C1[n], dr[n])
            scr["BG"] = k.alloc("BG", [16, 16])
            gdn_pass_a(k, dr, C, X, scr)
            gdn_pass_b(k, dr, C, C1, X, scr, dr["ln3_g"], dr["ln3_b"])
        if "moe" in phases:
            if SPARSE_MOE:
                phase_moe_sparse(k, dr, C, X, dr["ln4_g"], dr["ln4_b"], out)
            else:
                phase_ffn(k, dr, C, X, [(dr["mgu%d" % e], dr["mwd%d" % e]) for e in range(8)], dr["ln4_g"], dr["ln4_b"],
                          out_dram=out, router=(dr["w_router"], dr["b_router"]))
        if last not in ("ffn", "moe"):
            for n in range(NT):
                k.dma("sp", out[n * 128:(n + 1) * 128, :], X.sub((slice(None), n, slice(None)), n))
        print("ops", len(k.P.ops), "arena peak?", k.off, flush=True)
        k.P.emit()
    return nc


def phase_moe_sparse(k, dr, C, X, ln_g, ln_b, out_dram):
    BLK = 512
    k.P.barrier()
    m0 = k.mark()
    Xb = k.alloc("Xb", [16, 1024], BF16)
    M = k.alloc("M", [16, 8])
    CB = k.alloc("CB", [16, 8])
    RK = k.alloc("RK", [16, 8])
    OFF = k.alloc("OFF", [17, 8])
    BLOCKS = [(0, 512), (512, 128), (640, 128), (768, 256), (1024, 512), (1536, 512)]
    NB = len(BLOCKS)
    Ff = k.alloc("Ff", [NB, 8])
    Fi = k.alloc("Fi", [NB, 8], I32)
    iota = k.alloc("iota", [512])
    k.dma("sp", iota, dr["iota512"])
    slt = k.alloc("slt", [128])
    k.dma("sp", slt, dr["slt"])
    m1 = k.mark()
    Wr = k.alloc("Wr", [8, 8])
    k.dma("sp", Wr, dr["w_router"].rearrange("(c p) e -> p c e", p=128))
    br = k.alloc("br", [8])
    k.dma("sp", br[0:1, :], dr["b_router"])
    XT32 = k.alloc("XT32", [8, 128])
    rt = k.alloc("rt", [64])
    for n in range(NT):
        Xn = X.sub((slice(None), n, slice(None)), n)
        for g in range(2):
            ps = k.psum()
            for c in range(4):
                k.tr(ps[:, c * 128:(c + 1) * 128], Xn[:, (g * 4 + c) * 128:(g * 4 + c + 1) * 128], C["ident"])
            k.cp(XT32[:, g * 4:g * 4 + 4, :], ps.re("p (a b) -> p a b", b=128), eng="dve" if g else "act")
        ps = k.psum()
        for c in range(8):
            k.mm(ps[:, 0:8], XT32[:, c, :], Wr[:, c, :], start=(c == 0), stop=False)
        k.mm(ps[:, 0:8], C["ones"][0:1, :], br[0:1, :], start=False, stop=True)
        lg = rt[:, 0:8].k("lg")
        k.cp(lg, ps[:, 0:8])
        k.red(rt[:, 8:9].k("m1"), lg, op=ALU.max)
        k.ts(rt[:, 16:24].k("eq1"), lg, rt[:, 8:9].k("m1"), None, ALU.is_equal)
        k.stt(rt[:, 24:32].k("l2"), rt[:, 16:24].k("eq1"), -1e30, lg, ALU.mult, ALU.add)
        k.red(rt[:, 9:10].k("m2"), rt[:, 24:32].k("l2"), op=ALU.max)
        k.ts(rt[:, 32:40].k("eq2"), rt[:, 24:32].k("l2"), rt[:, 9:10].k("m2"), None, ALU.is_equal)
        k.tt(rt[:, 10:11].k("d"), rt[:, 9:10].k("m2"), rt[:, 8:9].k("m1"), ALU.subtract)
        k.act(rt[:, 11:12].k("e"), rt[:, 10:11].k("d"), AF.Exp)
        k.ts(rt[:, 12:13].k("den"), rt[:, 11:12].k("e"), 1.0, None, ALU.add)
        k.recip(rt[:, 13:14].k("g1"), rt[:, 12:13].k("den"))
        k.tt(rt[:, 14:15].k("g2"), rt[:, 11:12].k("e"), rt[:, 13:14].k("g1"), ALU.mult)
        cbn = CB[:, n, :].k(n)
        k.ts(cbn, rt[:, 16:24].k("eq1"), rt[:, 13:14].k("g1"), None, ALU.mult)
        k.stt(cbn, rt[:, 32:40].k("eq2"), rt[:, 14:15].k("g2"), cbn, ALU.mult, ALU.add)
        k.tt(M[:, n, :].k(n), rt[:, 16:24].k("eq1"), rt[:, 32:40].k("eq2"), ALU.add)
        k.cp(Xb[:, n, :].k(n), Xn, eng="act")
        k.act(Xn, Xn, AF.Identity, scale=ALPHA)
    Mf = M.re("p a b -> p (a b)")
    ps = k.psum()
    k.mm(ps[:, 0:128], slt, Mf)
    k.mm(ps[:, 128:256], C["ones"], Mf)
    TOT = k.alloc("TOT", [16, 8])
    k.cp(RK.re("p a b -> p (a b)"), ps[:, 0:128])
    k.cp(TOT.re("p a b -> p (a b)"), ps[:, 128:256])
    k.memset(OFF[:, 0, :].k(0), 0.0)
    for t in range(16):
        k.tt(OFF[:, t + 1, :].k(t + 1), OFF[:, t, :].k(t), TOT[:, t, :], ALU.add)
    k.tt(RK, RK, OFF[:, 0:16, :], ALU.add)
    for j, (boff, bw) in enumerate(BLOCKS):
        k.ts(Ff[:, j, :], OFF[:, 16, :].k(16), float(boff), None, ALU.is_gt)
    k.cp(Fi, Ff)
    k.P.barrier()
    k.reset(m1)
    Sel = k.alloc("Sel", [16, 512], BF16)
    SelT = k.alloc("SelT", [4, 8, 128], BF16)
    XTe = k.alloc("XTe", [8, 512], BF16)
    actb = k.alloc("actb", [28, 512], BF16)
    Ye = k.alloc("Ye", [4, 1024], BF16)
    NW = 3
    wgu = [k.alloc("wgu%d" % i, [8, 256], BF16) for i in range(NW)]
    ND = 4
    wdb = [k.alloc("wd%d" % i, [1024], BF16) for i in range(ND)]
    sg = [k.alloc("sg%d" % i, [512]) for i in range(2)]
    rj = k.alloc("rj", [16])
    identb = k.alloc("identb", [128], BF16)
    k.cp(identb, C["ident"])
    wi = di = si = 0
    for e in range(8):
        wgu_l = dr["mgu%d" % e]
        wd = dr["mwd%d" % e]
        for j, (boff, W) in enumerate(BLOCKS):
            NS = W // 128
            k.P.begin_region(Fi.ap[0:1, j, e:e + 1], Fi.key)
            k.ts(rj, RK[:, :, e], -float(boff), None, ALU.add)
            for t in range(16):
                k.ts(Sel[:, t, 0:W].k(t), iota[:, 0:W], rj[:, t:t + 1], M[:, t, e:e + 1], ALU.is_equal, ALU.mult)
            for c in range(8):
                ps = k.psum()
                for t in range(16):
                    k.mm(ps[:, 0:W], Xb[:, t, c * 128:(c + 1) * 128], Sel[:, t, 0:W].k(t), start=(t == 0), stop=(t == 15))
                k.cp(XTe[:, c, 0:W].k(c), ps[:, 0:W], eng="act" if c % 2 else "dve")
            for f in range(28):
                wb = wgu[wi % NW]
                wi += 1
                k.dma("pool", wb, wgu_l[f])
                psG = k.psum()
                psU = k.psum()
                for c in range(8):
                    k.mm(psG[:, 0:W], wb[:, c, 0:128], XTe[:, c, 0:W].k(c), start=(c == 0), stop=(c == 7))
                for c in range(8):
                    k.mm(psU[:, 0:W], wb[:, c, 128:256], XTe[:, c, 0:W].k(c), start=(c == 0), stop=(c == 7))
                s = sg[si % 2]
                si += 1
                k.act(s[:, 0:W], psG[:, 0:W], AF.Silu)
                k.tt(actb[:, f, 0:W].k(f), s[:, 0:W], psU[:, 0:W], ALU.mult)
            pss = [k.psum() for _ in range(2 * NS)]
            for f in range(28):
                db = wdb[di % ND]
                di += 1
                k.dma("pool", db, wd[f * 128:(f + 1) * 128, :])
                for s4 in range(NS):
                    for hh in range(2):
                        k.mm(pss[s4 * 2 + hh], actb[:, f, s4 * 128:(s4 + 1) * 128].k(f), db[:, hh * 512:(hh + 1) * 512],
                             start=(f == 0), stop=(f == 27))
            for s4 in range(NS):
                for hh in range(2):
                    k.cp(Ye[:, s4, hh * 512:(hh + 1) * 512].k(s4, hh), pss[s4 * 2 + hh], eng="act" if hh else "dve")
            for th in range(2):
                for s4 in range(NS):
                    psT = k.psum(BF16)
                    for t8 in range(8):
                        t = th * 8 + t8
                        k.tr(psT[:, t8 * 128:(t8 + 1) * 128], Sel[:, t, s4 * 128:(s4 + 1) * 128].k(t), identb)
                    k.cp(SelT[:, s4, :, :].k(s4), psT.re("p (a b) -> p a b", b=128), eng="act" if s4 % 2 else "dve")
                for t8 in range(8):
                    t = th * 8 + t8
                    Xn = X.sub((slice(None), t, slice(None)), t)
                    for hh in range(2):
                        ps = k.psum()
                        for s4 in range(NS):
                            k.mm(ps, SelT[:, s4, t8, :].k(s4), Ye[:, s4, hh * 512:(hh + 1) * 512].k(s4, hh),
                                 start=(s4 == 0), stop=(s4 == NS - 1))
                        xs = Xn[:, hh * 512:(hh + 1) * 512]
                        k.stt(xs, ps, CB[:, t, e:e + 1].k(t), xs, ALU.mult, ALU.add)
            k.P.end_region()
    k.P.barrier()
    k.reset(m1)
    g_bc = k.alloc("ln_g", [1024])
    b_bc = k.alloc("ln_b", [1024])
    k.dma("sp", g_bc, ln_g.partition_broadcast(128))
    k.dma("sp", b_bc, ln_b.partition_broadcast(128))
    junk = k.alloc("junk", [1024])
    lnst = k.alloc("lnst", [8])
    for n in range(NT):
        Xn = X.sub((slice(None), n, slice(None)), n)
        ln_tile(k, Xn, g_bc, b_bc, Xn, lnst, junk)
        k.dma("sp", out_dram[n * 128:(n + 1) * 128, :], Xn)
    k.reset(m0)


_NC_CACHE = {}


def kernel(**inputs):
    inp = {k_: np.asarray(v) for k_, v in inputs.items()}
    shared = shared_inputs(inp)
    if "nc" not in _NC_CACHE:
        _NC_CACHE["nc"] = build_program(("l0", "ffn", "gdn", "moe"))
    nc = _NC_CACHE["nc"]
    in_maps = []
    for b in range(8):
        m = dict(shared)
        m["x"] = np.ascontiguousarray(inp["x"][b], dtype=np.float32)
        m["pos"] = np.ascontiguousarray(inp["positions"][b].astype(np.int32).reshape(16, 128).T)
        in_maps.append(m)
    res = run_bass_kernel_spmd(nc, in_maps, core_ids=list(range(8)))
    return np.stack([np.asarray(r["out"], dtype=np.float32) for r in res.results], axis=0)
```

```python
import contextlib
from concourse.bass_utils import run_bass_kernel_spmd
import numpy as np
import concourse.bass as bass
import concourse.mybir as mybir

F32 = mybir.dt.float32
BF16 = mybir.dt.bfloat16
I32 = mybir.dt.int32
AF = mybir.ActivationFunctionType
ALU = mybir.AluOpType
AX = mybir.AxisListType

ENGS = ("pe", "act", "dve", "pool", "sp")
N_DMA_SLOTS = 24


class _Op:
    __slots__ = ("eng", "fn", "reads", "writes", "dma", "deps", "signal", "idx",
                 "chan", "val", "slot", "region")

    def __init__(self, eng, fn, reads, writes, dma):
        self.eng = eng
        self.fn = fn
        self.reads = reads
        self.writes = writes
        self.dma = dma
        self.deps = ()
        self.signal = dma
        self.chan = None
        self.val = 0
        self.slot = None


def _conflict(a, b):
    n = min(len(a), len(b))
    return a[:n] == b[:n]


class Prog:
    def __init__(self, nc):
        self.nc = nc
        self.ops = []
        self.state = {}
        self.pending = {e: set() for e in ENGS}
        self.bar_start = 0
        self.cur_region = None
        self.regions = {}

    def barrier(self):
        deps = set()
        last = {}
        for o in self.ops[self.bar_start:]:
            if o.dma:
                deps.add(o.idx)
            else:
                last[o.eng] = o.idx
        deps |= set(last.values())
        for e in ENGS:
            self.pending[e] |= deps
        self.bar_start = len(self.ops)

    def op(self, eng, fn, reads=(), writes=(), dma=False):
        rk = tuple(self._k(r) for r in reads)
        wk = tuple(self._k(w) for w in writes)
        wk = wk + tuple(r for r in rk if r[0] == "ps" and r not in wk)
        o = _Op(eng, fn, rk, wk, dma)
        o.region = self.cur_region
        o.idx = len(self.ops)
        deps = set()
        for k in o.reads:
            grp = self.state.get(k[0])
            if grp:
                for k2, st in grp.items():
                    if _conflict(k, k2) and st[0] is not None:
                        deps.add(st[0])
        for k in o.writes:
            grp = self.state.setdefault(k[0], {})
            dead = []
            for k2, st in grp.items():
                if _conflict(k, k2):
                    if st[0] is not None:
                        deps.add(st[0])
                    deps.update(st[1])
                    if len(k2) >= len(k):
                        dead.append(k2)
            for k2 in dead:
                del grp[k2]
        for k in o.writes:
            self.state[k[0]][k] = [o.idx, []]
        for k in o.reads:
            grp = self.state.setdefault(k[0], {})
            if k in grp:
                grp[k][1].append(o.idx)
            else:
                w = None
                for k2, st in grp.items():
                    if _conflict(k, k2) and st[0] is not None:
                        w = st[0] if w is None else max(w, st[0])
                grp[k] = [w, [o.idx]]
        if self.pending[eng]:
            deps |= self.pending[eng]
            self.pending[eng] = set()
        deps.discard(o.idx)
        real = []
        best = {}
        for d in deps:
            od = self.ops[d]
            if o.eng == "pe" and od.eng == "pe" and not od.dma and not o.dma:
                continue
            if od.dma:
                real.append(d)
            elif d > best.get(od.eng, -1):
                best[od.eng] = d
        real.extend(best.values())
        for d in real:
            self.ops[d].signal = True
        o.deps = tuple(sorted(real))
        self.ops.append(o)
        return o

    def begin_region(self, flag_ap, flag_key):
        rid = len(self.regions) + 1
        fdep = None
        grp = self.state.get(flag_key[0])
        if grp:
            for k2, st in grp.items():
                if _conflict(flag_key, k2) and st[0] is not None:
                    fdep = st[0] if fdep is None else max(fdep, st[0])
        if fdep is not None:
            self.ops[fdep].signal = True
        self.regions[rid] = (flag_ap, fdep)
        self.cur_region = rid

    def end_region(self):
        self.cur_region = None

    @staticmethod
    def _k(k):
        return k if isinstance(k, tuple) else (k,)

    def emit(self, extra_ctx=()):
        nc = self.nc
        import contextlib
        with contextlib.ExitStack() as es:
            sems = {e: es.enter_context(nc.semaphore("s_" + e)) for e in ENGS}
            dsem = {}
            for q in ("sp", "act", "pool"):
                dsem[q] = [es.enter_context(nc.semaphore("d_%s_%d" % (q, i))) for i in range(N_DMA_SLOTS)]
            cnt = {e: 0 for e in ENGS}
            dcnt = {q: 0 for q in dsem}
            slot_uses = {q: [0] * N_DMA_SLOTS for q in dsem}
            for o in self.ops:
                if o.dma:
                    q = o.eng
                    s = dcnt[q] % N_DMA_SLOTS
                    dcnt[q] += 1
                    slot_uses[q][s] += 1
                    o.slot = s
                    o.chan = ("d", q, s)
                    o.val = 16 * slot_uses[q][s]
                elif o.signal:
                    cnt[o.eng] += 1
                    o.chan = ("c", o.eng)
                    o.val = cnt[o.eng]
            self.n_sig = dict(cnt)
            per = {e: [o for o in self.ops if o.eng == e] for e in ENGS}

            def semof(chan):
                if chan[0] == "c":
                    return sems[chan[1]]
                return dsem[chan[1]][chan[2]]

            ETYPE = {"pe": mybir.EngineType.PE, "act": mybir.EngineType.Activation, "dve": mybir.EngineType.DVE,
                     "pool": mybir.EngineType.Pool, "sp": mybir.EngineType.SP}
            flagregs = nc.alloc_registers("rflag", engines=list(ETYPE.values())) if self.regions else None

            def emit_one(o, eng, waited):
                need = {}
                for d in o.deps:
                    od = self.ops[d]
                    if od.val > need.get(od.chan, 0):
                        need[od.chan] = od.val
                if o.dma and o.val > 16:
                    ch = o.chan
                    if o.val - 16 > need.get(ch, 0):
                        need[ch] = o.val - 16
                for ch, v in need.items():
                    if waited.get(ch, 0) >= v:
                        continue
                    eng.wait_ge(semof(ch), v)
                    waited[ch] = v
                ins = o.fn(eng)
                if o.chan is not None:
                    ins.then_inc(semof(o.chan), 16 if o.dma else 1)

            def run(e, eng):
                waited = {}
                ops = per[e]
                i = 0
                while i < len(ops):
                    o = ops[i]
                    if o.region is None:
                        emit_one(o, eng, waited)
                        i += 1
                        continue
                    j = i
                    while j < len(ops) and ops[j].region == o.region:
                        j += 1
                    grp = ops[i:j]
                    rid = o.region
                    flag_ap, fdep = self.regions[rid]
                    if fdep is not None:
                        od = self.ops[fdep]
                        if waited.get(od.chan, 0) < od.val:
                            eng.wait_ge(semof(od.chan), od.val)
                            waited[od.chan] = od.val
                    reg = flagregs[ETYPE[e]]
                    eng.reg_load(reg, flag_ap)
                    snap = dict(waited)
                    with eng.If_ne(reg, 0):
                        for g in grp:
                            emit_one(g, eng, waited)
                    comp = {}
                    first = {}
                    for g in grp:
                        if g.chan is not None:
                            inc = 16 if g.dma else 1
                            comp[g.chan] = comp.get(g.chan, 0) + inc
                            first.setdefault(g.chan, g.val - inc)
                    if comp:
                        with eng.Else():
                            for ch, c in comp.items():
                                if first[ch] > 0:
                                    eng.wait_ge(semof(ch), first[ch])
                                eng.sem_inc(semof(ch), c)
                    waited.clear()
                    waited.update(snap)
                    i = j
            last = {}
            for o in self.ops:
                if o.dma:
                    last[o.chan] = max(last.get(o.chan, 0), o.val)

            with nc.Block() as block:
                @block.tensor
                def _(eng):
                    run("pe", eng)

                @block.scalar
                def _(eng):
                    run("act", eng)

                @block.vector
                def _(eng):
                    run("dve", eng)

                @block.gpsimd
                def _(eng):
                    run("pool", eng)

                @block.sync
                def _(eng):
                    run("sp", eng)
                    for ch, v in last.items():
                        eng.wait_ge(semof(ch), v)


T = 2048
NT = 16
D = 1024
ALPHA = 4.0 ** 0.25
EPS = 1e-5


class B:
    __slots__ = ("ap", "key")

    def __init__(self, ap, key):
        self.ap = ap
        self.key = key if isinstance(key, tuple) else (key,)

    def __getitem__(self, idx):
        return B(self.ap[idx], self.key)

    def sub(self, idx, *k):
        return B(self.ap[idx], self.key + tuple(k))

    def k(self, *k):
        return B(self.ap, self.key + tuple(k))

    def re(self, pat, **kw):
        return B(self.ap.rearrange(pat, **kw), self.key)

    def bc(self, shape, axis):
        return B(self.ap.unsqueeze(axis).to_broadcast(shape), self.key)


def _keys(*bs):
    return [b.key for b in bs if isinstance(b, B)]


def _a(x):
    return x.ap if isinstance(x, B) else x


class K:
    def __init__(self, nc, arena, arena_cols, psum_banks):
        self.nc = nc
        self.P = Prog(nc)
        self.arena = arena
        self.cols = arena_cols
        self.off = 0
        self.banks = psum_banks
        self.nbank = 0
        self.uid = 0
        self.defer = None
        self.bank_range = (0, len(psum_banks))
        self.bank_ctr = {}

    def _op(self, *a, **kw):
        if self.defer is not None:
            self.defer.append((a, kw))
        else:
            self.P.op(*a, **kw)

    def record(self, fn, banks=None):
        old = self.defer
        oldb = self.bank_range
        self.defer = []
        if banks is not None:
            self.bank_range = banks
        fn()
        ops = self.defer
        self.defer = old
        self.bank_range = oldb
        return ops

    @staticmethod
    def merge_lists(a, b):
        out = []
        na, nb = len(a), len(b)
        ia = ib = 0
        while ia < na or ib < nb:
            if ib >= nb or (ia < na and ia * nb <= ib * na):
                out.append(a[ia]); ia += 1
            else:
                out.append(b[ib]); ib += 1
        return out

    def issue_merged(self, a, b):
        na, nb = len(a), len(b)
        ia = ib = 0
        while ia < na or ib < nb:
            if ib >= nb or (ia < na and ia * nb <= ib * na):
                x = a[ia]; ia += 1
            else:
                x = b[ib]; ib += 1
            self._op(*x[0], **x[1])

    def alloc(self, name, free_shape, dt=F32):
        n = int(np.prod(free_shape))
        words = n if dt == F32 or dt == I32 else (n + 1) // 2
        assert self.off + words <= self.cols, (name, self.off, words, self.cols)
        ap = self.arena[:, self.off:self.off + words]
        self.off += words
        if dt == BF16:
            ap = ap.bitcast(BF16)
            if ap.shape[-1] != n:
                ap = ap[:, 0:n]
        elif dt == I32:
            ap = ap.bitcast(I32)
        if len(free_shape) == 2:
            ap = ap.rearrange("p (a b) -> p a b", b=free_shape[1])
        elif len(free_shape) == 3:
            ap = ap.rearrange("p (a b c) -> p a b c", b=free_shape[1], c=free_shape[2])
        return B(ap, name)

    def mark(self):
        return self.off

    def reset(self, m):
        self.off = m

    def psum(self, dt=F32):
        lo, hi = self.bank_range
        b = lo + self.bank_ctr.setdefault((lo, hi), 0) % (hi - lo)
        self.bank_ctr[(lo, hi)] += 1
        ap = self.banks[b]
        if dt == BF16:
            ap = ap.bitcast(BF16)
        return B(ap, ("ps", b))

    def mm(self, out, lhsT, rhs, start=True, stop=True):
        self._op("pe", lambda e: e.matmul(out.ap, lhsT.ap, rhs.ap, start=start, stop=stop),
                  reads=_keys(lhsT, rhs), writes=_keys(out))

    def tr(self, out, in_, ident):
        self._op("pe", lambda e: e.transpose(out.ap, in_.ap, ident.ap),
                  reads=_keys(in_, ident), writes=_keys(out))

    def act(self, out, in_, func, bias=None, scale=1.0, accum=None):
        kw = {}
        if bias is not None:
            kw["bias"] = _a(bias)
        if accum is not None:
            kw["accum_out"] = accum.ap
        self._op("act", lambda e: e.activation(out.ap, in_.ap, func, scale=_a(scale), **kw),
                  reads=_keys(in_, bias, scale), writes=_keys(out, accum))

    def tt(self, out, a, b, op, eng="dve"):
        self._op(eng, lambda e: e.tensor_tensor(out.ap, a.ap, b.ap, op), reads=_keys(a, b), writes=_keys(out))

    def ts(self, out, a, s1, s2, op0, op1=None, eng="dve"):
        if op1 is None:
            f = lambda e: e.tensor_scalar(out.ap, a.ap, _a(s1), None, op0)
        else:
            f = lambda e: e.tensor_scalar(out.ap, a.ap, _a(s1), _a(s2), op0, op1)
        self._op(eng, f, reads=_keys(a, s1, s2), writes=_keys(out))

    def stt(self, out, a, s, b, op0, op1, eng="dve"):
        self._op(eng, lambda e: e.scalar_tensor_tensor(out.ap, a.ap, _a(s), b.ap, op0, op1),
                  reads=_keys(a, s, b), writes=_keys(out))

    def cp(self, out, in_, eng="dve"):
        if eng == "act":
            self._op("act", lambda e: e.copy(out.ap, in_.ap), reads=_keys(in_), writes=_keys(out))
        else:
            self._op(eng, lambda e: e.tensor_copy(out.ap, in_.ap), reads=_keys(in_), writes=_keys(out))

    def red(self, out, in_, op=None, eng="dve"):
        op = op or ALU.add
        self._op(eng, lambda e: e.tensor_reduce(out.ap, in_.ap, AX.X, op), reads=_keys(in_), writes=_keys(out))

    def recip(self, out, in_):
        self._op("dve", lambda e: e.reciprocal(out.ap, in_.ap), reads=_keys(in_), writes=_keys(out))

    def memset(self, out, val, eng="dve"):
        self._op(eng, lambda e: e.memset(out.ap, val), writes=_keys(out))

    def dma(self, q, out, in_):
        rk = _keys(in_)
        wk = _keys(out)
        self._op(q, lambda e: e.dma_start(out=_a(out), in_=_a(in_)), reads=rk, writes=wk, dma=True)


def host_consts():
    c = {}
    c["ident"] = np.eye(128, dtype=np.float32)
    j = np.arange(128)[:, None]
    i = np.arange(128)[None, :]
    c["tri"] = (j <= i).astype(np.float32)
    c["upp"] = (j > i).astype(np.float32)
    c["ones"] = np.ones((128, 128), np.float32)
    lg = np.log(1.0 - 2.0 ** (-5.0 - np.arange(4, dtype=np.float64)))
    p = np.arange(128, dtype=np.float64)[:, None]
    eq = 0.125 * np.exp(lg[None, :] * (p + 1.0))
    ek = np.exp(-lg[None, :] * (p + 1.0))
    ee = np.exp(lg[None, :] * (127.0 - p))
    rep = lambda a: np.repeat(a, 64, axis=1)
    c["ret_eqk"] = np.concatenate([rep(eq), rep(ek)], 1).astype(np.float32)
    c["ret_ee"] = rep(ee).astype(np.float32)
    dS = np.exp(lg * 128.0)
    ds = np.zeros((128, 2), np.float64)
    for h in range(4):
        ds[(h % 2) * 64:(h % 2) * 64 + 64, h // 2] = dS[h]
    c["ret_ds"] = ds.astype(np.float32)
    c["inv_freq"] = (10000.0 ** (-np.arange(0, 64, 2, dtype=np.float32) / 64)).astype(np.float32)[None, :]
    return c


L0_PERM = None


def l0_perm():
    r = lambda a, b: list(range(a, b))
    return np.array(r(0, 256) + r(256, 512) + r(1552, 1808) + r(1808, 2064) + r(512, 1024) + r(2064, 2576)
                    + r(1040, 1552) + r(2576, 3088) + r(1024, 1040))


def layer_norm_tile(k, Y, g_bc, b_bc, out, scr):
    st = scr["st"]
    junk = scr["junk"]
    k.red(st[:, 0:1].k(0), Y)
    k.tt(junk, Y, Y, ALU.mult, eng="pool")
    k.red(st[:, 1:2].k(1), junk)
    k.ts(st[:, 2:3].k(2), st[:, 0:1].k(0), 1.0 / D, None, ALU.mult)
    k.tt(st[:, 3:4].k(3), st[:, 2:3].k(2), st[:, 2:3].k(2), ALU.mult)
    k.stt(st[:, 4:5].k(4), st[:, 1:2].k(1), 1.0 / D, st[:, 3:4].k(3), ALU.mult, ALU.subtract)
    k.act(st[:, 5:6].k(5), st[:, 4:5].k(4), AF.Sqrt, bias=scr["eps"], scale=1.0)
    k.recip(st[:, 6:7].k(6), st[:, 5:6].k(5))
    k.stt(st[:, 7:8].k(7), st[:, 2:3].k(2), -1.0, st[:, 6:7].k(6), ALU.mult, ALU.mult)
    k.act(junk, Y, AF.Identity, bias=st[:, 7:8].k(7), scale=st[:, 6:7].k(6))
    k.tt(junk, junk, g_bc, ALU.mult, eng="pool")
    k.tt(out, junk, b_bc, ALU.add)


def ln_tile(k, Y, g_bc, b_bc, out, st, junk):
    k.red(st[:, 0:1].k(0), Y)
    k.act(junk, Y, AF.Square)
    k.red(st[:, 1:2].k(1), junk)
    k.ts(st[:, 2:3].k(2), st[:, 0:1].k(0), 1.0 / D, None, ALU.mult)
    k.tt(st[:, 3:4].k(3), st[:, 2:3].k(2), st[:, 2:3].k(2), ALU.mult)
    k.stt(st[:, 4:5].k(4), st[:, 1:2].k(1), 1.0 / D, st[:, 3:4].k(3), ALU.mult, ALU.subtract)
    k.act(st[:, 5:6].k(5), st[:, 4:5].k(4), AF.Sqrt, bias=EPS, scale=1.0)
    k.recip(st[:, 6:7].k(6), st[:, 5:6].k(5))
    k.stt(st[:, 7:8].k(7), st[:, 2:3].k(2), -1.0, st[:, 6:7].k(6), ALU.mult, ALU.mult)
    k.act(junk, Y, AF.Identity, bias=st[:, 7:8].k(7), scale=st[:, 6:7].k(6))
    k.tt(junk, junk, g_bc, ALU.mult)
    k.tt(out, junk, b_bc, ALU.add)


def load_consts(k, dr):
    C = {}
    for n in ("ident", "tri", "upp", "ones"):
        C[n] = k.alloc(n, [128])
        k.dma("sp", C[n], dr[n])
    return C


def transpose_tile(k, C, src, dst_bf, nchunk=8, dst32=None):
    for g in range(0, nchunk, 4):
        ps = k.psum()
        n = min(4, nchunk - g)
        for c in range(n):
            k.tr(ps[:, c * 128:(c + 1) * 128], src[:, (g + c) * 128:(g + c + 1) * 128], C["ident"])
        psv = ps[:, 0:n * 128].re("p (a b) -> p a b", b=128)
        if (g // 4) % 2 == 0:
            k.cp(dst_bf[:, g:g + n, :], psv, eng="act")
        else:
            k.cp(dst_bf[:, g:g + n, :], psv, eng="dve")
        if dst32 is not None:
            k.cp(dst32[:, g:g + n, :], psv, eng="dve")


def phase_l0(k, dr, C, X):
    m0 = k.mark()
    Win = k.alloc("Win", [8, 3088], BF16)
    wv = dr["w_in0"].rearrange("(c p) n -> p c n", p=128)
    for c in range(8):
        k.dma("pool", Win.sub((slice(None), c, slice(None)), c), wv[:, c, :])
    Wout = k.alloc("Wout", [8, 1024], BF16)
    wo = dr["w_out0"].rearrange("(c p) n -> p c n", p=128)
    for c in range(0, 8, 4):
        k.dma("pool", Wout.sub((slice(None), slice(c, c + 4), slice(None)), c), wo[:, c:c + 4, :])
    wg2 = k.alloc("wg2", [256])
    k.dma("sp", wg2[0:16, :], dr["w_gate2"])
    bg = k.alloc("bg", [256])
    k.dma("sp", bg[0:1, :], dr["b_gate"])
    normw = k.alloc("normw", [1024])
    k.dma("sp", normw, dr["norm0"].partition_broadcast(128))
    g_bc = k.alloc("ln_g", [1024])
    b_bc = k.alloc("ln_b", [1024])
    k.dma("sp", g_bc, dr["ln1_g"].partition_broadcast(128))
    k.dma("sp", b_bc, dr["ln1_b"].partition_broadcast(128))
    Etab = k.alloc("Etab", [1024])
    k.dma("sp", Etab[:, 512:1024], dr["ret_eqk"])
    Ee = k.alloc("Ee", [2, 256])
    k.dma("sp", Ee[:, 1, :], dr["ret_ee"])
    dS_2 = [k.alloc("dS%d" % i_, [4]) for i_ in range(2)]
    for i_ in range(2):
        k.dma("sp", dS_2[i_][:, 2:4], dr["ret_ds"])
    invf = k.alloc("invf", [32])
    k.dma("sp", invf, dr["inv_freq"].partition_broadcast(128))
    posi = k.alloc("posi", [16], I32)
    k.dma("sp", posi, dr["pos"])
    posf = k.alloc("posf", [16])
    k.cp(posf, posi)
    cosT = k.alloc("cosT", [16, 32])
    sinT = k.alloc("sinT", [16, 32])
    ang = k.alloc("ang", [16, 32])
    k.tt(ang, invf.bc([128, 16, 32], 1), posf.bc([128, 16, 32], 2), ALU.mult)
    PI = float(np.pi)
    rr = k.alloc("rr", [16, 32])
    qi = k.alloc("qi", [16, 32], I32)
    qf = k.alloc("qf", [16, 32])
    gg = k.alloc("gg", [16, 32])

    def sin_table(dst, shift):
        k.ts(rr, ang, 1.0 / (2 * PI), shift, ALU.mult, ALU.add)
        k.cp(qi, rr)
        k.cp(qf, qi)
        k.tt(rr, rr, qf, ALU.subtract)
        k.ts(gg, rr, 0.5, None, ALU.is_gt)
        k.tt(rr, rr, gg, ALU.subtract)
        k.ts(gg, rr, -0.5, None, ALU.is_lt)
        k.tt(rr, rr, gg, ALU.add)
        k.act(dst, rr, AF.Sin, scale=2 * PI)

    sin_table(sinT, 0.0)
    sin_table(cosT, 0.25)

    XTn = k.alloc("XTn", [8, 128], BF16)
    qk32 = k.alloc("qk32", [1024])
    v8b_2 = [k.alloc("v8b%d" % i_, [1024], BF16) for i_ in range(2)]
    Gb_2 = [k.alloc("Gb%d" % i_, [1024], BF16) for i_ in range(2)]
    ga32 = k.alloc("ga32", [16])
    gaT = k.alloc("gaT", [128])
    l32 = k.alloc("l32", [256])
    kend_2 = [k.alloc("kend%d" % i_, [2, 256], BF16) for i_ in range(2)]
    qkT_2 = [k.alloc("qkT%d" % i_, [8, 128], BF16) for i_ in range(2)]
    attm = [k.alloc("attm%d" % i, [4, 128], BF16) for i in range(2)]
    o32 = k.alloc("o32", [1024])
    junk = k.alloc("junk", [1024])
    junkb = k.alloc("junkb", [1024])
    OT = k.alloc("OT", [8, 128], BF16)
    S32 = k.alloc("S32", [4, 128])
    Sb = k.alloc("Sb", [4, 128], BF16)
    st = k.alloc("st", [64])
    lnst = k.alloc("lnst", [8])
    k.memset(S32, 0.0)
    k.memset(Sb, 0.0, eng="pool")
    k.memset(st, 0.0)
    cols = [(0, 512), (512, 512), (1024, 512), (1536, 512), (2048, 512), (2560, 512), (3072, 16)]

    def front(n):
        i = n % 2
        v8b, Gb, kend, qkT, dS = v8b_2[i], Gb_2[i], kend_2[i], qkT_2[i], dS_2[i]
        Xn = X.sub((slice(None), n, slice(None)), n)
        transpose_tile(k, C, Xn, XTn)
        for gi, (c0, w) in enumerate(cols):
            ps = k.psum()
            for c in range(8):
                k.mm(ps[:, 0:w], XTn[:, c, :], Win[:, c, c0:c0 + w].k(c), start=(c == 0), stop=(c == 7))
            if gi < 2:
                k.cp(qk32[:, c0:c0 + w], ps[:, 0:w], eng="dve")
            elif gi < 4:
                k.cp(v8b[:, c0 - 1024:c0 - 1024 + w], ps[:, 0:w], eng="act")
            elif gi < 6:
                k.act(junk[:, 0:512], ps[:, 0:w], AF.Silu)
                k.tt(Gb[:, c0 - 2048:c0 - 2048 + w], junk[:, 0:512], normw[:, c0 - 2048:c0 - 2048 + w], ALU.mult, eng="pool")
            else:
                k.cp(ga32, ps[:, 0:16], eng="dve")
        ps = k.psum()
        k.tr(ps[0:16, 0:128], ga32, C["ident"])
        k.cp(gaT[0:16, :], ps[0:16, 0:128], eng="dve")
        psz = k.psum()
        k.mm(psz[:, 0:256], gaT[0:16, :], wg2[0:16, :], start=True, stop=False)
        k.mm(psz[:, 0:256], C["ones"][0:1, :], bg[0:1, :], start=False, stop=True)
        k.act(l32, psz[:, 0:256], AF.Exp, scale=-1.0)
        k.act(l32, l32, AF.Ln, bias=1.0, scale=1.0)
        psb = k.psum()
        k.mm(psb[:, 0:256], C["tri"], l32)
        k.mm(psb[:, 256:512], C["upp"], l32)
        psc = k.psum()
        k.mm(psc[:, 0:1], l32[:, 0:128], C["ones"][:, 0:1])
        k.mm(psc[:, 1:2], l32[:, 128:256], C["ones"][:, 0:1])
        k.act(Etab[:, 0:256], psb[:, 0:256], AF.Exp, scale=-1.0 / 16.0)
        k.ts(Etab[:, 0:256], Etab[:, 0:256], 0.125, None, ALU.mult)
        k.act(Etab[:, 256:512], psb[:, 0:256], AF.Exp, scale=1.0 / 16.0)
        k.act(Ee[:, 0, :], psb[:, 256:512], AF.Exp, scale=-1.0 / 16.0)
        k.act(dS[:, 0:2], psc[:, 0:2], AF.Exp, scale=-1.0 / 16.0)
        rv = qk32[:, 512:1024].re("p (h t m) -> p h t m", t=2, m=32)
        x1 = rv[:, :, 0, :]
        x2 = rv[:, :, 1, :]
        cs = B(cosT.ap[:, n, :].unsqueeze(1).to_broadcast([128, 8, 32]), cosT.key)
        sn = B(sinT.ap[:, n, :].unsqueeze(1).to_broadcast([128, 8, 32]), sinT.key)
        jv = junk.re("p (a h m) -> p a h m", h=8, m=32)
        k.tt(jv[:, 0], x1, cs, ALU.mult)
        k.tt(jv[:, 1], x2, sn, ALU.mult)
        k.tt(jv[:, 2], x1, sn, ALU.mult, eng="pool")
        k.tt(jv[:, 3], x2, cs, ALU.mult, eng="pool")
        k.tt(x1, jv[:, 0], jv[:, 1], ALU.subtract)
        k.tt(x2, jv[:, 2], jv[:, 3], ALU.add)
        kv = qk32.re("p (a b) -> p a b", b=512)[:, :, 256:512]
        k.tt(kend, kv, Ee, ALU.mult)
        k.tt(qk32, qk32, Etab, ALU.mult)
        transpose_tile(k, C, qk32, qkT)

    def back(n):
        i = n % 2
        v8b, Gb, kend, qkT, dS = v8b_2[i], Gb_2[i], kend_2[i], qkT_2[i], dS_2[i]
        junk = junkb
        Xn = X.sub((slice(None), n, slice(None)), n)
        for hg in range(2):
            psA = [k.psum(), k.psum()]
            for h4 in range(4):
                qc = hg * 4 + h4 // 2
                kc = qc + 2
                r0 = (h4 % 2) * 64
                k.mm(psA[h4 % 2][:, (h4 // 2) * 128:(h4 // 2 + 1) * 128], qkT[r0:r0 + 64, kc, :], qkT[r0:r0 + 64, qc, :])
            am = attm[hg]
            amv = am.re("p (a b) c -> p a b c", b=2)
            for par in range(2):
                k.tt(amv[:, :, par, :], psA[par][:, 0:256].re("p (a b) -> p a b", b=128), C["tri"].bc([128, 2, 128], 1), ALU.mult)
            psO = [k.psum(), k.psum()]
            for h4 in range(4):
                h = hg * 4 + h4
                qc = hg * 4 + h4 // 2
                r0 = (h4 % 2) * 64
                pair = hg * 2 + h4 // 2
                po = psO[h4 % 2][:, (h4 // 2) * 128:(h4 // 2 + 1) * 128]
                k.mm(po, qkT[r0:r0 + 64, qc, :], Sb[r0:r0 + 64, pair, :], start=True, stop=False)
                k.mm(po, am[:, h4, :], v8b[:, h * 128:(h + 1) * 128], start=False, stop=True)
            ovh = o32[:, hg * 512:(hg + 1) * 512].re("p (a b c) -> p a b c", b=2, c=128)
            for par in range(2):
                k.cp(B(ovh.ap[:, :, par, :], ("o32", hg)), psO[par][:, 0:256].re("p (a b) -> p a b", b=128), eng="act")
            psD = k.psum()
            for h4 in range(4):
                h = hg * 4 + h4
                k.mm(psD[:, h4 * 128:(h4 + 1) * 128], kend[:, hg, (h4 // 2) * 128:(h4 // 2 + 1) * 128], v8b[:, h * 128:(h + 1) * 128])
            for h4 in range(4):
                r0 = (h4 % 2) * 64
                pair = hg * 2 + h4 // 2
                k.stt(S32[r0:r0 + 64, pair, :].k(hg), S32[r0:r0 + 64, pair, :].k(hg), dS[r0:r0 + 64, pair:pair + 1],
                      psD[r0:r0 + 64, h4 * 128:(h4 + 1) * 128], ALU.mult, ALU.add)
            k.cp(Sb[:, hg * 2:hg * 2 + 2, :].k(hg), S32[:, hg * 2:hg * 2 + 2, :].k(hg), eng="pool")
        ov = o32.re("p (h v) -> p h v", v=128)
        k.tt(junk, o32, o32, ALU.mult, eng="pool")
        k.red(st[:, 0:8].k("s2"), junk.re("p (h v) -> p h v", v=128))
        k.red(st[:, 12:16].k("s1"), ov[:, 4:8, :])
        k.ts(st[:, 16:24].k("mean"), st[:, 8:16].k("s1"), 1.0 / 128, None, ALU.mult)
        k.tt(st[:, 24:32].k("msq"), st[:, 16:24].k("mean"), st[:, 16:24].k("mean"), ALU.mult)
        k.stt(st[:, 32:40].k("var"), st[:, 0:8].k("s2"), 1.0 / 128, st[:, 24:32].k("msq"), ALU.mult, ALU.subtract)
        k.act(st[:, 32:40].k("var"), st[:, 32:40].k("var"), AF.Sqrt, bias=EPS, scale=1.0)
        k.recip(st[:, 40:48].k("rstd"), st[:, 32:40].k("var"))
        k.stt(st[:, 48:56].k("nb"), st[:, 16:24].k("mean"), -1.0, st[:, 40:48].k("rstd"), ALU.mult, ALU.mult)
        k.tt(ov, ov, st[:, 40:48].k("rstd").bc([128, 8, 128], 2), ALU.mult)
        k.tt(ov, ov, st[:, 48:56].k("nb").bc([128, 8, 128], 2), ALU.add)
        k.tt(o32, o32, Gb, ALU.mult)
        transpose_tile(k, C, o32, OT)
        for half in range(2):
            ps = k.psum()
            for c in range(8):
                k.mm(ps, OT[:, c, :], Wout[:, c, half * 512:(half + 1) * 512], start=(c == 0), stop=(c == 7))
            k.stt(Xn[:, half * 512:(half + 1) * 512], Xn[:, half * 512:(half + 1) * 512], ALPHA, ps, ALU.mult, ALU.add)
        ln_tile(k, Xn, g_bc, b_bc, Xn, lnst, junk)

    FB, BB = (0, 5), (5, 8)
    k.issue_merged(k.record(lambda: front(0), banks=FB), [])
    for n in range(NT):
        fr = k.record(lambda: front(n + 1), banks=FB) if n + 1 < NT else []
        bk = k.record(lambda: back(n), banks=BB)
        k.issue_merged(bk, fr)
    k.reset(m0)


def phase_ffn(k, dr, C, X, experts, ln_g, ln_b, out_dram=None, router=None):
    k.P.barrier()
    m0 = k.mark()
    g_bc = k.alloc("ln_g", [1024])
    b_bc = k.alloc("ln_b", [1024])
    k.dma("sp", g_bc, ln_g.partition_broadcast(128))
    k.dma("sp", b_bc, ln_b.partition_broadcast(128))
    XTh = k.alloc("XTh", [8, 1024], BF16)
    actb = k.alloc("actb", [28, 1024], BF16)
    NW = 3
    wgu = [k.alloc("wgu%d" % i, [8, 256], BF16) for i in range(NW)]
    ND = 6
    wdb = [k.alloc("wd%d" % i, [1024], BF16) for i in range(ND)]
    sg = [k.alloc("sg%d" % i, [512]) for i in range(2)]
    junk = k.alloc("junk", [1024])
    lnst = k.alloc("lnst", [8])
    wi = 0
    di = 0
    si = 0
    if router is not None:
        Wr = k.alloc("Wr", [8, 8])
        k.dma("sp", Wr, router[0].rearrange("(c p) e -> p c e", p=128))
        br = k.alloc("br", [8])
        k.dma("sp", br[0:1, :], router[1])
        CB = k.alloc("CB", [16, 8])
        XT32 = k.alloc("XT32", [8, 128])
        rt = k.alloc("rt", [64])
    work = []
    lns = []
    for half in range(2):
        k.defer = []
        for t in range(8):
            n = half * 8 + t
            Xn = X.sub((slice(None), n, slice(None)), n)
            if router is None:
                transpose_tile(k, C, Xn, B(XTh.ap[:, :, t * 128:(t + 1) * 128], ("XTh", t)))
            else:
                transpose_tile(k, C, Xn, B(XTh.ap[:, :, t * 128:(t + 1) * 128], ("XTh", t)), dst32=XT32)
                ps = k.psum()
                for c in range(8):
                    k.mm(ps[:, 0:8], XT32[:, c, :], Wr[:, c, :], start=(c == 0), stop=False)
                k.mm(ps[:, 0:8], C["ones"][0:1, :], br[0:1, :], start=False, stop=True)
                lg = rt[:, 0:8].k("lg")
                k.cp(lg, ps[:, 0:8])
                k.red(rt[:, 8:9].k("m1"), lg, op=ALU.max)
                k.ts(rt[:, 16:24].k("eq1"), lg, rt[:, 8:9].k("m1"), None, ALU.is_equal)
                k.stt(rt[:, 24:32].k("l2"), rt[:, 16:24].k("eq1"), -1e30, lg, ALU.mult, ALU.add)
                k.red(rt[:, 9:10].k("m2"), rt[:, 24:32].k("l2"), op=ALU.max)
                k.ts(rt[:, 32:40].k("eq2"), rt[:, 24:32].k("l2"), rt[:, 9:10].k("m2"), None, ALU.is_equal)
                k.tt(rt[:, 10:11].k("d"), rt[:, 9:10].k("m2"), rt[:, 8:9].k("m1"), ALU.subtract)
                k.act(rt[:, 11:12].k("e"), rt[:, 10:11].k("d"), AF.Exp)
                k.ts(rt[:, 12:13].k("den"), rt[:, 11:12].k("e"), 1.0, None, ALU.add)
                k.recip(rt[:, 13:14].k("g1"), rt[:, 12:13].k("den"))
                k.tt(rt[:, 14:15].k("g2"), rt[:, 11:12].k("e"), rt[:, 13:14].k("g1"), ALU.mult)
                cbn = CB[:, n, :].k(n)
                k.ts(cbn, rt[:, 16:24].k("eq1"), rt[:, 13:14].k("g1"), None, ALU.mult)
                k.stt(cbn, rt[:, 32:40].k("eq2"), rt[:, 14:15].k("g2"), cbn, ALU.mult, ALU.add)
                k.act(Xn, Xn, AF.Identity, scale=ALPHA)
        for ei, (wgu_l, wd) in enumerate(experts):
            comb = None if router is None else (CB, ei)
            for f in range(28):
                wb = wgu[wi % NW]
                wi += 1
                k.dma("pool", wb, wgu_l[f])
                for tb in range(2):
                    psG = k.psum()
                    psU = k.psum()
                    for c in range(8):
                        k.mm(psG, wb[:, c, 0:128], XTh[:, c, tb * 512:(tb + 1) * 512], start=(c == 0), stop=(c == 7))
                    for c in range(8):
                        k.mm(psU, wb[:, c, 128:256], XTh[:, c, tb * 512:(tb + 1) * 512], start=(c == 0), stop=(c == 7))
                    s = sg[si % 2]
                    si += 1
                    k.act(s, psG, AF.Silu)
                    k.tt(actb.sub((slice(None), f, slice(tb * 512, (tb + 1) * 512)), f, tb), s, psU, ALU.mult)
            for tg in range(2):
                pss = [k.psum() for _ in range(8)]
                for f in range(28):
                    db = wdb[di % ND]
                    di += 1
                    k.dma("pool", db, wd[f * 128:(f + 1) * 128, :])
                    for t4 in range(4):
                        t = tg * 4 + t4
                        for hh in range(2):
                            k.mm(pss[t4 * 2 + hh], actb[:, f, t * 128:(t + 1) * 128].k(f, t // 4),
                                 db[:, hh * 512:(hh + 1) * 512], start=(f == 0), stop=(f == 27))
                for t4 in range(4):
                    n = half * 8 + tg * 4 + t4
                    Xn = X.sub((slice(None), n, slice(None)), n)
                    for hh in range(2):
                        xs = Xn[:, hh * 512:(hh + 1) * 512]
                        if comb is None:
                            k.stt(xs, xs, ALPHA, pss[t4 * 2 + hh], ALU.mult, ALU.add)
                        else:
                            cb, e = comb
                            k.stt(xs, pss[t4 * 2 + hh], cb[:, n, e:e + 1].k(n), xs, ALU.mult, ALU.add)
        work.append(k.defer)
        k.defer = []
        for t in range(8):
            n = half * 8 + t
            Xn = X.sub((slice(None), n, slice(None)), n)
            ln_tile(k, Xn, g_bc, b_bc, Xn, lnst, junk)
            if out_dram is not None:
                k.dma("sp", out_dram[n * 128:(n + 1) * 128, :], Xn)
        lns.append(k.defer)
        k.defer = None
    k.issue_merged(work[0], [])
    k.issue_merged(work[1], lns[0])
    k.issue_merged(lns[1], [])
    k.reset(m0)


def host_consts_l1():
    c = {}
    p = np.arange(128)[:, None]
    f = np.arange(128)[None, :]
    same = (p // 64) == (f // 64)
    c["tri64"] = ((p <= f) & same).astype(np.float32)
    c["blk64"] = same.astype(np.float32)
    c["nstrict64"] = -((f < p) & same).astype(np.float32)
    c["slt"] = (p < f).astype(np.float32)
    c["iota512"] = np.tile(np.arange(512, dtype=np.float32)[None, :], (128, 1))
    return c


def gdn_pass_a(k, dr, C, X, scr):
    k.P.barrier()
    m0 = k.mark()
    Wg = k.alloc("Wg", [8, 1040], BF16)
    wv = dr["wg_l"].rearrange("(c p) n -> p c n", p=128)
    for c in range(0, 8, 4):
        k.dma("pool", Wg.sub((slice(None), slice(c, c + 4), slice(None)), c), wv[:, c:c + 4, :])
    cw = k.alloc("cw", [24, 4])
    k.dma("sp", cw, dr["convw_l"])
    cnb = k.alloc("cnb", [1024])
    k.dma("sp", cnb, dr["c_norm"].partition_broadcast(128))
    dtb = k.alloc("dtb", [8])
    k.dma("sp", dtb, dr["c_dt_bias"].partition_broadcast(128))
    nA = k.alloc("nA", [8])
    k.dma("sp", nA, dr["c_a_log"].partition_broadcast(128))
    k.act(nA, nA, AF.Exp)
    k.ts(nA, nA, -1.0, None, ALU.mult)
    XTb = k.alloc("XTb", [8, 512], BF16)
    NWB = 3
    wqb = [k.alloc("wq%d" % i, [8, 128], BF16) for i in range(NWB)]
    pre = [k.alloc("pre%d" % i, [515]) for i in range(2)]
    acc = [k.alloc("acc%d" % i, [512]) for i in range(3)]
    sbuf_s = [k.alloc("s%d" % i, [512]) for i in range(3)]
    sq = [k.alloc("sq%d" % i, [512]) for i in range(3)]
    rsb = [k.alloc("rs%d" % i, [512]) for i in range(3)]
    outb = [k.alloc("ob%d" % i, [512], BF16) for i in range(4)]
    carry = k.alloc("carry", [24, 3])
    k.memset(carry, 0.0)
    junk = k.alloc("junk", [1024])
    Gt = [k.alloc("Gt%d" % i, [1024], BF16) for i in range(2)]
    ab = [k.alloc("ab%d" % i, [16]) for i in range(2)]
    zt = k.alloc("zt", [8])
    BG = scr["BG"]
    it = 0
    for b in range(4):
        for t in range(4):
            n = 4 * b + t
            Xn = X.sub((slice(None), n, slice(None)), n)
            transpose_tile(k, C, Xn, B(XTb.ap[:, :, t * 128:(t + 1) * 128], ("XTb", t)))
        def head(ch, it):
            wb = wqb[it % NWB]
            pr = pre[it % 2]
            ac = acc[it % 3]
            if it < 2:
                k.dma("pool", wb, dr["wqkv_l"][ch])
            ps = k.psum()
            for c in range(8):
                k.mm(ps, wb[:, c, :], XTb[:, c, :], start=(c == 0), stop=(c == 7))
            if it + 2 < 96:
                k.dma("pool", wqb[(it + 2) % NWB], dr["wqkv_l"][(it + 2) % 24])
            k.cp(pr[:, 3:515], ps, eng="act")
            k.cp(pr[:, 0:3], carry[:, ch, :].k(ch), eng="pool")
            k.cp(carry[:, ch, :].k(ch), pr[:, 512:515], eng="pool")
            k.ts(ac, pr[:, 0:512], cw[:, ch, 0:1], None, ALU.mult)
            k.stt(ac, pr[:, 1:513], cw[:, ch, 1:2], ac, ALU.mult, ALU.add)
            k.stt(ac, pr[:, 2:514], cw[:, ch, 2:3], ac, ALU.mult, ALU.add)
            k.stt(ac, pr[:, 3:515], cw[:, ch, 3:4], ac, ALU.mult, ALU.add)

        def mid(ch, it):
            ac = acc[it % 3]
            s = sbuf_s[it % 3]
            q2 = sq[it % 3]
            rs = rsb[it % 3]
            ob = outb[it % 4]
            if ch < 16:
                k.act(s, ac, AF.Silu)
                k.tt(q2, s, s, ALU.mult, eng="pool")
                pss = k.psum()
                k.mm(pss[0:1, :], C["ones"][:, 0:1], q2)
                k.act(rs[0:1, :], pss[0:1, :], AF.Ln, bias=1e-6, scale=1.0)
                k.act(rs[0:1, :], rs[0:1, :], AF.Exp, scale=-0.5)
            else:
                k.act(ob, ac, AF.Silu)

        def tail(ch, it):
            s = sbuf_s[it % 3]
            rs = rsb[it % 3]
            ob = outb[it % 4]
            if ch < 16:
                psb = k.psum()
                k.mm(psb, C["ones"][0:1, :], rs[0:1, :])
                k.stt(ob, s, (128.0 ** -0.5) if ch < 8 else 1.0, psb, ALU.mult, ALU.mult)
                dst = scr["qs"] if ch < 8 else scr["ks"]
                h = ch % 8
            else:
                dst = scr["vs"]
                h = ch - 16
            dv = dst.ap[4 * b:4 * b + 4, :, h, :].rearrange("n k t -> k n t")
            k._op("sp", (lambda dv, ob: lambda e: e.dma_start(out=dv, in_=ob.ap.rearrange("p (n t) -> p n t", t=128)))(dv, ob),
                  reads=[ob.key], writes=[dst.key + (4 * b + tt_, h) for tt_ in range(4)], dma=True)

        def gates(b):
            for t in range(4):
                n = 4 * b + t
                G = Gt[n % 2]
                a = ab[n % 2]
                for gi, (c0, w) in enumerate([(0, 512), (512, 512), (1024, 16)]):
                    ps = k.psum()
                    for c in range(8):
                        k.mm(ps[:, 0:w], XTb[:, c, t * 128:(t + 1) * 128], Wg[:, c, c0:c0 + w], start=(c == 0), stop=(c == 7))
                    if gi < 2:
                        k.act(junk[:, 0:512], ps, AF.Silu)
                        k.tt(G[:, c0:c0 + 512], junk[:, 0:512], cnb[:, c0:c0 + 512], ALU.mult, eng="pool")
                    else:
                        k.cp(a, ps[:, 0:16])
                k.dma("sp", scr["gs"].sub(n, n), G)
                k.act(BG[:, n, 8:16].k(n), a[:, 8:16], AF.Sigmoid)
                k.tt(zt, a[:, 0:8], dtb, ALU.add)
                k.act(zt, zt, AF.Exp)
                k.act(zt, zt, AF.Ln, bias=1.0, scale=1.0)
                k.tt(BG[:, n, 0:8].k(n), zt, nA, ALU.mult)

        HB, MB, TB, GB = (0, 3), (3, 5), (5, 7), (7, 8)
        seq = []
        seq += k.record(lambda: head(0, it), banks=HB)
        seq += k.merge_lists(k.record(lambda: mid(0, it), banks=MB), k.record(lambda: head(1, it + 1), banks=HB))
        for ch in range(24):
            tl = k.record(lambda: tail(ch, it), banks=TB)
            md = k.record(lambda: mid(ch + 1, it + 1), banks=MB) if ch + 1 < 24 else []
            hd_ops = k.record(lambda: head(ch + 2, it + 2), banks=HB) if ch + 2 < 24 else []
            seq += k.merge_lists(tl, k.merge_lists(md, hd_ops))
            it += 1
        gl = k.record(lambda: gates(b), banks=GB)
        k.issue_merged(seq, gl)
    k.reset(m0)


def gdn_pass_b(k, dr, C, C1, X, scr, ln_g, ln_b):
    k.P.barrier()
    m0 = k.mark()
    Wout = k.alloc("Wout", [8, 1024], BF16)
    wo = dr["w_out1"].rearrange("(c p) n -> p c n", p=128)
    for c in range(0, 8, 4):
        k.dma("pool", Wout.sub((slice(None), slice(c, c + 4), slice(None)), c), wo[:, c:c + 4, :])
    g_bc = k.alloc("ln_g", [1024])
    b_bc = k.alloc("ln_b", [1024])
    k.dma("sp", g_bc, ln_g.partition_broadcast(128))
    k.dma("sp", b_bc, ln_b.partition_broadcast(128))
    identb = k.alloc("identb", [128], BF16)
    k.cp(identb, C["ident"])
    BG = scr["BG"]
    qT = [k.alloc("qT%d" % i, [8, 128], BF16) for i in range(2)]
    kT = [k.alloc("kT%d" % i, [8, 128], BF16) for i in range(2)]
    vT = [k.alloc("vT%d" % i, [8, 128], BF16) for i in range(2)]
    Gt = [k.alloc("G%d" % i, [1024], BF16) for i in range(2)]
    sm = k.alloc("sm", [64])
    junk = k.alloc("junk", [8, 128])
    junk2 = k.alloc("junk2", [8, 128])
    egr_2 = [k.alloc("egr%d" % i_, [8, 128]) for i_ in range(2)]
    E = k.alloc("E", [8, 128])
    Dl = k.alloc("Dl", [8, 128])
    aqk_2 = [k.alloc("aqk%d" % i_, [8, 128], BF16) for i_ in range(2)]
    Pm = k.alloc("Pm", [8, 128])
    PT = k.alloc("PT", [8, 128])
    RT = k.alloc("RT", [8, 128])
    Tb = k.alloc("Tb", [8, 128], BF16)
    Pb = k.alloc("Pb", [8, 128], BF16)
    PTb = k.alloc("PTb", [8, 128], BF16)
    vb = k.alloc("vb", [8, 128], BF16)
    kbg = k.alloc("kbg", [8, 128], BF16)
    kdec_2 = [k.alloc("kdec%d" % i_, [8, 128], BF16) for i_ in range(2)]
    u32_2 = [k.alloc("u32%d" % i_, [8, 128]) for i_ in range(2)]
    wT_2 = [k.alloc("wT%d" % i_, [8, 128], BF16) for i_ in range(2)]
    qdT_2 = [k.alloc("qdT%d" % i_, [8, 128], BF16) for i_ in range(2)]
    vnew = k.alloc("vnew", [8, 128], BF16)
    o32 = k.alloc("o32", [8, 128])
    OT = k.alloc("OT", [8, 128], BF16)
    S32 = k.alloc("S32", [8, 128])
    Sb = k.alloc("Sb", [8, 128], BF16)
    st = k.alloc("st", [16])
    lnst = k.alloc("lnst", [8])
    k.memset(S32, 0.0)
    k.memset(Sb, 0.0, eng="pool")
    tri = C1["tri64"]
    HG = [(0, 4), (4, 8)]

    def g4(buf, hg):
        return B(buf.ap[:, hg * 4:hg * 4 + 4, :], buf.key + (hg,))

    def hd(buf, h):
        return B(buf.ap[:, h, :], buf.key + (h // 4,))

    def ps4(ps):
        return ps.re("p (a b) -> p a b", b=128)

    def load(n):
        i = n % 2
        k.dma("sp", qT[i], scr["qs"].sub(n, n))
        k.dma("sp", kT[i], scr["ks"].sub(n, n))
        k.dma("sp", vT[i], scr["vs"].sub(n, n))
        k.dma("sp", Gt[i], scr["gs"].sub(n, n))

    load(0)

    def prep(n):
        i = n % 2
        q_, k_, v_, G = qT[i], kT[i], vT[i], Gt[i]
        egr, aqk, kdec, u32, wT, qdT = egr_2[i], aqk_2[i], kdec_2[i], u32_2[i], wT_2[i], qdT_2[i]
        g32 = BG[:, n, 0:8].k(n)
        beta = BG[:, n, 8:16].k(n)
        ps = k.psum()
        k.mm(ps[:, 0:8], tri, g32)
        k.mm(ps[:, 8:16], C1["blk64"], g32)
        gc = sm[:, 0:8].k("gc")
        k.cp(gc, ps[:, 0:8])
        k.act(sm[:, 8:16].k("egc"), ps[:, 0:8], AF.Exp)
        k.tt(sm[:, 16:24].k("bge"), beta, sm[:, 8:16].k("egc"), ALU.mult)
        k.tt(sm[:, 24:32].k("dd"), ps[:, 8:16], gc, ALU.subtract)
        k.act(sm[:, 32:40].k("edec"), sm[:, 24:32].k("dd"), AF.Exp)
        k.cp(junk, g32.bc([128, 8, 128], 2))
        psR = [k.psum(), k.psum()]
        for h in range(8):
            k.mm(psR[h // 4][:, (h % 4) * 128:(h % 4 + 1) * 128], junk[:, h, :], tri)
        for hg in range(2):
            k.act(g4(egr, hg), ps4(psR[hg]), AF.Exp)
            k.tt(g4(E, hg), ps4(psR[hg]), gc[:, hg * 4:hg * 4 + 4].bc([128, 4, 128], 2), ALU.subtract)
        k.ts(Dl, E, 0.0, -1.0, ALU.max, ALU.mult)
        k.act(Dl, Dl, AF.Exp)
        k.tt(Dl, Dl, C1["nstrict64"].bc([128, 8, 128], 1), ALU.mult, eng="pool")
        k.tt(Dl, Dl, beta.bc([128, 8, 128], 2), ALU.mult)
        k.ts(E, E, 0.0, None, ALU.min)
        k.act(E, E, AF.Exp)
        k.tt(E, E, tri.bc([128, 8, 128], 1), ALU.mult, eng="pool")
        psKQ = [k.psum(), k.psum()]
        psKK = [k.psum(), k.psum()]
        for h in range(8):
            sl = slice((h % 4) * 128, (h % 4 + 1) * 128)
            k.mm(psKQ[h // 4][:, sl], k_[:, h, :], q_[:, h, :])
            k.mm(psKK[h // 4][:, sl], k_[:, h, :], k_[:, h, :])
        for hg in range(2):
            k.tt(g4(aqk, hg), ps4(psKQ[hg]), g4(E, hg), ALU.mult)
            k.tt(g4(Pm, hg), ps4(psKK[hg]), g4(Dl, hg), ALU.mult)
        psT = [k.psum(), k.psum()]
        for h in range(8):
            k.tr(psT[h // 4][:, (h % 4) * 128:(h % 4 + 1) * 128], hd(Pm, h), C["ident"])
        for hg in range(2):
            k.cp(g4(PT, hg), ps4(psT[hg]), eng="act")
            k.tt(g4(RT, hg), ps4(psT[hg]), C["ident"].bc([128, 4, 128], 1), ALU.add)
        for m in range(1, 6):
            lo = m >= 2
            Pa, PTa, RTa = (Pb, PTb, Tb) if lo else (Pm, PT, RT)
            psP = [k.psum(), k.psum()]
            psPT = [k.psum(), k.psum()] if m < 5 else None
            for hg in range(2):
                for h in range(hg * 4, hg * 4 + 4):
                    k.mm(psP[hg][:, (h % 4) * 128:(h % 4 + 1) * 128], hd(PTa, h), hd(Pa, h))
                if m < 5:
                    for h in range(hg * 4, hg * 4 + 4):
                        k.mm(psPT[hg][:, (h % 4) * 128:(h % 4 + 1) * 128], hd(Pa, h), hd(PTa, h))
            nlo = m >= 1
            Po, PTo = (Pb, PTb) if m >= 2 else (Pm, PT)
            for hg in range(2):
                k.cp(g4(Po, hg), ps4(psP[hg]), eng="act")
                if m < 5:
                    k.cp(g4(PTo, hg), ps4(psPT[hg]), eng="dve")
            psR2 = [k.psum(), k.psum()]
            for h in range(8):
                k.mm(psR2[h // 4][:, (h % 4) * 128:(h % 4 + 1) * 128], hd(Po, h), hd(RTa, h))
            for hg in range(2):
                k.tt(g4(RT, hg), g4(RT, hg), ps4(psR2[hg]), ALU.add)
                if m < 5:
                    k.cp(g4(Tb, hg), g4(RT, hg), eng="act")
                if m == 1:
                    k.cp(g4(Pb, hg), g4(Pm, hg), eng="act")
                    k.cp(g4(PTb, hg), g4(PT, hg), eng="pool")
        k.cp(Tb, RT, eng="act")
        psK = k.psum(BF16)
        psV = k.psum(BF16)
        for h in range(8):
            k.tr(psK[:, h * 128:(h + 1) * 128], k_[:, h, :], identb)
        for h in range(8):
            k.tr(psV[:, h * 128:(h + 1) * 128], v_[:, h, :], identb)
        pK = psK.re("p (a b) -> p a b", b=128)
        pV = psV.re("p (a b) -> p a b", b=128)
        k.tt(vb, pV, beta.bc([128, 8, 128], 2), ALU.mult)
        k.tt(kbg, pK, sm[:, 16:24].k("bge").bc([128, 8, 128], 2), ALU.mult)
        k.tt(kdec, pK, sm[:, 32:40].k("edec").bc([128, 8, 128], 2), ALU.mult)
        psU = [k.psum(), k.psum()]
        psW = [k.psum(), k.psum()]
        for h in range(8):
            sl = slice((h % 4) * 128, (h % 4 + 1) * 128)
            k.mm(psU[h // 4][:, sl], hd(Tb, h), hd(vb, h))
            k.mm(psW[h // 4][:, sl], hd(kbg, h), hd(Tb, h))
        for hg in range(2):
            k.cp(g4(u32, hg), ps4(psU[hg]), eng="act")
            k.cp(g4(wT, hg), ps4(psW[hg]), eng="dve")
        k.tt(qdT, q_, egr, ALU.mult, eng="pool")

    def rec(n):
        i = n % 2
        G = Gt[i]
        egr, aqk, kdec, u32, wT, qdT = egr_2[i], aqk_2[i], kdec_2[i], u32_2[i], wT_2[i], qdT_2[i]
        Xn = X.sub((slice(None), n, slice(None)), n)
        for c in range(2):
            r = slice(c * 64, c * 64 + 64)
            psWS = [k.psum(), k.psum()]
            for h in range(8):
                k.mm(psWS[h // 4][:, (h % 4) * 128:(h % 4 + 1) * 128], hd(wT, h), Sb[:, h, :])
            for hg in range(2):
                k.tt(vnew[r, hg * 4:hg * 4 + 4, :], u32[r, hg * 4:hg * 4 + 4, :], ps4(psWS[hg])[r], ALU.subtract)
            psS = [k.psum(), k.psum()]
            for h in range(8):
                k.mm(psS[h // 4][:, (h % 4) * 128:(h % 4 + 1) * 128], kdec[r, h, :], vnew[r, h, :])
            col = c * 64 + 63
            for h in range(8):
                k.stt(S32[:, h, :], S32[:, h, :], egr[:, h, col:col + 1], psS[h // 4][:, (h % 4) * 128:(h % 4 + 1) * 128],
                      ALU.mult, ALU.add)
            psO = [k.psum(), k.psum()]
            for h in range(8):
                po = psO[h // 4][:, (h % 4) * 128:(h % 4 + 1) * 128]
                k.mm(po, hd(qdT, h), Sb[:, h, :], start=True, stop=False)
                k.mm(po, aqk[r, h, :], vnew[r, h, :], start=False, stop=True)
            for hg in range(2):
                k.cp(o32[r, hg * 4:hg * 4 + 4, :], ps4(psO[hg])[r], eng="act")
            k.cp(Sb, S32, eng="act")
        k.tt(junk2, o32, o32, ALU.mult, eng="pool")
        k.red(st[:, 0:8].k("s2"), junk2)
        k.act(st[:, 0:8].k("s2"), st[:, 0:8].k("s2"), AF.Sqrt, bias=EPS, scale=1.0 / 128)
        k.recip(st[:, 8:16].k("rstd"), st[:, 0:8].k("s2"))
        k.tt(o32, o32, st[:, 8:16].k("rstd").bc([128, 8, 128], 2), ALU.mult)
        of = o32.re("p a b -> p (a b)")
        k.tt(of, of, G, ALU.mult)
        transpose_tile(k, C, of, OT)
        for half in range(2):
            ps = k.psum()
            for c in range(8):
                k.mm(ps, OT[:, c, :], Wout[:, c, half * 512:(half + 1) * 512], start=(c == 0), stop=(c == 7))
            k.stt(Xn[:, half * 512:(half + 1) * 512], Xn[:, half * 512:(half + 1) * 512], ALPHA, ps, ALU.mult, ALU.add)
        ln_tile(k, Xn, g_bc, b_bc, Xn, lnst, junk2.re("p a b -> p (a b)"))

    pr = k.record(lambda: prep(0), banks=(0, 5))
    k.issue_merged(pr, [])
    for n in range(NT):
        if n + 1 < NT:
            load(n + 1)
            pr = k.record(lambda: prep(n + 1), banks=(0, 5))
        else:
            pr = []
        rc_ = k.record(lambda: rec(n), banks=(5, 8))
        k.issue_merged(rc_, pr)
    k.reset(m0)


ARENA = 53120
SPARSE_MOE = True


def wgu_layout(w):
    g = w[:, :3584].reshape(8, 128, 28, 128)
    u = w[:, 3584:].reshape(8, 128, 28, 128)
    gu = np.concatenate([g, u], axis=3)
    return np.ascontiguousarray(gu.transpose(2, 1, 0, 3))


def shared_inputs(inp):
    m = dict(host_consts())
    m.update(host_consts_l1())
    m["inv_freq"] = m["inv_freq"].reshape(32)
    m["w_in0"] = np.ascontiguousarray(inp["ab_w_in"][0][:, l0_perm()])
    m["w_out0"] = np.ascontiguousarray(inp["ab_w_out"][0])
    m["w_gate2"] = np.ascontiguousarray(inp["gla_w_gate2"][0])
    m["b_gate"] = np.ascontiguousarray(inp["gla_b_gate"][0].reshape(1, 256))
    m["norm0"] = np.concatenate([inp["gla_norm"][0].reshape(512), inp["ret_norm"][0].reshape(512)])
    m["ln1_g"] = np.ascontiguousarray(inp["ab_ln1_g"][0]); m["ln1_b"] = np.ascontiguousarray(inp["ab_ln1_b"][0])
    m["ln2_g"] = np.ascontiguousarray(inp["ab_ln2_g"][0]); m["ln2_b"] = np.ascontiguousarray(inp["ab_ln2_b"][0])
    m["wgu0"] = wgu_layout(inp["ffn_w_gu"][0]); m["wd0"] = np.ascontiguousarray(inp["ffn_w_down"][0])
    cw = inp["c_w_in"][0]
    m["wqkv_l"] = np.ascontiguousarray(cw[:, 0:3072].reshape(8, 128, 24, 128).transpose(2, 1, 0, 3))
    m["wg_l"] = np.ascontiguousarray(np.concatenate([cw[:, 3088:4112], cw[:, 3072:3088]], axis=1))
    m["convw_l"] = np.ascontiguousarray(inp["c_conv_w"][0].reshape(4, 24, 128).transpose(2, 1, 0))
    m["c_a_log"] = np.ascontiguousarray(inp["c_a_log"][0]); m["c_dt_bias"] = np.ascontiguousarray(inp["c_dt_bias"][0])
    m["c_norm"] = np.ascontiguousarray(inp["c_norm"][0].reshape(1024))
    m["w_out1"] = np.ascontiguousarray(inp["c_w_out"][0])
    m["ln3_g"] = np.ascontiguousarray(inp["c_ln1_g"][0]); m["ln3_b"] = np.ascontiguousarray(inp["c_ln1_b"][0])
    m["ln4_g"] = np.ascontiguousarray(inp["c_ln2_g"][0]); m["ln4_b"] = np.ascontiguousarray(inp["c_ln2_b"][0])
    m["w_router"] = np.ascontiguousarray(inp["moe_w_router"][0]); m["b_router"] = np.ascontiguousarray(inp["moe_b_router"][0].reshape(1, 8))
    for e in range(8):
        m["mgu%d" % e] = wgu_layout(inp["moe_w_gu"][0, e])
        m["mwd%d" % e] = np.ascontiguousarray(inp["moe_w_down"][0, e])
    return m


IN_SPECS = [("x", [T, D], F32), ("pos", [128, 16], I32)] + \
    [(n, [128, 128], F32) for n in ("ident", "tri", "upp", "ones", "tri64", "blk64", "nstrict64", "slt")] + [("iota512", [128, 512], F32)] + \
    [("ret_eqk", [128, 512], F32), ("ret_ee", [128, 256], F32), ("ret_ds", [128, 2], F32), ("inv_freq", [32], F32),
     ("w_in0", [1024, 3088], F32), ("w_out0", [1024, 1024], F32), ("w_gate2", [16, 256], F32), ("b_gate", [1, 256], F32),
     ("norm0", [1024], F32)] + [("ln%d_%s" % (i, s), [1024], F32) for i in (1, 2, 3, 4) for s in "gb"] + \
    [("wgu0", [28, 128, 8, 256], F32), ("wd0", [3584, 1024], F32),
     ("wqkv_l", [24, 128, 8, 128], F32), ("wg_l", [1024, 1040], F32), ("convw_l", [128, 24, 4], F32),
     ("c_a_log", [8], F32), ("c_dt_bias", [8], F32), ("c_norm", [1024], F32), ("w_out1", [1024, 1024], F32),
     ("w_router", [1024, 8], F32), ("b_router", [1, 8], F32)] + \
    [("mgu%d" % e, [28, 128, 8, 256], F32) for e in range(8)] + [("mwd%d" % e, [3584, 1024], F32) for e in range(8)]


def build_program(phases=("l0", "ffn", "gdn", "moe"), need=None):
    nc = bass.Bass("TRN2", target_bir_lowering=False)
    dr = {}
    for (n, shp, dt) in IN_SPECS:
        if need is None or n in need:
            dr[n] = nc.dram_tensor(n, list(shp), dt, kind="ExternalInput").ap()
    out = nc.dram_tensor("out", [T, D], F32, kind="ExternalOutput").ap()
    scr = {}
    for n in ("qs", "ks", "vs"):
        scr[n] = B(nc.dram_tensor("scr_" + n, [16, 128, 8, 128], BF16).ap(), n)
    scr["gs"] = B(nc.dram_tensor("scr_gs", [16, 128, 1024], BF16).ap(), "gs")
    with contextlib.ExitStack() as es:
        arena = es.enter_context(nc.sbuf_tensor("arena", [128, ARENA], F32))
        banks = [es.enter_context(nc.psum_tensor("pb%d" % i, [128, 512], F32)) for i in range(8)]
        k = K(nc, arena, ARENA, [b[:, :] for b in banks])
        C = load_consts(k, dr)
        X = k.alloc("X", [16, 1024])
        for n in range(NT):
            k.dma("sp", X.sub((slice(None), n, slice(None)), n), dr["x"][n * 128:(n + 1) * 128, :])
        last = phases[-1]
        if "l0" in phases:
            phase_l0(k, dr, C, X)
        if "ffn" in phases:
            phase_ffn(k, dr, C, X, [(dr["wgu0"], dr["wd0"])], dr["ln2_g"], dr["ln2_b"],
                      out_dram=out if last == "ffn" else None)
        if "gdn" in phases:
            k.P.barrier()
            C1 = {}
            for n in ("tri64", "blk64", "nstrict64"):
                C1[n] = k.alloc(n, [128])
                k.dma("sp", C1[n], dr[n])
            scr["BG"] = k.alloc("BG", [16, 16])
            gdn_pass_a(k, dr, C, X, scr)
            gdn_pass_b(k, dr, C, C1, X, scr, dr["ln3_g"], dr["ln3_b"])
        if "moe" in phases:
            if SPARSE_MOE:
                phase_moe_sparse(k, dr, C, X, dr["ln4_g"], dr["ln4_b"], out)
            else:
                phase_ffn(k, dr, C, X, [(dr["mgu%d" % e], dr["mwd%d" % e]) for e in range(8)], dr["ln4_g"], dr["ln4_b"],
                          out_dram=out, router=(dr["w_router"], dr["b_router"]))
        if last not in ("ffn", "moe"):
            for n in range(NT):
                k.dma("sp", out[n * 128:(n + 1) * 128, :], X.sub((slice(None), n, slice(None)), n))
        print("ops", len(k.P.ops), "arena peak?", k.off, flush=True)
        k.P.emit()
    return nc


def phase_moe_sparse(k, dr, C, X, ln_g, ln_b, out_dram):
    BLK = 512
    k.P.barrier()
    m0 = k.mark()
    Xb = k.alloc("Xb", [16, 1024], BF16)
    M = k.alloc("M", [16, 8])
    CB = k.alloc("CB", [16, 8])
    RK = k.alloc("RK", [16, 8])
    OFF = k.alloc("OFF", [17, 8])
    BLOCKS = [(0, 512), (512, 128), (640, 128), (768, 256), (1024, 512), (1536, 512)]
    NB = len(BLOCKS)
    Ff = k.alloc("Ff", [NB, 8])
    Fi = k.alloc("Fi", [NB, 8], I32)
    iota = k.alloc("iota", [512])
    k.dma("sp", iota, dr["iota512"])
    slt = k.alloc("slt", [128])
    k.dma("sp", slt, dr["slt"])
    m1 = k.mark()
    Wr = k.alloc("Wr", [8, 8])
    k.dma("sp", Wr, dr["w_router"].rearrange("(c p) e -> p c e", p=128))
    br = k.alloc("br", [8])
    k.dma("sp", br[0:1, :], dr["b_router"])
    XT32 = k.alloc("XT32", [8, 128])
    rt = k.alloc("rt", [64])
    for n in range(NT):
        Xn = X.sub((slice(None), n, slice(None)), n)
        for g in range(2):
            ps = k.psum()
            for c in range(4):
                k.tr(ps[:, c * 128:(c + 1) * 128], Xn[:, (g * 4 + c) * 128:(g * 4 + c + 1) * 128], C["ident"])
            k.cp(XT32[:, g * 4:g * 4 + 4, :], ps.re("p (a b) -> p a b", b=128), eng="dve" if g else "act")
        ps = k.psum()
        for c in range(8):
            k.mm(ps[:, 0:8], XT32[:, c, :], Wr[:, c, :], start=(c == 0), stop=False)
        k.mm(ps[:, 0:8], C["ones"][0:1, :], br[0:1, :], start=False, stop=True)
        lg = rt[:, 0:8].k("lg")
        k.cp(lg, ps[:, 0:8])
        k.red(rt[:, 8:9].k("m1"), lg, op=ALU.max)
        k.ts(rt[:, 16:24].k("eq1"), lg, rt[:, 8:9].k("m1"), None, ALU.is_equal)
        k.stt(rt[:, 24:32].k("l2"), rt[:, 16:24].k("eq1"), -1e30, lg, ALU.mult, ALU.add)
        k.red(rt[:, 9:10].k("m2"), rt[:, 24:32].k("l2"), op=ALU.max)
        k.ts(rt[:, 32:40].k("eq2"), rt[:, 24:32].k("l2"), rt[:, 9:10].k("m2"), None, ALU.is_equal)
        k.tt(rt[:, 10:11].k("d"), rt[:, 9:10].k("m2"), rt[:, 8:9].k("m1"), ALU.subtract)
        k.act(rt[:, 11:12].k("e"), rt[:, 10:11].k("d"), AF.Exp)
        k.ts(rt[:, 12:13].k("den"), rt[:, 11:12].k("e"), 1.0, None, ALU.add)
        k.recip(rt[:, 13:14].k("g1"), rt[:, 12:13].k("den"))
        k.tt(rt[:, 14:15].k("g2"), rt[:, 11:12].k("e"), rt[:, 13:14].k("g1"), ALU.mult)
        cbn = CB[:, n, :].k(n)
        k.ts(cbn, rt[:, 16:24].k("eq1"), rt[:, 13:14].k("g1"), None, ALU.mult)
        k.stt(cbn, rt[:, 32:40].k("eq2"), rt[:, 14:15].k("g2"), cbn, ALU.mult, ALU.add)
        k.tt(M[:, n, :].k(n), rt[:, 16:24].k("eq1"), rt[:, 32:40].k("eq2"), ALU.add)
        k.cp(Xb[:, n, :].k(n), Xn, eng="act")
        k.act(Xn, Xn, AF.Identity, scale=ALPHA)
    Mf = M.re("p a b -> p (a b)")
    ps = k.psum()
    k.mm(ps[:, 0:128], slt, Mf)
    k.mm(ps[:, 128:256], C["ones"], Mf)
    TOT = k.alloc("TOT", [16, 8])
    k.cp(RK.re("p a b -> p (a b)"), ps[:, 0:128])
    k.cp(TOT.re("p a b -> p (a b)"), ps[:, 128:256])
    k.memset(OFF[:, 0, :].k(0), 0.0)
    for t in range(16):
        k.tt(OFF[:, t + 1, :].k(t + 1), OFF[:, t, :].k(t), TOT[:, t, :], ALU.add)
    k.tt(RK, RK, OFF[:, 0:16, :], ALU.add)
    for j, (boff, bw) in enumerate(BLOCKS):
        k.ts(Ff[:, j, :], OFF[:, 16, :].k(16), float(boff), None, ALU.is_gt)
    k.cp(Fi, Ff)
    k.P.barrier()
    k.reset(m1)
    Sel = k.alloc("Sel", [16, 512], BF16)
    SelT = k.alloc("SelT", [4, 8, 128], BF16)
    XTe = k.alloc("XTe", [8, 512], BF16)
    actb = k.alloc("actb", [28, 512], BF16)
    Ye = k.alloc("Ye", [4, 1024], BF16)
    NW = 3
    wgu = [k.alloc("wgu%d" % i, [8, 256], BF16) for i in range(NW)]
    ND = 4
    wdb = [k.alloc("wd%d" % i, [1024], BF16) for i in range(ND)]
    sg = [k.alloc("sg%d" % i, [512]) for i in range(2)]
    rj = k.alloc("rj", [16])
    identb = k.alloc("identb", [128], BF16)
    k.cp(identb, C["ident"])
    wi = di = si = 0
    for e in range(8):
        wgu_l = dr["mgu%d" % e]
        wd = dr["mwd%d" % e]
        for j, (boff, W) in enumerate(BLOCKS):
            NS = W // 128
            k.P.begin_region(Fi.ap[0:1, j, e:e + 1], Fi.key)
            k.ts(rj, RK[:, :, e], -float(boff), None, ALU.add)
            for t in range(16):
                k.ts(Sel[:, t, 0:W].k(t), iota[:, 0:W], rj[:, t:t + 1], M[:, t, e:e + 1], ALU.is_equal, ALU.mult)
            for c in range(8):
                ps = k.psum()
                for t in range(16):
                    k.mm(ps[:, 0:W], Xb[:, t, c * 128:(c + 1) * 128], Sel[:, t, 0:W].k(t), start=(t == 0), stop=(t == 15))
                k.cp(XTe[:, c, 0:W].k(c), ps[:, 0:W], eng="act" if c % 2 else "dve")
            for f in range(28):
                wb = wgu[wi % NW]
                wi += 1
                k.dma("pool", wb, wgu_l[f])
                psG = k.psum()
                psU = k.psum()
                for c in range(8):
                    k.mm(psG[:, 0:W], wb[:, c, 0:128], XTe[:, c, 0:W].k(c), start=(c == 0), stop=(c == 7))
                for c in range(8):
                    k.mm(psU[:, 0:W], wb[:, c, 128:256], XTe[:, c, 0:W].k(c), start=(c == 0), stop=(c == 7))
                s = sg[si % 2]
                si += 1
                k.act(s[:, 0:W], psG[:, 0:W], AF.Silu)
                k.tt(actb[:, f, 0:W].k(f), s[:, 0:W], psU[:, 0:W], ALU.mult)
            pss = [k.psum() for _ in range(2 * NS)]
            for f in range(28):
                db = wdb[di % ND]
                di += 1
                k.dma("pool", db, wd[f * 128:(f + 1) * 128, :])
                for s4 in range(NS):
                    for hh in range(2):
                        k.mm(pss[s4 * 2 + hh], actb[:, f, s4 * 128:(s4 + 1) * 128].k(f), db[:, hh * 512:(hh + 1) * 512],
                             start=(f == 0), stop=(f == 27))
            for s4 in range(NS):
                for hh in range(2):
                    k.cp(Ye[:, s4, hh * 512:(hh + 1) * 512].k(s4, hh), pss[s4 * 2 + hh], eng="act" if hh else "dve")
            for th in range(2):
                for s4 in range(NS):
                    psT = k.psum(BF16)
                    for t8 in range(8):
                        t = th * 8 + t8
                        k.tr(psT[:, t8 * 128:(t8 + 1) * 128], Sel[:, t, s4 * 128:(s4 + 1) * 128].k(t), identb)
                    k.cp(SelT[:, s4, :, :].k(s4), psT.re("p (a b) -> p a b", b=128), eng="act" if s4 % 2 else "dve")
                for t8 in range(8):
                    t = th * 8 + t8
                    Xn = X.sub((slice(None), t, slice(None)), t)
                    for hh in range(2):
                        ps = k.psum()
                        for s4 in range(NS):
                            k.mm(ps, SelT[:, s4, t8, :].k(s4), Ye[:, s4, hh * 512:(hh + 1) * 512].k(s4, hh),
                                 start=(s4 == 0), stop=(s4 == NS - 1))
                        xs = Xn[:, hh * 512:(hh + 1) * 512]
                        k.stt(xs, ps, CB[:, t, e:e + 1].k(t), xs, ALU.mult, ALU.add)
            k.P.end_region()
    k.P.barrier()
    k.reset(m1)
    g_bc = k.alloc("ln_g", [1024])
    b_bc = k.alloc("ln_b", [1024])
    k.dma("sp", g_bc, ln_g.partition_broadcast(128))
    k.dma("sp", b_bc, ln_b.partition_broadcast(128))
    junk = k.alloc("junk", [1024])
    lnst = k.alloc("lnst", [8])
    for n in range(NT):
        Xn = X.sub((slice(None), n, slice(None)), n)
        ln_tile(k, Xn, g_bc, b_bc, Xn, lnst, junk)
        k.dma("sp", out_dram[n * 128:(n + 1) * 128, :], Xn)
    k.reset(m0)


_NC_CACHE = {}


def kernel(**inputs):
    inp = {k_: np.asarray(v) for k_, v in inputs.items()}
    shared = shared_inputs(inp)
    if "nc" not in _NC_CACHE:
        _NC_CACHE["nc"] = build_program(("l0", "ffn", "gdn", "moe"))
    nc = _NC_CACHE["nc"]
    in_maps = []
    for b in range(8):
        m = dict(shared)
        m["x"] = np.ascontiguousarray(inp["x"][b], dtype=np.float32)
        m["pos"] = np.ascontiguousarray(inp["positions"][b].astype(np.int32).reshape(16, 128).T)
        in_maps.append(m)
    res = run_bass_kernel_spmd(nc, in_maps, core_ids=list(range(8)))
    return np.stack([np.asarray(r["out"], dtype=np.float32) for r in res.results], axis=0)
```
